# Optimizing a Trainium2 kernel written in Bass

```python
import math
import jax
import jax.numpy as jnp
from jax import lax
import numpy as np

D_MODEL = 4096
BATCH = 4
SEQ = 2048
DEPTH = 1

SSM_HEAD_DIM = 64
SSM_D_INNER = D_MODEL // 2
SSM_HEADS = SSM_D_INNER // SSM_HEAD_DIM
SSM_HEADS_PER_GROUP = 4
SSM_GROUPS = SSM_HEADS // SSM_HEADS_PER_GROUP
SSM_STATE = 128
SSM_CONV = 5
SSM_CHUNK = 128
SSM_CONV_DIM = SSM_D_INNER + 2 * SSM_GROUPS * SSM_STATE
DT_MIN = 0.001
DT_MAX = 0.1

ATTN_PATTERNS = ((128, 1), (512, 4), (2048, 16))
ATTN_HEAD_DIM = 128
ATTN_HEADS_PER_GROUP = D_MODEL // 512
ATTN_HEADS = len(ATTN_PATTERNS) * ATTN_HEADS_PER_GROUP
ATTN_WIDTH = ATTN_HEADS * ATTN_HEAD_DIM
ATTN_OUT_WIDTH = ATTN_HEADS_PER_GROUP * ATTN_HEAD_DIM
ROPE_THETA = 10000.0
NEG_INF = -1e30

N_EXPERTS = 16
EXPERT_FF = D_MODEL // 2
CAPACITY_FACTOR = 2

N_MOD = 6
NORM_EPS = 1e-6

IN_SIZES = (SSM_D_INNER, SSM_CONV_DIM, 2 * SSM_HEADS, ATTN_WIDTH, ATTN_WIDTH, ATTN_WIDTH, 2 * D_MODEL)
IN_DIM = sum(IN_SIZES)

kernel_name = 'hybrid_ssd_dilated_attn_ec_moe_block'


def rms_norm(t, w):
    t32 = t.astype(jnp.float32)
    y = t32 * lax.rsqrt(jnp.mean(t32 * t32, axis=-1, keepdims=True) + NORM_EPS)
    return (y * w.astype(jnp.float32)).astype(t.dtype)


def split_sizes(t, sizes):
    offs = []
    acc = 0
    for s in sizes[:-1]:
        acc += s
        offs.append(acc)
    return jnp.split(t, offs, axis=-1)


def rope(t, pos):
    half = t.shape[-1] // 2
    inv_freq = ROPE_THETA ** (-jnp.arange(half, dtype=jnp.float32) / half)
    ang = pos[:, None] * inv_freq[None, :]
    cos = jnp.cos(ang)[None, :, None, :]
    sin = jnp.sin(ang)[None, :, None, :]
    t32 = t.astype(jnp.float32)
    t1, t2 = t32[..., :half], t32[..., half:]
    return jnp.concatenate([t1 * cos - t2 * sin, t2 * cos + t1 * sin], axis=-1).astype(t.dtype)


def centred_depthwise_conv(t, w, b):
    ch = t.shape[-1]
    width = w.shape[0]
    out = lax.conv_general_dilated(
        t, w[:, None, :].astype(t.dtype), window_strides=(1,),
        padding=((width // 2, width // 2),),
        dimension_numbers=('NWC', 'WIO', 'NWC'), feature_group_count=ch)
    return out + b.astype(t.dtype)


def segsum(a):
    T = a.shape[-1]
    cs = jnp.cumsum(a, axis=-1)
    diff = cs[..., :, None] - cs[..., None, :]
    mask = jnp.tril(jnp.ones((T, T), dtype=bool))
    return jnp.where(mask, diff, -jnp.inf)


def ssd_chunked(xs, dt, a, bm, cm):
    Bsz, S, H, P = xs.shape
    G, N = bm.shape[2], bm.shape[3]
    K = H // G
    T = SSM_CHUNK
    nc = -(-S // T)
    Sp = nc * T
    pad = ((0, 0), (0, Sp - S))
    xd = jnp.pad(xs.astype(jnp.float32) * dt[..., None], pad + ((0, 0), (0, 0)))
    da = jnp.pad(dt * a, pad + ((0, 0),))
    bc = jnp.pad(bm.astype(jnp.float32), pad + ((0, 0), (0, 0))).reshape(Bsz, nc, T, G, N)
    cc = jnp.pad(cm.astype(jnp.float32), pad + ((0, 0), (0, 0))).reshape(Bsz, nc, T, G, N)
    xc = xd.reshape(Bsz, nc, T, G, K, P)
    dac = da.reshape(Bsz, nc, T, G, K).transpose(0, 3, 4, 1, 2)
    a_cs = jnp.cumsum(dac, axis=-1)
    lmat = jnp.exp(segsum(dac))
    cb = jnp.einsum('bclgn,bcsgn->bcgls', cc, bc)
    y_diag = jnp.einsum('bcgls,bgkcls,bcsgkp->bclgkp', cb, lmat, xc)
    decay_states = jnp.exp(a_cs[..., -1:] - a_cs)
    states = jnp.einsum('bclgn,bgkcl,bclgkp->bcgkpn', bc, decay_states, xc)
    states = jnp.concatenate([jnp.zeros_like(states[:, :1]), states], axis=1)
    chunk_decay = jnp.exp(segsum(jnp.pad(a_cs[..., -1], ((0, 0), (0, 0), (0, 0), (1, 0)))))
    states = jnp.einsum('bgkzc,bcgkpn->bzgkpn', chunk_decay, states)[:, :-1]
    y_off = jnp.einsum('bclgn,bcgkpn,bgkcl->bclgkp', cc, states, jnp.exp(a_cs))
    y = (y_diag + y_off).reshape(Bsz, Sp, H, P)[:, :S]
    return y.astype(xs.dtype)


def dilated_window_attention(q, k, v, dilation, radius):
    Bsz, S, H, E = q.shape
    L = -(-S // dilation)
    Sp = L * dilation
    nb = -(-L // radius)
    Lp = nb * radius

    def to_strided(t):
        t = jnp.pad(t, ((0, 0), (0, Sp - S), (0, 0), (0, 0)))
        t = t.reshape(Bsz, L, dilation, H, E).transpose(0, 2, 3, 1, 4)
        return jnp.pad(t, ((0, 0), (0, 0), (0, 0), (0, Lp - L), (0, 0)))

    def band(t):
        t = jnp.pad(t, ((0, 0), (0, 0), (0, 0), (radius, radius), (0, 0)))
        t = t.reshape(Bsz, dilation, H, nb + 2, radius, E)
        return jnp.concatenate([t[:, :, :, :-2], t[:, :, :, 1:-1], t[:, :, :, 2:]], axis=4)

    qb = to_strided(q).reshape(Bsz, dilation, H, nb, radius, E)
    kb = band(to_strided(k))
    vb = band(to_strided(v))
    scores = jnp.einsum('bdhnqe,bdhnke->bdhnqk', qb, kb).astype(jnp.float32) * (E ** -0.5)
    t_q = jnp.arange(nb)[:, None] * radius + jnp.arange(radius)[None, :]
    t_k = (jnp.arange(nb)[:, None] - 1) * radius + jnp.arange(3 * radius)[None, :]
    r_idx = jnp.arange(dilation)[:, None, None, None]
    valid = ((jnp.abs(t_k[:, None, :] - t_q[:, :, None]) <= radius)[None]
             & (t_k >= 0)[None, :, None, :]
             & (t_k[None, :, None, :] * dilation + r_idx < S))
    scores = jnp.where(valid[None, :, None], scores, NEG_INF)
    m = jnp.max(scores, axis=-1, keepdims=True)
    p = jnp.exp(scores - m)
    s = jnp.sum(p, axis=-1)
    o = jnp.einsum('bdhnqk,bdhnke->bdhnqe', p, vb.astype(jnp.float32)) / s[..., None]
    lse = m[..., 0] + jnp.log(s)
    o = o.reshape(Bsz, dilation, H, Lp, E)[:, :, :, :L].transpose(0, 3, 1, 2, 4)
    o = o.reshape(Bsz, Sp, H, E)[:, :S]
    lse = lse.reshape(Bsz, dilation, H, Lp)[:, :, :, :L].transpose(0, 3, 1, 2)
    lse = lse.reshape(Bsz, Sp, H)[:, :S]
    return o, lse


def hybrid_mixer(h, w_in, conv_w, conv_b, dt_bias_f, dt_bias_b, a_log_f, a_log_b, d_skip,
                 ssm_norm_w, w_ssm_out, w_attn_out, w_o):
    Bsz, S, _ = h.shape
    proj = jnp.einsum('bsd,de->bse', h, w_in)
    z, xbc, dt_raw, q, k, v, gates = split_sizes(proj, IN_SIZES)

    xbc = jax.nn.silu(centred_depthwise_conv(xbc, conv_w, conv_b))
    xs, b_ssm, c_ssm = split_sizes(xbc, (SSM_D_INNER, SSM_GROUPS * SSM_STATE, SSM_GROUPS * SSM_STATE))
    xs = xs.reshape(Bsz, S, SSM_HEADS, SSM_HEAD_DIM)
    b_ssm = b_ssm.reshape(Bsz, S, SSM_GROUPS, SSM_STATE)
    c_ssm = c_ssm.reshape(Bsz, S, SSM_GROUPS, SSM_STATE)
    dt_f, dt_b = jnp.split(dt_raw.astype(jnp.float32), 2, axis=-1)
    dt_f = jax.nn.softplus(dt_f + dt_bias_f.astype(jnp.float32))
    dt_b = jax.nn.softplus(dt_b + dt_bias_b.astype(jnp.float32))
    a_f = -jnp.exp(a_log_f.astype(jnp.float32))
    a_b = -jnp.exp(a_log_b.astype(jnp.float32))
    flip = lambda t: jnp.flip(t, axis=1)
    y_f = ssd_chunked(xs, dt_f, a_f, b_ssm, c_ssm)
    y_b = flip(ssd_chunked(flip(xs), flip(dt_b), a_b, flip(b_ssm), flip(c_ssm)))
    y = y_f + y_b + d_skip.astype(xs.dtype)[:, None] * xs
    y = y.reshape(Bsz, S, SSM_D_INNER) * jax.nn.silu(z)
    y_ssm = jnp.einsum('bse,ed->bsd', rms_norm(y, ssm_norm_w), w_ssm_out)

    pos = jnp.arange(S, dtype=jnp.float32)
    q = rope(q.reshape(Bsz, S, ATTN_HEADS, ATTN_HEAD_DIM), pos)
    k = rope(k.reshape(Bsz, S, ATTN_HEADS, ATTN_HEAD_DIM), pos)
    v = v.reshape(Bsz, S, ATTN_HEADS, ATTN_HEAD_DIM)
    outs, lses = [], []
    for g, (window, dilation) in enumerate(ATTN_PATTERNS):
        sl = slice(g * ATTN_HEADS_PER_GROUP, (g + 1) * ATTN_HEADS_PER_GROUP)
        o_g, lse_g = dilated_window_attention(q[:, :, sl], k[:, :, sl], v[:, :, sl],
                                              dilation, window // (2 * dilation))
        outs.append(o_g)
        lses.append(lse_g)
    mix = jax.nn.softmax(jnp.stack(lses, axis=0), axis=0)
    o = jnp.einsum('gbsh,gbshe->bshe', mix, jnp.stack(outs, axis=0)).astype(h.dtype)
    y_att = jnp.einsum('bse,ed->bsd', o.reshape(Bsz, S, ATTN_OUT_WIDTH), w_attn_out)

    g_ssm, g_att = jnp.split(jax.nn.sigmoid(gates), 2, axis=-1)
    merged = g_ssm * y_ssm + g_att * y_att
    return jnp.einsum('bsd,de->bse', merged, w_o)


def expert_choice_moe(h, w_router, w_gate_e, w_up_e, w_down_e):
    Bsz, S, D = h.shape
    cap = max(1, min(S, CAPACITY_FACTOR * S // N_EXPERTS))
    logits = jnp.einsum('bsd,de->bse', h, w_router).astype(jnp.float32)
    aff = jax.nn.softmax(logits, axis=-1)
    vals, idx = lax.top_k(jnp.swapaxes(aff, 1, 2), cap)
    xe = jax.vmap(lambda hb, ib: hb[ib])(h, idx)
    gate = jnp.einsum('becd,edf->becf', xe, w_gate_e)
    up = jnp.einsum('becd,edf->becf', xe, w_up_e)
    out = jnp.einsum('becf,efd->becd', jax.nn.silu(gate) * up, w_down_e)
    out = out * vals.astype(out.dtype)[..., None]
    return jax.vmap(lambda ib, ob: jnp.zeros((S, D), ob.dtype).at[ib.reshape(-1)].add(ob.reshape(-1, D)))(idx, out)


def setup_inputs(seed: int = 0) -> dict:
    key = jax.random.key(seed)
    ks = jax.random.split(key, 32)
    D = D_MODEL
    f32 = jnp.float32

    def nrm(k, shape, fan_in):
        return jax.random.normal(k, shape, f32) * (fan_in ** -0.5)

    def gain(k, shape):
        return 1.0 + 0.02 * jax.random.normal(k, shape, f32)

    def dt_bias(k):
        u = jax.random.uniform(k, (DEPTH, SSM_HEADS), f32)
        dt = jnp.exp(u * (math.log(DT_MAX) - math.log(DT_MIN)) + math.log(DT_MIN))
        return dt + jnp.log(-jnp.expm1(-dt))

    def a_log(k):
        return jnp.log(jax.random.uniform(k, (DEPTH, SSM_HEADS), f32, minval=1.0, maxval=16.0))

    return {
        'x': jax.random.normal(ks[0], (BATCH, SEQ, D), f32),
        'c': jax.random.normal(ks[1], (BATCH, D), f32),
        'norm1_w': gain(ks[2], (DEPTH, D)),
        'norm2_w': gain(ks[3], (DEPTH, D)),
        'normf_w': gain(ks[4], (D,)),
        'w_ada': nrm(ks[5], (DEPTH, D, N_MOD * D), D),
        'b_ada': 0.02 * jax.random.normal(ks[6], (DEPTH, N_MOD * D), f32),
        'w_in': nrm(ks[7], (DEPTH, D, IN_DIM), D),
        'conv_w': nrm(ks[8], (DEPTH, SSM_CONV, SSM_CONV_DIM), SSM_CONV),
        'conv_b': 0.02 * jax.random.normal(ks[9], (DEPTH, SSM_CONV_DIM), f32),
        'dt_bias_f': dt_bias(ks[10]),
        'dt_bias_b': dt_bias(ks[11]),
        'a_log_f': a_log(ks[12]),
        'a_log_b': a_log(ks[13]),
        'd_skip': 1.0 + 0.1 * jax.random.normal(ks[14], (DEPTH, SSM_HEADS), f32),
        'ssm_norm_w': gain(ks[15], (DEPTH, SSM_D_INNER)),
        'w_ssm_out': nrm(ks[16], (DEPTH, SSM_D_INNER, D), SSM_D_INNER),
        'w_attn_out': nrm(ks[17], (DEPTH, ATTN_OUT_WIDTH, D), ATTN_OUT_WIDTH),
        'w_o': nrm(ks[18], (DEPTH, D, D), D),
        'w_router': nrm(ks[19], (DEPTH, D, N_EXPERTS), D),
        'w_gate_e': nrm(ks[20], (DEPTH, N_EXPERTS, D, EXPERT_FF), D),
        'w_up_e': nrm(ks[21], (DEPTH, N_EXPERTS, D, EXPERT_FF), D),
        'w_down_e': nrm(ks[22], (DEPTH, N_EXPERTS, EXPERT_FF, D), EXPERT_FF),
    }


def reference(x, c, norm1_w, norm2_w, normf_w, w_ada, b_ada, w_in, conv_w, conv_b,
              dt_bias_f, dt_bias_b, a_log_f, a_log_b, d_skip, ssm_norm_w, w_ssm_out,
              w_attn_out, w_o, w_router, w_gate_e, w_up_e, w_down_e):
    c_act = jax.nn.silu(c)
    for layer in range(DEPTH):
        mod = jnp.einsum('bd,de->be', c_act, w_ada[layer]) + b_ada[layer]
        shift1, scale1, gate1, shift2, scale2, gate2 = [m[:, None, :] for m in jnp.split(mod, N_MOD, axis=-1)]
        h = rms_norm(x, norm1_w[layer]) * (1.0 + scale1) + shift1
        mixed = hybrid_mixer(h, w_in[layer], conv_w[layer], conv_b[layer], dt_bias_f[layer],
                             dt_bias_b[layer], a_log_f[layer], a_log_b[layer], d_skip[layer],
                             ssm_norm_w[layer], w_ssm_out[layer], w_attn_out[layer], w_o[layer])
        x = x + gate1 * mixed
        h = rms_norm(x, norm2_w[layer]) * (1.0 + scale2) + shift2
        x = x + gate2 * expert_choice_moe(h, w_router[layer], w_gate_e[layer], w_up_e[layer], w_down_e[layer])
    return rms_norm(x, normf_w)
```

```python
import math
import numpy as np
import concourse.bass as bass
import concourse.mybir as mybir
from concourse.bass_utils import run_bass_kernel_spmd

F32 = mybir.dt.float32
BF16 = mybir.dt.bfloat16
I32 = mybir.dt.int32
AF = mybir.ActivationFunctionType
ALU = mybir.AluOpType
AX = mybir.AxisListType

D = 4096
S = 2048
NT = S // 128
NCH = D // 128
IN_DIM = 23616
OFF_Z, OFF_XBC, OFF_DT, OFF_Q, OFF_K, OFF_V, OFF_G = 0, 2048, 6144, 6208, 9280, 12352, 15424
NEG = -30000.0


class Buf:
    __slots__ = ("name", "w", "r")

    def __init__(self, name):
        self.name = name
        self.w = {}
        self.r = {}


class Eng:
    def __init__(self, name, h, sem):
        self.name, self.h, self.sem = name, h, sem
        self.count = 0
        self.known = {}
        self.pend_r, self.pend_w = [], []


class K:
    def __init__(self, nc, stack):
        self.nc = nc
        self.stack = stack
        self.eng = {}
        for nm, h in (("pe", nc.tensor), ("dve", nc.vector), ("act", nc.scalar),
                      ("pool", nc.gpsimd), ("sp", nc.sync)):
            sem = stack.enter_context(nc.semaphore("s_" + nm))
            self.eng[nm] = Eng(nm, h, sem)
        self.dsem = {}
        for q, n in (("sp", 24), ("pool", 24), ("act", 8)):
            self.dsem[q] = [[stack.enter_context(nc.semaphore("d_%s%d" % (q, i))), 0] for i in range(n)]
        self.dptr = {"sp": 0, "pool": 0, "act": 0}
        self.nbuf = 0

    def buf(self, name=None):
        self.nbuf += 1
        return Buf(name or "b%d" % self.nbuf)

    def _waits(self, e, reads, writes):
        need = {}

        def add(ev):
            if ev is None:
                return
            s, v = ev
            k = id(s)
            if k not in need or need[k][1] < v:
                need[k] = (s, v)
        for b in reads:
            for ev in b.w.values():
                add(ev)
        for b in writes:
            for ev in b.w.values():
                add(ev)
            for ev in b.r.values():
                add(ev)
        for k, (s, v) in need.items():
            if e.name == "pe" and s is e.sem:
                continue
            if e.known.get(k, 0) < v:
                e.h.wait_ge(s, v)
                e.known[k] = v

    def _commit(self, ev, reads, writes):
        kk = id(ev[0])
        for b in reads:
            if kk not in b.r or b.r[kk][1] < ev[1]:
                b.r[kk] = ev
        for b in writes:
            if kk not in b.w or b.w[kk][1] < ev[1]:
                b.w[kk] = ev
            b.r = {}

    def op(self, en, fn, reads=(), writes=(), inc=True):
        e = self.eng[en]
        self._waits(e, reads, writes)
        ins = fn(e.h)
        if not inc:
            e.pend_r.extend(reads)
            e.pend_w.extend(writes)
            return ins
        e.count += 1
        ins.then_inc(e.sem, 1)
        ev = (e.sem, e.count)
        e.known[id(e.sem)] = max(e.known.get(id(e.sem), 0), 0)
        self._commit(ev, list(reads) + e.pend_r, list(writes) + e.pend_w)
        e.pend_r, e.pend_w = [], []
        return ins

    def dma(self, q, out, in_, reads=(), writes=(), **kw):
        e = self.eng[q]
        pool = self.dsem[q]
        i = self.dptr[q]
        self.dptr[q] = (i + 1) % len(pool)
        sem, cnt = pool[i]
        if cnt > 0 and e.known.get(id(sem), 0) < cnt:
            e.h.wait_ge(sem, cnt)
            e.known[id(sem)] = cnt
        self._waits(e, reads, writes)
        e.h.dma_start(out=out, in_=in_, **kw).then_inc(sem, 16)
        pool[i][1] = cnt + 16
        ev = (sem, cnt + 16)
        self._commit(ev, reads, writes)
        return ev

    def wait_all(self, en, bufs):
        e = self.eng[en]
        self._waits(e, bufs, bufs)

    def barrier(self):
        for e in self.eng.values():
            assert not e.pend_r and not e.pend_w, e.name
        for e in self.eng.values():
            for o in self.eng.values():
                if o.count > 0 and e.known.get(id(o.sem), 0) < o.count and o is not e:
                    e.h.wait_ge(o.sem, o.count)
                    e.known[id(o.sem)] = o.count
            for q in self.dsem:
                for sem, cnt in self.dsem[q]:
                    if cnt > 0 and e.known.get(id(sem), 0) < cnt:
                        e.h.wait_ge(sem, cnt)
                        e.known[id(sem)] = cnt


def cdiv(a, b):
    return (a + b - 1) // b


def build_program(phases=("all",), debug=(), feed=()):
    from contextlib import ExitStack
    nc = bass.Bass("TRN2", target_bir_lowering=False)
    ALLP = "all" in phases

    def inp(name, shape, dt=F32):
        return nc.dram_tensor(name, list(shape), dt, kind="ExternalInput").ap()

    def scr(name, shape, dt):
        kind = "ExternalOutput" if name in debug else ("ExternalInput" if name in feed else "Internal")
        return nc.dram_tensor(name, list(shape), dt, kind=kind).ap()

    IN_SHAPES = {
        "x": [S, D], "c": [NCH, 128], "norm1_w": [NCH, 128], "norm2_w": [NCH, 128], "normf_w": [NCH, 128],
        "w_ada": [D, 6 * D], "b_ada": [6 * NCH, 128], "w_in": [D, IN_DIM], "conv_w": [5, NCH, 128],
        "conv_b": [NCH, 128], "dt_bias_f": [32], "dt_bias_b": [32], "a_log_f": [32], "a_log_b": [32],
        "d_skip": [32], "ssm_norm_w": [16, 128], "w_ssm_out": [2048, D], "w_attn_out": [1024, D],
        "w_o": [D, D], "w_router": [D, 16], "w_gate_e": [16, D, 2048], "w_up_e": [16, D, 2048],
        "w_down_e": [16, 2048, D],
    }
    _ins = {}

    def I(name):
        if name not in _ins:
            _ins[name] = inp(name, IN_SHAPES[name])
        return _ins[name]

    out = nc.dram_tensor("out", [S, D], F32, kind="ExternalOutput").ap() if (ALLP or "out" in debug) else None

    modc_s = scr("modc_s", [128, 6 * NCH], F32)
    modrow_s = scr("modrow_s", [6 * NCH, 128], F32)
    hT_s = scr("hT_s", [D, S], BF16) if "hT_s" in debug else None
    zT_s = scr("zT_s", [2048, S], BF16)
    xs_s = scr("xs_s", [S, 2048], BF16)
    xsT_s = scr("xsT_s", [2048, S], BF16)
    BT_s = scr("BT_s", [1024, S], BF16)
    CT_s = scr("CT_s", [1024, S], BF16)
    dt_s = scr("dt_s", [S, 64], F32)
    csT_s = scr("csT_s", [64, S], F32)
    qT_s = scr("qT_s", [3072, S], BF16)
    kT_s = scr("kT_s", [3072, S], BF16)
    v_s = scr("v_s", [S, 3072], BF16)
    sgT_s = scr("sgT_s", [8192, S], BF16)
    ygT_s = scr("ygT_s", [2048, S], BF16)
    oT_s = scr("oT_s", [1024, S], BF16)
    mT_s = scr("mT_s", [D, S], BF16)
    x1_s = scr("x1_s", [S, D], F32)
    h2_s = scr("h2_s", [S, D], BF16)
    y_s = scr("y_s", [16 * 256, D], BF16)

    with ExitStack() as st:
        k = K(nc, st)

        def sb(name, shape, dt):
            return st.enter_context(nc.sbuf_tensor(name, list(shape), dt))

        def ps(name, shape, dt=F32):
            return st.enter_context(nc.psum_tensor(name, list(shape), dt))

        ident = sb("ident", [128, 128], F32)
        identb = sb("identb", [128, 128], BF16)
        b_ident = k.buf("ident")
        k.op("pool", lambda e: e.memset(ident[:], 0.0), writes=[b_ident])
        k.op("pool", lambda e: e.affine_select(out=ident[:], in_=ident[:], pattern=[[-1, 128]],
                                               compare_op=ALU.not_equal, fill=1.0, base=0,
                                               channel_multiplier=1), reads=[b_ident], writes=[b_ident])
        b_identb = k.buf("identb")
        k.op("pool", lambda e: e.tensor_copy(out=identb[:], in_=ident[:]), reads=[b_ident], writes=[b_identb])

        NGB = 6
        psum_banks = [ps("pb%d" % i, [128, 512], F32) for i in range(NGB)]
        pbuf = [k.buf("pb%d" % i) for i in range(NGB)]
        pctr = [0]

        def next_bank(lo=0, hi=NGB):
            i = lo + pctr[0] % (hi - lo)
            pctr[0] += 1
            return psum_banks[i], pbuf[i]
        ptb = [ps("ptb%d" % i, [128, 1024], BF16) for i in range(2)]
        ptbb = [k.buf("ptb%d" % i) for i in range(2)]
        ptctr = [0]

        def next_tbank():
            i = ptctr[0] % 2
            ptctr[0] += 1
            return ptb[i], ptbb[i]

        evac_ctr = [0]

        def evac_eng():
            evac_ctr[0] += 1
            return "act" if evac_ctr[0] % 2 else "dve"

        modc = sb("modc", [128, 6 * NCH], F32)
        b_modc = k.buf("modc")
        n1col = sb("n1col", [128, NCH], F32)
        n2col = sb("n2col", [128, NCH], F32)
        b_ncol = k.buf("ncol")

        def row2col(src_ap, nrows, dst_ap, name):
            with ExitStack() as sx:
                nrow = sx.enter_context(nc.sbuf_tensor("r2c_" + name, [nrows, 128], F32))
                b_nrow = k.buf()
                k.dma("sp", nrow[:], src_ap, writes=[b_nrow])
                pb, pbb = next_bank(0, 5)
                k.op("pe", lambda e: e.transpose(pb[:, 0:nrows], nrow[:], ident[0:nrows, 0:nrows]),
                     reads=[b_nrow, b_ident], writes=[pbb])
                bd = k.buf()
                k.op("dve", lambda e: e.tensor_copy(out=dst_ap, in_=pb[:, 0:nrows]), reads=[pbb], writes=[bd])
                k.barrier()
            return bd

        if ALLP or "p1" in phases or "pB" in phases:
            row2col(I("norm1_w")[:, :], NCH, n1col[:], "n1")
            row2col(I("norm2_w")[:, :], NCH, n2col[:], "n2")

        if ALLP or "p0" in phases:
            with ExitStack() as s0:
                def sb0(name, shape, dt):
                    return s0.enter_context(nc.sbuf_tensor(name, list(shape), dt))
                crow = sb0("crow", [NCH, 128], F32)
                b_crow = k.buf()
                k.dma("sp", crow[:], I("c")[:, :], writes=[b_crow])
                k.op("act", lambda e: e.activation(out=crow[:], in_=crow[:], func=AF.Silu),
                     reads=[b_crow], writes=[b_crow])
                pb, pbb = next_bank(0, 5)
                k.op("pe", lambda e: e.transpose(pb[:, 0:NCH], crow[:], ident[0:NCH, 0:NCH]),
                     reads=[b_crow, b_ident], writes=[pbb])
                ccol = sb0("ccol", [128, NCH], BF16)
                b_ccol = k.buf()
                k.op("dve", lambda e: e.tensor_copy(out=ccol[:], in_=pb[:, 0:NCH]), reads=[pbb], writes=[b_ccol])
                brow = sb0("brow", [64, 3, 128], F32)
                b_brow = k.buf()
                k.dma("sp", brow[:], I("b_ada").rearrange("(a r) p -> r a p", r=64), writes=[b_brow])
                bcol = sb0("bcol", [128, 6 * NCH], F32)
                b_bcol = k.buf()
                for a in range(3):
                    pb, pbb = next_bank(0, 5)
                    k.op("pe", lambda e, pb=pb, a=a: e.transpose(pb[:, 0:64], brow[:, a, :], ident[0:64, 0:64]),
                         reads=[b_brow, b_ident], writes=[pbb])
                    k.op("dve", lambda e, pb=pb, a=a: e.tensor_copy(out=bcol[:, a * 64:(a + 1) * 64], in_=pb[:, 0:64]),
                         reads=[pbb], writes=[b_bcol])
                WA_N = 256
                nwt = 6 * D // WA_N
                NWB = 5
                wbufs = [sb0("wada%d" % i, [128, NCH, WA_N], BF16) for i in range(NWB)]
                wb = [k.buf() for _ in range(NWB)]
                w_ada_v = I("w_ada").rearrange("(c p) n -> p c n", p=128)
                pmod, b_pmod = psum_banks[5], pbuf[5]
                for t in range(nwt):
                    wt, wtb = wbufs[t % NWB], wb[t % NWB]
                    k.dma("pool", wt[:], w_ada_v[:, :, t * WA_N:(t + 1) * WA_N], writes=[wtb])
                    for j in range(WA_N // 128):
                        col = t * (WA_N // 128) + j
                        for c in range(NCH):
                            k.op("pe", lambda e, wt=wt, j=j, c=c, col=col: e.matmul(
                                pmod[:, col:col + 1], lhsT=wt[:, c, j * 128:(j + 1) * 128], rhs=ccol[:, c:c + 1],
                                start=(c == 0), stop=(c == NCH - 1)),
                                reads=[wtb, b_ccol], writes=[b_pmod], inc=(c == NCH - 1))
                k.op("dve", lambda e: e.tensor_tensor(out=modc[:], in0=pmod[:, 0:6 * NCH], in1=bcol[:], op=ALU.add),
                     reads=[b_pmod, b_bcol], writes=[b_modc])
                if "modc_s" in debug:
                    k.dma("sp", modc_s[:, :], modc[:], reads=[b_modc], writes=[k.buf()])
                mrow = sb0("mrow", [96, 2, 128], F32)
                b_mrow = k.buf()
                for a in range(2):
                    pb, pbb = next_bank(0, 5)
                    k.op("pe", lambda e, pb=pb, a=a: e.transpose(pb[0:96, 0:128], modc[:, a * 96:(a + 1) * 96], ident[:]),
                         reads=[b_modc, b_ident], writes=[pbb])
                    k.op("dve", lambda e, pb=pb, a=a: e.tensor_copy(out=mrow[:, a, :], in_=pb[0:96, 0:128]),
                         reads=[pbb], writes=[b_mrow])
                k.dma("sp", modrow_s.rearrange("(a r) p -> r a p", a=2), mrow[:], reads=[b_mrow], writes=[k.buf()])
                k.barrier()

        if not (ALLP or "p0" in phases) and "modc_s" in feed:
            k.dma("sp", modc[:], modc_s[:, :], writes=[b_modc])

        def mm(out, lhsT, rhs, start, stop, reads, writes, inc=True):
            return k.op("pe", lambda e: e.matmul(out, lhsT=lhsT, rhs=rhs, start=start, stop=stop),
                        reads=reads, writes=writes, inc=inc)

        def actf(out, in_, func, reads, writes, **kw):
            return k.op("act", lambda e: e.activation(out=out, in_=in_, func=func, **kw), reads=reads, writes=writes)

        def cp(en, out, in_, reads, writes):
            if en == "act":
                return k.op("act", lambda e: e.copy(out=out, in_=in_), reads=reads, writes=writes)
            return k.op(en, lambda e: e.tensor_copy(out=out, in_=in_), reads=reads, writes=writes)

        def tt(en, out, in0, in1, op, reads, writes):
            return k.op(en, lambda e: e.tensor_tensor(out=out, in0=in0, in1=in1, op=op), reads=reads, writes=writes)

        def ts(en, out, in0, s1, s2, op0, op1, reads, writes):
            if s2 is None:
                return k.op(en, lambda e: e.tensor_scalar(out=out, in0=in0, scalar1=s1, scalar2=None, op0=op0),
                            reads=reads, writes=writes)
            return k.op(en, lambda e: e.tensor_scalar(out=out, in0=in0, scalar1=s1, scalar2=s2, op0=op0, op1=op1),
                        reads=reads, writes=writes)

        def stt(en, out, in0, scalar, in1, op0, op1, reads, writes):
            return k.op(en, lambda e: e.scalar_tensor_tensor(out=out, in0=in0, scalar=scalar, in1=in1, op0=op0, op1=op1),
                        reads=reads, writes=writes)

        def norm_to_hT(sx, src, ncol, m_shift, m_scale, hT, b_hT, tag):
            def sb1(name, shape, dt):
                return sx.enter_context(nc.sbuf_tensor(name + tag, list(shape), dt))
            s1col = sb1("s1col", [128, NCH], F32)
            b_s1 = k.buf()
            stt("dve", s1col[:], modc[:, m_scale * NCH:(m_scale + 1) * NCH], 1.0, ncol[:], ALU.add, ALU.mult,
                [b_modc, b_ncol], [b_s1])
            xts = [sb1("xt%d" % i, [128, D], F32) for i in range(2)]
            b_xt = [k.buf() for _ in range(2)]
            junk = sb1("junk", [128, D], BF16)
            b_junk = k.buf()
            ss = sb1("ss", [128, NT], F32)
            rstd = sb1("rstd", [128, NT], F32)
            b_ss = k.buf()
            k.op("pool", lambda e: e.memset(ss[:], 0.0), writes=[b_ss])
            for tt_ in range(NT):
                xt, bx = xts[tt_ % 2], b_xt[tt_ % 2]
                k.dma("sp", xt[:], src[tt_ * 128:(tt_ + 1) * 128, :], writes=[bx])
                actf(junk[:], xt[:], AF.Square, [bx], [b_junk, b_ss], accum_out=ss[:, tt_:tt_ + 1])
                ts("dve", rstd[:, tt_:tt_ + 1], ss[:, tt_:tt_ + 1], 1.0 / D, 1e-6, ALU.mult, ALU.add, [b_ss], [b_ss])
                k.op("act", lambda e, tt_=tt_: e.sqrt(out=rstd[:, tt_:tt_ + 1], in_=rstd[:, tt_:tt_ + 1]),
                     reads=[b_ss], writes=[b_ss])
                k.op("dve", lambda e, tt_=tt_: e.reciprocal(out=rstd[:, tt_:tt_ + 1], in_=rstd[:, tt_:tt_ + 1]),
                     reads=[b_ss], writes=[b_ss])
                actf(xt[:], xt[:], AF.Copy, [bx, b_ss], [bx], scale=rstd[:, tt_:tt_ + 1])
                for g4 in range(NCH // 4):
                    pb, pbb = next_bank()
                    for j in range(4):
                        c = g4 * 4 + j
                        k.op("pe", lambda e, pb=pb, j=j, c=c, xt=xt: e.transpose(
                            pb[:, j * 128:(j + 1) * 128], xt[:, c * 128:(c + 1) * 128], ident[:]),
                            reads=[bx, b_ident], writes=[pbb], inc=(j == 3))
                    for j in range(4):
                        c = g4 * 4 + j
                        o_ap = hT[:, c, tt_ * 128:(tt_ + 1) * 128]
                        i_ap = pb[:, j * 128:(j + 1) * 128]
                        if evac_eng() == "act":
                            actf(o_ap, i_ap, AF.Identity, [pbb, b_s1, b_modc], [b_hT[tt_]],
                                 scale=s1col[:, c:c + 1], bias=modc[:, m_shift * NCH + c:m_shift * NCH + c + 1])
                        else:
                            ts("dve", o_ap, i_ap, s1col[:, c:c + 1], modc[:, m_shift * NCH + c:m_shift * NCH + c + 1],
                               ALU.mult, ALU.add, [pbb, b_s1, b_modc], [b_hT[tt_]])

        DIL = (1, 4, 16)

        if ALLP or "pA" in phases:
            with ExitStack() as sA:
                def sbA(name, shape, dt):
                    return sA.enter_context(nc.sbuf_tensor(name, list(shape), dt))
                hT = sbA("hT", [128, NCH, S], BF16)
                b_hT = [k.buf("hT%d" % i) for i in range(NT)]
                with ExitStack() as s1:
                    norm_to_hT(s1, I("x"), n1col, 0, 1, hT, b_hT, "a")
                    k.barrier()
                if "hT_s" in debug:
                    k.dma("sp", hT_s.rearrange("(c p) t -> p c t", p=128), hT[:], reads=b_hT, writes=[k.buf()])

                WN = 128
                wts = [sbA("win%d" % i, [128, NCH, WN], BF16) for i in range(2)]
                b_wt = [k.buf() for _ in range(2)]
                wctr = [0]
                w_in_v = I("w_in").rearrange("(c p) n -> p c n", p=128)

                def load_w(col0, n):
                    i = wctr[0] % 2
                    wctr[0] += 1
                    k.dma("pool", wts[i][:, :, 0:n], w_in_v[:, :, col0:col0 + n], writes=[b_wt[i]])
                    return wts[i], b_wt[i]

                def gemm_fm(wt, bw, n0=0):
                    res = []
                    for tb in range(4):
                        pb, pbb = next_bank()
                        for c in range(NCH):
                            mm(pb[:, :], wt[:, c, n0:n0 + 128], hT[:, c, tb * 512:(tb + 1) * 512], c == 0, c == NCH - 1,
                               [bw] + b_hT[4 * tb:4 * tb + 4], [pbb], inc=(c == NCH - 1))
                        res.append((pb, pbb))
                    return res

                def gemm_tm(wt, bw, n, tt_):
                    pb, pbb = next_bank()
                    for c in range(NCH):
                        mm(pb[:, 0:n], hT[:, c, tt_ * 128:(tt_ + 1) * 128], wt[:, c, 0:n], c == 0, c == NCH - 1,
                           [bw, b_hT[tt_]], [pbb], inc=(c == NCH - 1))
                    return pb, pbb

                bufA = sbA("bufA", [128, S + 4], F32)
                bufB = sbA("bufB", [128, S], F32)
                bufCs = [sbA("bufC%d" % i, [128, S], BF16) for i in range(2)]
                bufDs = [sbA("bufD%d" % i, [128, NT, 128], BF16) for i in range(2)]
                bCs = [k.buf(), k.buf()]
                bDs = [k.buf(), k.buf()]
                cctr = [0]

                def rotC():
                    i = cctr[0] % 2
                    cctr[0] += 1
                    return bufCs[i], bCs[i], bufDs[i], bDs[i]
                bufC, bufD = bufCs[0], bufDs[0]
                dtst = sbA("dtst", [128, NT, 64], F32)
                bA, bB, bC, bD, bTM, bDT = (k.buf() for _ in range(6))
                k.op("pool", lambda e: e.memset(bufA[:], 0.0), writes=[bA])

                def fm_job(col0, nblk, dst, func):
                    nonlocal bufC, bC, bufD, bD
                    for blk in range(nblk):
                        bufC, bC, bufD, bD = rotC()
                        wt, bw = load_w(col0 + blk * 128, 128)
                        res = gemm_fm(wt, bw)
                        for tb, (pb, pbb) in enumerate(res):
                            actf(bufC[:, tb * 512:(tb + 1) * 512], pb[:, :], func, [pbb], [bC])
                        k.dma("sp", dst[blk * 128:(blk + 1) * 128, :], bufC[:], reads=[bC], writes=[k.buf()])

                if ALLP or "A_z" in phases:
                    fm_job(OFF_Z, 16, zT_s, AF.Silu)
                if ALLP or "A_dt" in phases:
                    wt, bw = load_w(OFF_DT, 64)
                    for tt_ in range(NT):
                        pb, pbb = gemm_tm(wt, bw, 64, tt_)
                        cp("dve", dtst[:, tt_, :], pb[:, 0:64], [pbb], [bDT])
                    k.dma("sp", dt_s.rearrange("(tt p) n -> p tt n", p=128), dtst[:], reads=[bDT], writes=[k.buf()])
                def fm_to_tm(dstv):
                    for half in range(2):
                        pt, ptbf = next_tbank()
                        for j in range(8):
                            tt_ = half * 8 + j
                            k.op("pe", lambda e, pt=pt, j=j, tt_=tt_: e.transpose(
                                pt[:, j * 128:(j + 1) * 128], bufC[:, tt_ * 128:(tt_ + 1) * 128], identb[:]),
                                reads=[bC, b_identb], writes=[ptbf], inc=(j == 7))
                        cp(evac_eng(), bufD[:, half * 8:(half + 1) * 8, :],
                           pt[:, :].rearrange("p (a b) -> p a b", b=128), [ptbf], [bD])
                    k.dma("sp", dstv, bufD[:], reads=[bD], writes=[k.buf()])

                if ALLP or "A_v" in phases:
                    for blk in range(24):
                        bufC, bC, bufD, bD = rotC()
                        wt, bw = load_w(OFF_V + blk * 128, 128)
                        res = gemm_fm(wt, bw)
                        for tb, (pb, pbb) in enumerate(res):
                            cp(evac_eng(), bufC[:, tb * 512:(tb + 1) * 512], pb[:, :], [pbb], [bC])
                        fm_to_tm(v_s.rearrange("(tt p) n -> p tt n", p=128)[:, :, blk * 128:(blk + 1) * 128])
                if ALLP or "A_g" in phases:
                    fm_job(OFF_G, 64, sgT_s, AF.Sigmoid)
                if ALLP or "A_x" in phases:
                    cwc = sbA("cwc", [128, 6, NCH], F32)
                    for kk in range(5):
                        row2col(I("conv_w")[kk, :, :], NCH, cwc[:, kk, :], "cw%d" % kk)
                    bcw = row2col(I("conv_b")[:, :], NCH, cwc[:, 5, :], "cb")
                    for blk in range(32):
                        bufC, bC, bufD, bD = rotC()
                        wt, bw = load_w(OFF_XBC + blk * 128, 128)
                        res = gemm_fm(wt, bw)
                        for tb, (pb, pbb) in enumerate(res):
                            cp(evac_eng(), bufA[:, 2 + tb * 512:2 + (tb + 1) * 512], pb[:, :], [pbb], [bA])
                        ts("dve", bufB[:], bufA[:, 0:S], cwc[:, 0, blk:blk + 1], cwc[:, 5, blk:blk + 1], ALU.mult, ALU.add,
                           [bA, bcw], [bB])
                        for kk in range(1, 5):
                            stt("dve", bufB[:], bufA[:, kk:kk + S], cwc[:, kk, blk:blk + 1], bufB[:], ALU.mult, ALU.add,
                                [bA, bB, bcw], [bB])
                        actf(bufC[:], bufB[:], AF.Silu, [bB], [bC])
                        if blk >= 24:
                            k.dma("sp", CT_s[(blk - 24) * 128:(blk - 23) * 128, :], bufC[:], reads=[bC], writes=[k.buf()])
                            continue
                        if blk >= 16:
                            k.dma("sp", BT_s[(blk - 16) * 128:(blk - 15) * 128, :], bufC[:], reads=[bC], writes=[k.buf()])
                            continue
                        k.dma("sp", xsT_s[blk * 128:(blk + 1) * 128, :], bufC[:], reads=[bC], writes=[k.buf()])
                        for half in range(2):
                            pt, ptbf = next_tbank()
                            for j in range(8):
                                tt_ = half * 8 + j
                                k.op("pe", lambda e, pt=pt, j=j, tt_=tt_: e.transpose(
                                    pt[:, j * 128:(j + 1) * 128], bufC[:, tt_ * 128:(tt_ + 1) * 128], identb[:]),
                                    reads=[bC, b_identb], writes=[ptbf], inc=(j == 7))
                            cp(evac_eng(), bufD[:, half * 8:(half + 1) * 8, :],
                               pt[:, :].rearrange("p (a b) -> p a b", b=128), [ptbf], [bD])
                        dstv = xs_s.rearrange("(tt p) n -> p tt n", p=128)[:, :, blk * 128:(blk + 1) * 128]
                        k.dma("sp", dstv, bufD[:], reads=[bD], writes=[k.buf()])
                if ALLP or "A_qk" in phases:
                    cost = sbA("cost", [128, S], F32)
                    sint = sbA("sint", [128, S], F32)
                    perm = sbA("perm", [128, 128], F32)
                    colf = sbA("colf", [128, 4], F32)
                    b_tab, b_perm, b_colf = k.buf(), k.buf(), k.buf()
                    k.op("pool", lambda e: e.memset(perm[:], 0.0), writes=[b_perm])
                    for bs in (-64, 64):
                        k.op("pool", lambda e, bs=bs: e.affine_select(out=perm[:], in_=perm[:], pattern=[[-1, 128]],
                                                                      compare_op=ALU.not_equal, fill=1.0, base=bs,
                                                                      channel_multiplier=1), reads=[b_perm], writes=[b_perm])
                    k.op("pool", lambda e: e.iota(colf[:, 0:1], pattern=[[0, 1]], base=0, channel_multiplier=1,
                                                  allow_small_or_imprecise_dtypes=True), writes=[b_colf])
                    ts("dve", colf[:, 3:4], colf[:, 0:1], 64.0, None, ALU.is_ge, None, [b_colf], [b_colf])
                    stt("dve", colf[:, 1:2], colf[:, 3:4], -64.0, colf[:, 0:1], ALU.mult, ALU.add, [b_colf], [b_colf])
                    actf(colf[:, 2:3], colf[:, 1:2], AF.Exp, [b_colf], [b_colf], scale=-math.log(10000.0) / 64.0)
                    ts("dve", colf[:, 3:4], colf[:, 3:4], 2.0, -1.0, ALU.mult, ALU.add, [b_colf], [b_colf])
                    k.op("pool", lambda e: e.iota(bufB[:], pattern=[[1, S]], base=0, channel_multiplier=0,
                                                  allow_small_or_imprecise_dtypes=True), writes=[bB])
                    ts("dve", bufB[:], bufB[:], colf[:, 2:3], None, ALU.mult, None, [bB, b_colf], [bB])
                    TWO_PI = 2.0 * math.pi
                    MAGIC = 12582912.0

                    def sin_table(dst, shift):
                        ts("dve", dst[:], bufB[:], shift, 1.0 / TWO_PI, ALU.add, ALU.mult, [bB], [b_tab])
                        ts("dve", dst[:], dst[:], MAGIC, None, ALU.add, None, [b_tab], [b_tab])
                        ts("dve", dst[:], dst[:], -MAGIC, -TWO_PI, ALU.add, ALU.mult, [b_tab], [b_tab])
                        stt("dve", dst[:], bufB[:], shift, dst[:], ALU.add, ALU.add, [bB, b_tab], [b_tab])
                        ts("dve", dst[:], dst[:], -3.1415925, 3.1415925, ALU.max, ALU.min, [b_tab], [b_tab])
                        actf(dst[:], dst[:], AF.Sin, [b_tab], [b_tab])
                    sin_table(sint, 0.0)
                    ts("dve", sint[:], sint[:], colf[:, 3:4], None, ALU.mult, None, [b_tab, b_colf], [b_tab])
                    sin_table(cost, math.pi / 2.0)
                    tmpf = bufA
                    k.barrier()
                    bBq = [k.buf() for _ in range(4)]
                    bAq = [k.buf() for _ in range(4)]
                    for which, (off, dstT) in enumerate(((OFF_Q, qT_s), (OFF_K, kT_s))):
                        nheads = 24 if (ALLP or "A_qk_full" in phases) else 2
                        for hd in range(nheads):
                            dd = DIL[hd // 8]
                            bufC, bC, bufD, bD = rotC()
                            wt, bw = load_w(off + hd * 128, 128)
                            res = gemm_fm(wt, bw)
                            for tb, (pb, pbb) in enumerate(res):
                                sl = slice(tb * 512, (tb + 1) * 512)
                                cp(evac_eng(), bufB[:, sl], pb[:, :], [pbb], [bBq[tb]])
                            for tb in range(4):
                                sl = slice(tb * 512, (tb + 1) * 512)
                                pb, pbb = next_bank()
                                mm(pb[:, :], perm[:], bufB[:, sl], True, True, [bBq[tb], b_perm], [pbb])
                                tt("dve", tmpf[:, sl], pb[:, :], sint[:, sl], ALU.mult, [pbb, b_tab], [bAq[tb]])
                                tt("pool", bufB[:, sl], bufB[:, sl], cost[:, sl], ALU.mult, [bBq[tb], b_tab, pbb], [bBq[tb]])
                                tt("dve", bufB[:, sl], bufB[:, sl], tmpf[:, sl], ALU.add, [bBq[tb], bAq[tb]], [bBq[tb]])
                                if dd == 1:
                                    cp("act", bufC[:, sl], bufB[:, sl], [bBq[tb]], [bC])
                                else:
                                    n_i = 512 // dd
                                    o_ap = bufC[:, :].rearrange("e (r i) -> e r i", r=dd)[:, :, tb * n_i:(tb + 1) * n_i]
                                    i_ap = bufB[:, sl].rearrange("e (i r) -> e r i", r=dd)
                                    cp("act", o_ap, i_ap, [bBq[tb]], [bC])
                            k.dma("sp", dstT[hd * 128:(hd + 1) * 128, :], bufC[:], reads=[bC], writes=[k.buf()])
                k.barrier()
        if ALLP or "pB" in phases:
            with ExitStack() as sB:
                def sbB(name, shape, dt):
                    return sB.enter_context(nc.sbuf_tensor(name, list(shape), dt))
                triI = sbB("triI", [128, 128], F32)
                triE = sbB("triE", [128, 128], F32)
                onesf = sbB("onesf", [128, 128], F32)
                b_tri = k.buf()
                k.op("pool", lambda e: e.memset(onesf[:], 1.0), writes=[b_tri])
                k.op("pool", lambda e: e.memset(triI[:], 1.0), writes=[b_tri])
                k.op("pool", lambda e: e.affine_select(out=triI[:], in_=triI[:], pattern=[[1, 128]], compare_op=ALU.is_ge,
                                                       fill=0.0, base=0, channel_multiplier=-1), reads=[b_tri], writes=[b_tri])
                k.op("pool", lambda e: e.memset(triE[:], 1.0), writes=[b_tri])
                k.op("pool", lambda e: e.affine_select(out=triE[:], in_=triE[:], pattern=[[1, 128]], compare_op=ALU.is_ge,
                                                       fill=0.0, base=-1, channel_multiplier=-1), reads=[b_tri], writes=[b_tri])
                dtx = sbB("dtx", [128, NT, 64], F32)
                dtv = sbB("dtv", [128, NT, 64], F32)
                da = sbB("da", [128, NT, 64], F32)
                nb_ = sbB("nbias", [128, NT, 64], F32)
                brep = sbB("brep", [128, 64], F32)
                arep = sbB("arep", [128, 64], F32)
                dsk = sbB("dsk", [128, 32], F32)
                b_dtx, b_dtv, b_da, b_nb, b_brep, b_arep, b_dsk = (k.buf() for _ in range(7))
                k.dma("sp", dtx[:], dt_s.rearrange("(j p) n -> p j n", p=128), writes=[b_dtx])
                k.dma("sp", brep[:, 0:32], I("dt_bias_f").partition_broadcast(128), writes=[b_brep])
                k.dma("sp", brep[:, 32:64], I("dt_bias_b").partition_broadcast(128), writes=[b_brep])
                k.dma("sp", arep[:, 0:32], I("a_log_f").partition_broadcast(128), writes=[b_arep])
                k.dma("sp", arep[:, 32:64], I("a_log_b").partition_broadcast(128), writes=[b_arep])
                k.dma("sp", dsk[:], I("d_skip").partition_broadcast(128), writes=[b_dsk])
                actf(arep[:], arep[:], AF.Exp, [b_arep], [b_arep])
                ts("dve", arep[:], arep[:], -1.0, None, ALU.mult, None, [b_arep], [b_arep])
                for j in range(NT):
                    tt("dve", dtx[:, j, :], dtx[:, j, :], brep[:], ALU.add, [b_dtx, b_brep], [b_dtx])
                dtxf = dtx[:, :, :].rearrange("p a b -> p (a b)")
                dtvf = dtv[:, :, :].rearrange("p a b -> p (a b)")
                stt("dve", dtvf, dtxf, -1.0, dtxf, ALU.mult, ALU.max, [b_dtx], [b_dtv])
                actf(dtvf, dtvf, AF.Exp, [b_dtv], [b_dtv], scale=-1.0)
                ts("dve", dtvf, dtvf, 1.0, None, ALU.add, None, [b_dtv], [b_dtv])
                actf(dtvf, dtvf, AF.Ln, [b_dtv], [b_dtv])
                stt("dve", dtvf, dtxf, 0.0, dtvf, ALU.max, ALU.add, [b_dtx, b_dtv], [b_dtv])
                for j in range(NT):
                    tt("dve", da[:, j, :], dtv[:, j, :], arep[:], ALU.mult, [b_dtv, b_arep], [b_da])
                for half, tri in ((0, triI), (1, triE)):
                    for i in range(NT):
                        pbk, pbkb = psum_banks[i // 8], pbuf[i // 8]
                        o0 = (i % 8) * 64 + half * 32
                        for j in range(i + 1):
                            mm(pbk[:, o0:o0 + 32], (tri if j == i else onesf)[:], da[:, j, half * 32:half * 32 + 32],
                               j == 0, j == i, [b_da, b_tri], [pbkb], inc=(half == 1 and i % 8 == 7 and j == i))
                for b2 in range(2):
                    pv = psum_banks[b2][:, :].rearrange("p (a b) -> p a b", b=64)
                    ts("dve", nb_[:, b2 * 8:(b2 + 1) * 8, 0:32], pv[:, :, 0:32], -1.0, None, ALU.mult, None, [pbuf[b2]], [b_nb])
                    cp("act", nb_[:, b2 * 8:(b2 + 1) * 8, 32:64], pv[:, :, 32:64], [pbuf[b2]], [b_nb])
                lndt = sbB("lndt", [128, NT, 64], F32)
                b_lndt = k.buf()
                actf(lndt[:, :, :].rearrange("p a b -> p (a b)"), dtvf, AF.Ln, [b_dtv], [b_lndt])
                nbf = nb_[:, :, :].rearrange("p a b -> p (a b)")
                tt("dve", nbf, nbf, lndt[:, :, :].rearrange("p a b -> p (a b)"), ALU.add, [b_nb, b_lndt], [b_nb])
                csr = sbB("csr", [32, S], F32)
                b_csr = k.buf()
                for half, tri in ((0, triI), (1, triE)):
                    for i in range(NT):
                        bi = 2 + i // 4
                        pbk, pbkb = psum_banks[bi], pbuf[bi]
                        o0 = (i % 4) * 128
                        for j in range(i + 1):
                            mm(pbk[0:32, o0:o0 + 128], da[:, j, half * 32:half * 32 + 32], (tri if j == i else onesf)[:],
                               j == 0, j == i, [b_da, b_tri], [pbkb], inc=(i % 4 == 3 and j == i))
                    for q4 in range(4):
                        cp(evac_eng(), csr[:, q4 * 512:(q4 + 1) * 512], psum_banks[2 + q4][0:32, :], [pbuf[2 + q4]], [b_csr])
                    k.dma("sp", csT_s[half * 32:(half + 1) * 32, :], csr[:], reads=[b_csr], writes=[k.buf()])
                k.barrier()
                if "nb_s" in debug:
                    nb_s = scr("nb_s", [128, NT * 64], F32)
                    dtv_s = scr("dtv_s", [128, NT * 64], F32)
                    k.dma("sp", nb_s[:, :], nb_[:, :, :].rearrange("p a b -> p (a b)"), reads=[b_nb], writes=[k.buf()])
                    k.dma("sp", dtv_s[:, :], dtvf, reads=[b_dtv], writes=[k.buf()])

                GT = sbB("GT", [128, NT, S], BF16)
                BTt = sbB("BTt", [128, S], BF16)
                CTt = sbB("CTt", [128, S], BF16)
                csrep = [sbB("csrep%d" % i, [128, S], F32) for i in range(2)]
                Xh = sbB("Xh", [128, NT, 64], BF16)
                xsTh = sbB("xsTh", [64, S], BF16)
                zTh = sbB("zTh", [64, S], BF16)
                Eb = [sbB("Eb%d" % i, [128, 512], BF16) for i in range(3)]
                Mb = [sbB("Mb%d" % i, [128, 512], BF16) for i in range(3)]
                ytmp = sbB("ytmp", [64, 512], F32)
                yout = sbB("yout", [64, S], BF16)
                b_GT = [k.buf() for _ in range(NT)]
                b_BT, b_CT, b_X, b_xsT, b_zT, b_ytmp, b_yout = (k.buf() for _ in range(7))
                b_csrep = [k.buf(), k.buf()]
                b_E = [k.buf() for _ in range(3)]
                b_M = [k.buf() for _ in range(3)]
                sctr = 0
                ngroups = 8 if (ALLP or "B_full" in phases) else 1
                for g in range(ngroups):
                    k.dma("sp", BTt[:], BT_s[g * 128:(g + 1) * 128, :], writes=[b_BT])
                    k.dma("sp", CTt[:], CT_s[g * 128:(g + 1) * 128, :], writes=[b_CT])
                    for j in range(NT):
                        for tb in range(4):
                            pb, pbb = next_bank(0, 4)
                            mm(pb[:, :], BTt[:, j * 128:(j + 1) * 128], CTt[:, tb * 512:(tb + 1) * 512], True, True,
                               [b_BT, b_CT], [pbb])
                            cp(evac_eng(), GT[:, j, tb * 512:(tb + 1) * 512], pb[:, :], [pbb], [b_GT[j]])
                    for kh in range(4):
                        h = g * 4 + kh
                        k.dma("sp", csrep[0][:], csT_s[h:h + 1, :].partition_broadcast(128), writes=[b_csrep[0]])
                        k.dma("sp", csrep[1][:], csT_s[32 + h:33 + h, :].partition_broadcast(128), writes=[b_csrep[1]])
                        k.dma("sp", Xh[:], xs_s.rearrange("(j p) n -> p j n", p=128)[:, :, h * 64:(h + 1) * 64], writes=[b_X])
                        k.dma("sp", xsTh[:], xsT_s[h * 64:(h + 1) * 64, :], writes=[b_xsT])
                        k.dma("sp", zTh[:], zT_s[h * 64:(h + 1) * 64, :], writes=[b_zT])
                        for tb in range(4):
                            py, pyb = psum_banks[4 + tb % 2], pbuf[4 + tb % 2]
                            segs = []
                            for j in range(NT):
                                if j < 4 * tb:
                                    segs.append((j, 0, tb * 512, (tb + 1) * 512, False))
                                elif j > 4 * tb + 3:
                                    segs.append((j, 1, tb * 512, (tb + 1) * 512, False))
                                else:
                                    segs.append((j, 0, j * 128, (tb + 1) * 512, True))
                                    segs.append((j, 1, tb * 512, (j + 1) * 128, True))
                            for si, (j, dr, t0, t1, diag) in enumerate(segs):
                                w = t1 - t0
                                E_, bE = Eb[sctr % 3], b_E[sctr % 3]
                                M_, bM = Mb[sctr % 3], b_M[sctr % 3]
                                sctr += 1
                                actf(E_[:, 0:w], csrep[dr][:, t0:t1], AF.Exp, [b_csrep[dr], b_nb], [bE],
                                     bias=nb_[:, j, dr * 32 + h:dr * 32 + h + 1], scale=(1.0 if dr == 0 else -1.0))
                                if diag:
                                    if dr == 0:
                                        k.op("pool", lambda e, E_=E_: e.affine_select(
                                            out=E_[:, 0:128], in_=E_[:, 0:128], pattern=[[1, 128]], compare_op=ALU.is_ge,
                                            fill=0.0, base=0, channel_multiplier=-1), reads=[bE], writes=[bE])
                                    else:
                                        k.op("pool", lambda e, E_=E_, w=w: e.affine_select(
                                            out=E_[:, w - 128:w], in_=E_[:, w - 128:w], pattern=[[-1, 128]], compare_op=ALU.is_ge,
                                            fill=0.0, base=0, channel_multiplier=1), reads=[bE], writes=[bE])
                                tt("pool" if (sctr % 3 == 0 and not diag) else "dve", M_[:, 0:w], E_[:, 0:w], GT[:, j, t0:t1],
                                   ALU.mult, [bE, b_GT[j]], [bM])
                                mm(py[0:64, t0 - tb * 512:t1 - tb * 512], Xh[:, j, :], M_[:, 0:w], si == 0, si == len(segs) - 1,
                                   [b_X, bM], [pyb], inc=True)
                            sl = slice(tb * 512, (tb + 1) * 512)
                            stt("dve", ytmp[:, :], xsTh[:, sl], dsk[0:64, h:h + 1], py[0:64, :], ALU.mult, ALU.add,
                                [b_xsT, b_dsk, pyb], [b_ytmp])
                            tt("pool", yout[:, sl], ytmp[:, :], zTh[:, sl], ALU.mult, [b_ytmp, b_zT], [b_yout])
                        k.dma("sp", ygT_s[h * 64:(h + 1) * 64, :], yout[:], reads=[b_yout], writes=[k.buf()])
                k.barrier()
        if ALLP or "pC" in phases:
            with ExitStack() as sC:
                def sbC(name, shape, dt):
                    return sC.enter_context(nc.sbuf_tensor(name, list(shape), dt))
                maskB = sbC("maskB", [128, 384], BF16)
                onesb = sbC("onesb128", [128, 128], BF16)
                b_mask = k.buf()
                k.op("pool", lambda e: e.memset(onesb[:], 1.0), writes=[b_mask])
                k.op("pool", lambda e: e.memset(maskB[:], 1.0), writes=[b_mask])
                k.op("pool", lambda e: e.affine_select(out=maskB[:], in_=maskB[:], pattern=[[1, 384]], compare_op=ALU.is_ge,
                                                       fill=0.0, base=-64, channel_multiplier=-1), reads=[b_mask], writes=[b_mask])
                k.op("pool", lambda e: e.affine_select(out=maskB[:], in_=maskB[:], pattern=[[-1, 384]], compare_op=ALU.is_ge,
                                                       fill=0.0, base=192, channel_multiplier=1), reads=[b_mask], writes=[b_mask])
                Oacc = sbC("Oacc", [128, S], F32)
                Zacc = sbC("Zacc", [128, S], F32)
                qTh = sbC("qTh", [128, S], BF16)
                kTh = sbC("kTh", [128, S], BF16)
                vt = sbC("vt", [128, NT, 128], BF16)
                PBt = [sbC("PBt%d" % i, [128, 8, 384], BF16) for i in range(2)]
                oTo = sbC("oTo", [128, S], BF16)
                b_O, b_Z, b_q, b_k, b_v, b_oT = (k.buf() for _ in range(6))
                b_PB = [[k.buf() for _ in range(8)] for _ in range(2)]
                pctr2 = 0
                sm_scale = 1.0 / math.sqrt(128.0)
                nslots = 8 if (ALLP or "C_full" in phases) else 1
                for hs in range(nslots):
                    for g in range(3):
                        hd = g * 8 + hs
                        d = DIL[g]
                        L = S // d
                        nt = L // 128
                        k.dma("sp", qTh[:], qT_s[hd * 128:(hd + 1) * 128, :], writes=[b_q])
                        k.dma("sp", kTh[:], kT_s[hd * 128:(hd + 1) * 128, :], writes=[b_k])
                        vview = v_s.rearrange("(j kk r) n -> r kk j n", kk=128, r=d)
                        for r in range(d):
                            k.dma("act" if r % 2 else "sp", vt[:, r * nt:(r + 1) * nt, :], vview[r][:, :, hd * 128:(hd + 1) * 128],
                                  writes=[b_v])
                        for c0 in range(0, S, 512):
                            pO, pOb = psum_banks[4], pbuf[4]
                            pZ, pZb = psum_banks[5], pbuf[5]
                            tiles = []
                            for qi in range(4):
                                col = c0 + qi * 128
                                tiles.append((qi, col // L, (col % L) // 128))
                            PB_, bPB = PBt[pctr2 % 2], b_PB[pctr2 % 2]
                            pctr2 += 1
                            slot = 0
                            for r in sorted(set(t_[1] for t_ in tiles)):
                                tl = [t_ for t_ in tiles if t_[1] == r]
                                ilo, ihi = tl[0][2], tl[-1][2]
                                qi0 = tl[0][0]
                                jlo, jhi = max(ilo - 1, 0), min(ihi + 1, nt - 1)
                                info = {}
                                for j in range(jlo, jhi + 1):
                                    a_ = max(j - 1, ilo)
                                    b_ = min(j + 1, ihi)
                                    w = (b_ - a_ + 1) * 128
                                    ps_, psb = next_bank(0, 4)
                                    qc0 = r * L + a_ * 128
                                    mm(ps_[:, 0:w], kTh[:, r * L + j * 128:r * L + (j + 1) * 128], qTh[:, qc0:qc0 + w], True, True,
                                       [b_q, b_k], [psb])
                                    actf(PB_[:, slot, 0:w], ps_[:, 0:w], AF.Exp, [psb], [bPB[slot]], scale=sm_scale)
                                    off = 128 + (a_ - j) * 128
                                    tt("dve", PB_[:, slot, 0:w], PB_[:, slot, 0:w], maskB[:, off:off + w], ALU.mult, [bPB[slot], b_mask], [bPB[slot]])
                                    info[j] = (slot, a_)
                                    slot += 1
                                for i in range(ilo, ihi + 1):
                                    qi = qi0 + (i - ilo)
                                    cj = list(range(max(i - 1, 0), min(i + 1, nt - 1) + 1))
                                    for n, j in enumerate(cj):
                                        sl_, a_ = info[j]
                                        pc = (i - a_) * 128
                                        mm(pO[:, qi * 128:(qi + 1) * 128], vt[:, r * nt + j, :], PB_[:, sl_, pc:pc + 128], n == 0, n == len(cj) - 1,
                                           [b_v, bPB[sl_]], [pOb], inc=False)
                                    for n, j in enumerate(cj):
                                        sl_, a_ = info[j]
                                        pc = (i - a_) * 128
                                        mm(pZ[:, qi * 128:(qi + 1) * 128], onesb[:], PB_[:, sl_, pc:pc + 128], n == 0, n == len(cj) - 1,
                                           [b_mask, bPB[sl_]], [pZb], inc=(n == len(cj) - 1))
                            if d == 1:
                                ovO, ovZ = Oacc[:, c0:c0 + 512], Zacc[:, c0:c0 + 512]
                                pvO, pvZ = pO[:, :], pZ[:, :]
                            elif d == 4:
                                r = c0 // 512
                                ovO = Oacc[:, :].rearrange("e (i r) -> e r i", r=4)[:, r, :]
                                ovZ = Zacc[:, :].rearrange("e (i r) -> e r i", r=4)[:, r, :]
                                pvO, pvZ = pO[:, :], pZ[:, :]
                            else:
                                r0 = c0 // 128
                                ovO = Oacc[:, :].rearrange("e (i r) -> e r i", r=16)[:, r0:r0 + 4, :]
                                ovZ = Zacc[:, :].rearrange("e (i r) -> e r i", r=16)[:, r0:r0 + 4, :]
                                pvO = pO[:, :].rearrange("e (r i) -> e r i", r=4)
                                pvZ = pZ[:, :].rearrange("e (r i) -> e r i", r=4)
                            if g == 0:
                                cp("act", ovO, pvO, [pOb], [b_O])
                                cp("dve", ovZ, pvZ, [pZb], [b_Z])
                            else:
                                tt("dve", ovO, ovO, pvO, ALU.add, [pOb, b_O], [b_O])
                                tt("dve", ovZ, ovZ, pvZ, ALU.add, [pZb, b_Z], [b_Z])
                    k.op("dve", lambda e: e.reciprocal(out=Zacc[:], in_=Zacc[:]), reads=[b_Z], writes=[b_Z])
                    tt("dve", oTo[:], Oacc[:], Zacc[:], ALU.mult, [b_O, b_Z], [b_oT])
                    k.dma("sp", oT_s[hs * 128:(hs + 1) * 128, :], oTo[:], reads=[b_oT], writes=[k.buf()])
                k.barrier()
        if ALLP or "pD" in phases:
            with ExitStack() as sD:
                def sbD(name, shape, dt):
                    return sD.enter_context(nc.sbuf_tensor(name, list(shape), dt))
                yg = sbD("yg", [128, 16, S], BF16)
                oTt = sbD("oTt", [128, 8, S], BF16)
                b_yg, b_oTt = k.buf(), k.buf()
                k.dma("sp", yg[:], ygT_s.rearrange("(c p) t -> p c t", p=128), writes=[b_yg])
                k.dma("act", oTt[:], oT_s.rearrange("(c p) t -> p c t", p=128), writes=[b_oTt])
                wncol = sbD("wncol", [128, 16], F32)
                b_wn = row2col(I("ssm_norm_w")[:, :], 16, wncol[:], "wn")
                onesb = sbD("onesbD", [128, 128], BF16)
                b_ones = k.buf()
                k.op("pool", lambda e: e.memset(onesb[:], 1.0), writes=[b_ones])
                sq = [sbD("sq%d" % i, [128, S], BF16) for i in range(2)]
                b_sq = [k.buf(), k.buf()]
                rrep = sbD("rrep", [128, S], F32)
                b_rrep = k.buf()
                for c in range(16):
                    tt("pool" if c % 2 else "dve", sq[c % 2][:], yg[:, c, :], yg[:, c, :], ALU.mult, [b_yg], [b_sq[c % 2]])
                    for tb in range(4):
                        mm(psum_banks[tb][:, :], onesb[:], sq[c % 2][:, tb * 512:(tb + 1) * 512], c == 0, c == 15,
                           [b_ones, b_sq[c % 2]], [pbuf[tb]], inc=True)
                for tb in range(4):
                    ts("dve", rrep[:, tb * 512:(tb + 1) * 512], psum_banks[tb][:, :], 1.0 / 2048.0, 1e-6, ALU.mult, ALU.add,
                       [pbuf[tb]], [b_rrep])
                k.op("act", lambda e: e.sqrt(out=rrep[:], in_=rrep[:]), reads=[b_rrep], writes=[b_rrep])
                k.op("dve", lambda e: e.reciprocal(out=rrep[:], in_=rrep[:]), reads=[b_rrep], writes=[b_rrep])
                for c in range(16):
                    stt("dve", yg[:, c, :], yg[:, c, :], wncol[:, c:c + 1], rrep[:], ALU.mult, ALU.mult,
                        [b_yg, b_wn, b_rrep], [b_yg])
                if "ynT_s" in debug:
                    ynT_s = scr("ynT_s", [2048, S], BF16)
                    k.dma("sp", ynT_s.rearrange("(c p) t -> p c t", p=128), yg[:], reads=[b_yg], writes=[k.buf()])
                wso = [sbD("wso%d" % i, [128, 16, 128], BF16) for i in range(2)]
                wao = [sbD("wao%d" % i, [128, 8, 128], BF16) for i in range(2)]
                sg1 = [sbD("sg1%d" % i, [128, S], BF16) for i in range(2)]
                sg2 = [sbD("sg2%d" % i, [128, S], BF16) for i in range(2)]
                b_wso, b_wao, b_sg1, b_sg2 = ([k.buf(), k.buf()] for _ in range(4))
                t1 = [sbD("t1%d" % i, [128, 512], F32) for i in range(2)]
                t2 = [sbD("t2%d" % i, [128, 512], F32) for i in range(2)]
                b_t1, b_t2 = [k.buf(), k.buf()], [k.buf(), k.buf()]
                mrg = [sbD("mrg%d" % i, [128, S], BF16) for i in range(2)]
                b_mrg = [k.buf(), k.buf()]
                wso_v = I("w_ssm_out").rearrange("(c p) n -> p c n", p=128)
                wao_v = I("w_attn_out").rearrange("(c p) n -> p c n", p=128)
                ndc = NCH if (ALLP or "D_full" in phases) else 2
                cnt = 0
                for dc in range(ndc):
                    i2 = dc % 2
                    k.dma("pool", wso[i2][:], wso_v[:, :, dc * 128:(dc + 1) * 128], writes=[b_wso[i2]])
                    k.dma("pool", wao[i2][:], wao_v[:, :, dc * 128:(dc + 1) * 128], writes=[b_wao[i2]])
                    k.dma("sp", sg1[i2][:], sgT_s[dc * 128:(dc + 1) * 128, :], writes=[b_sg1[i2]])
                    k.dma("sp", sg2[i2][:], sgT_s[D + dc * 128:D + (dc + 1) * 128, :], writes=[b_sg2[i2]])
                    for tb in range(4):
                        sl = slice(tb * 512, (tb + 1) * 512)
                        p1, p1b = next_bank()
                        for c in range(16):
                            mm(p1[:, :], wso[i2][:, c, :], yg[:, c, sl], c == 0, c == 15, [b_wso[i2], b_yg], [p1b], inc=(c == 15))
                        p2, p2b = next_bank()
                        for c in range(8):
                            mm(p2[:, :], wao[i2][:, c, :], oTt[:, c, sl], c == 0, c == 7, [b_wao[i2], b_oTt], [p2b], inc=(c == 7))
                        j2 = cnt % 2
                        cnt += 1
                        tt("dve", t1[j2][:], p1[:, :], sg1[i2][:, sl], ALU.mult, [p1b, b_sg1[i2]], [b_t1[j2]])
                        tt("dve", t2[j2][:], p2[:, :], sg2[i2][:, sl], ALU.mult, [p2b, b_sg2[i2]], [b_t2[j2]])
                        tt("pool", mrg[i2][:, sl], t1[j2][:], t2[j2][:], ALU.add, [b_t1[j2], b_t2[j2]], [b_mrg[i2]])
                    k.dma("sp", mT_s[dc * 128:(dc + 1) * 128, :], mrg[i2][:], reads=[b_mrg[i2]], writes=[k.buf()])
                k.barrier()
            with ExitStack() as sD:
                def sbD(name, shape, dt):
                    return sD.enter_context(nc.sbuf_tensor(name, list(shape), dt))
                mT = sbD("mT", [128, NCH, S], BF16)
                b_mT = k.buf()
                mT_v = mT_s.rearrange("(c p) t -> p c t", p=128)
                for q4 in range(4):
                    k.dma("sp" if q4 % 2 else "act", mT[:, q4 * 8:(q4 + 1) * 8, :], mT_v[:, q4 * 8:(q4 + 1) * 8, :], writes=[b_mT])
                g1rep = sbD("g1rep", [128, D], F32)
                b_g1 = k.buf()
                k.dma("sp", g1rep[:], modrow_s.rearrange("(m c) p -> m (c p)", m=6)[2:3, :].partition_broadcast(128), writes=[b_g1])
                WB = 256
                wo = [sbD("wo%d" % i, [128, NCH, WB], BF16) for i in range(2)]
                b_wo = [k.buf(), k.buf()]
                xt_ = [sbD("xtD%d" % i, [128, WB], F32) for i in range(3)]
                b_xt_ = [k.buf() for _ in range(3)]
                tmpD = [sbD("tmpD%d" % i, [128, WB], F32) for i in range(2)]
                b_tmpD = [k.buf(), k.buf()]
                wo_v = I("w_o").rearrange("(c p) n -> p c n", p=128)
                nnb = D // WB if (ALLP or "D_full" in phases) else 1
                cnt = 0
                for nb2 in range(nnb):
                    i2 = nb2 % 2
                    cs_ = slice(nb2 * WB, (nb2 + 1) * WB)
                    k.dma("pool", wo[i2][:], wo_v[:, :, cs_], writes=[b_wo[i2]])
                    for tt_ in range(NT):
                        i3 = cnt % 3
                        j2 = cnt % 2
                        cnt += 1
                        k.dma("sp", xt_[i3][:], I("x")[tt_ * 128:(tt_ + 1) * 128, cs_], writes=[b_xt_[i3]])
                        pb, pbb = next_bank()
                        for c in range(NCH):
                            mm(pb[:, 0:WB], mT[:, c, tt_ * 128:(tt_ + 1) * 128], wo[i2][:, c, :], c == 0, c == NCH - 1,
                               [b_mT, b_wo[i2]], [pbb], inc=(c == NCH - 1))
                        tt("dve", tmpD[j2][:], pb[:, 0:WB], g1rep[:, cs_], ALU.mult, [pbb, b_g1], [b_tmpD[j2]])
                        tt("pool", xt_[i3][:], xt_[i3][:], tmpD[j2][:], ALU.add, [b_xt_[i3], b_tmpD[j2]], [b_xt_[i3]])
                        k.dma("act", x1_s[tt_ * 128:(tt_ + 1) * 128, cs_], xt_[i3][:], reads=[b_xt_[i3]], writes=[k.buf()])
                k.barrier()
        CAP = 256
        NE = 16
        if ALLP or "pE" in phases:
            with ExitStack() as sE0:
                def sbE(name, shape, dt):
                    return sE0.enter_context(nc.sbuf_tensor(name, list(shape), dt))
                afft = sbE("afft", [128, NT, NE], F32)
                msk = sbE("msk", [128, NT, NE], F32)
                rnk = sbE("rnk", [128, NT, NE], F32)
                wtm = sbE("wtm", [128, NT, NE], F32)
                rankT = sbE("rankT", [NE, S], F32)
                wT = sbE("wT", [NE, S], F32)
                b_aff, b_msk, b_rnk, b_wtm, b_rankT, b_wT = (k.buf() for _ in range(6))
                with ExitStack() as sE1:
                    def sb1(name, shape, dt):
                        return sE1.enter_context(nc.sbuf_tensor(name, list(shape), dt))
                    h2T = sb1("h2T", [128, NCH, S], BF16)
                    b_h2T = [k.buf() for _ in range(NT)]
                    with ExitStack() as sE2:
                        norm_to_hT(sE2, x1_s, n2col, 3, 4, h2T, b_h2T, "e")
                        k.barrier()
                    if "h2T_s" in debug:
                        h2T_s = scr("h2T_s", [D, S], BF16)
                        k.dma("sp", h2T_s.rearrange("(c p) t -> p c t", p=128), h2T[:], reads=b_h2T, writes=[k.buf()])
                    wr = sb1("wr", [128, NCH, NE], BF16)
                    b_wr = k.buf()
                    k.dma("pool", wr[:], I("w_router").rearrange("(c p) n -> p c n", p=128), writes=[b_wr])
                    lg = sb1("lg", [128, NT, NE], F32)
                    b_lg = k.buf()
                    for tt_ in range(NT):
                        pb, pbb = next_bank()
                        for c in range(NCH):
                            mm(pb[:, 0:NE], h2T[:, c, tt_ * 128:(tt_ + 1) * 128], wr[:, c, :], c == 0, c == NCH - 1,
                               [b_wr, b_h2T[tt_]], [pbb], inc=(c == NCH - 1))
                        cp("dve", lg[:, tt_, :], pb[:, 0:NE], [pbb], [b_lg])
                    mx = sb1("mx", [128, NT], F32)
                    sm = sb1("sm", [128, NT], F32)
                    b_mx = k.buf()
                    k.op("dve", lambda e: e.tensor_reduce(out=mx[:], in_=lg[:], axis=AX.X, op=ALU.max), reads=[b_lg], writes=[b_mx])
                    ts("dve", mx[:], mx[:], -1.0, None, ALU.mult, None, [b_mx], [b_mx])
                    for tt_ in range(NT):
                        actf(afft[:, tt_, :], lg[:, tt_, :], AF.Exp, [b_lg, b_mx], [b_aff], bias=mx[:, tt_:tt_ + 1], scale=1.0)
                    k.op("dve", lambda e: e.tensor_reduce(out=sm[:], in_=afft[:], axis=AX.X, op=ALU.add), reads=[b_aff], writes=[b_mx])
                    k.op("dve", lambda e: e.reciprocal(out=sm[:], in_=sm[:]), reads=[b_mx], writes=[b_mx])
                    for tt_ in range(NT):
                        ts("dve", afft[:, tt_, :], afft[:, tt_, :], sm[:, tt_:tt_ + 1], None, ALU.mult, None, [b_aff, b_mx], [b_aff])
                    h2st = [sb1("h2st%d" % i, [128, D], BF16) for i in range(2)]
                    b_h2st = [k.buf(), k.buf()]
                    for tt_ in range(NT):
                        st_, bst = h2st[tt_ % 2], b_h2st[tt_ % 2]
                        for q8 in range(4):
                            pt, ptbf = next_tbank()
                            for j in range(8):
                                c = q8 * 8 + j
                                k.op("pe", lambda e, pt=pt, j=j, c=c, tt_=tt_: e.transpose(
                                    pt[:, j * 128:(j + 1) * 128], h2T[:, c, tt_ * 128:(tt_ + 1) * 128], identb[:]),
                                    reads=[b_h2T[tt_], b_identb], writes=[ptbf], inc=(j == 7))
                            cp(evac_eng(), st_[:, q8 * 1024:(q8 + 1) * 1024], pt[:, :], [ptbf], [bst])
                        k.dma("sp", h2_s[tt_ * 128:(tt_ + 1) * 128, :], st_[:], reads=[bst], writes=[k.buf()])
                    affT = sb1("affT", [NE, S], F32)
                    b_affT = k.buf()
                    for q4 in range(4):
                        pb, pbb = next_bank()
                        for j in range(4):
                            tt_ = q4 * 4 + j
                            k.op("pe", lambda e, pb=pb, j=j, tt_=tt_: e.transpose(
                                pb[0:NE, j * 128:(j + 1) * 128], afft[:, tt_, :], ident[:]),
                                reads=[b_aff, b_ident], writes=[pbb], inc=(j == 3))
                        cp("dve", affT[:, q4 * 512:(q4 + 1) * 512], pb[0:NE, :], [pbb], [b_affT])
                    lo = sb1("lo", [NE, 1], F32)
                    mid = sb1("mid", [NE, 1], F32)
                    cntc = sb1("cntc", [NE, 1], F32)
                    gec = sb1("gec", [NE, 1], F32)
                    cmpj = sb1("cmpj", [NE, S], F32)
                    b_lo, b_mid, b_cnt, b_ge, b_cmp = (k.buf() for _ in range(5))
                    k.op("dve", lambda e: e.memset(lo[:], 0.0), writes=[b_lo])
                    for it in range(1, 31):
                        wdt = 2.0 ** (-it)
                        ts("dve", mid[:], lo[:], wdt, None, ALU.add, None, [b_lo], [b_mid])
                        ts("dve", cmpj[:], affT[:], mid[:, 0:1], None, ALU.is_ge, None, [b_affT, b_mid], [b_cmp])
                        k.op("dve", lambda e: e.tensor_reduce(out=cntc[:], in_=cmpj[:], axis=AX.X, op=ALU.add),
                             reads=[b_cmp], writes=[b_cnt])
                        ts("dve", gec[:], cntc[:], float(CAP), None, ALU.is_ge, None, [b_cnt], [b_ge])
                        stt("dve", lo[:], gec[:], wdt, lo[:], ALU.mult, ALU.add, [b_ge, b_lo], [b_lo])
                    dg = sb1("dg", [NE, NE], F32)
                    ones16 = sb1("ones16", [NE, 128], F32)
                    threp = sb1("threp", [128, NE], F32)
                    b_dg, b_threp = k.buf(), k.buf()
                    k.op("pool", lambda e: e.memset(ones16[:], 1.0), writes=[b_dg])
                    ts("dve", dg[:], ident[0:NE, 0:NE], lo[:, 0:1], None, ALU.mult, None, [b_lo, b_ident], [b_dg])
                    pb, pbb = next_bank()
                    mm(pb[:, 0:NE], ones16[:], dg[:], True, True, [b_dg], [pbb])
                    cp("dve", threp[:], pb[:, 0:NE], [pbb], [b_threp])
                    for tt_ in range(NT):
                        tt("dve", msk[:, tt_, :], afft[:, tt_, :], threp[:], ALU.is_ge, [b_aff, b_threp], [b_msk])
                    mskf = msk[:, :, :].rearrange("p a b -> p (a b)")
                    tt("dve", wtm[:, :, :].rearrange("p a b -> p (a b)"), mskf, afft[:, :, :].rearrange("p a b -> p (a b)"),
                       ALU.mult, [b_msk, b_aff], [b_wtm])
                    triE2 = sb1("triE2", [128, 128], F32)
                    onesf2 = sb1("onesf2", [128, 128], F32)
                    b_tri2 = k.buf()
                    k.op("pool", lambda e: e.memset(onesf2[:], 1.0), writes=[b_tri2])
                    k.op("pool", lambda e: e.memset(triE2[:], 1.0), writes=[b_tri2])
                    k.op("pool", lambda e: e.affine_select(out=triE2[:], in_=triE2[:], pattern=[[1, 128]], compare_op=ALU.is_ge,
                                                           fill=0.0, base=-1, channel_multiplier=-1), reads=[b_tri2], writes=[b_tri2])
                    pbk, pbkb = next_bank()
                    for i in range(NT):
                        for j in range(i + 1):
                            mm(pbk[:, i * NE:(i + 1) * NE], (triE2 if j == i else onesf2)[:], msk[:, j, :], j == 0, j == i,
                               [b_msk, b_tri2], [pbkb], inc=(i == NT - 1 and j == i))
                    cp("dve", rnk[:, :, :].rearrange("p a b -> p (a b)"), pbk[:, 0:NT * NE], [pbkb], [b_rnk])
                    for src, srcb, dst, dstb in ((rnk, b_rnk, rankT, b_rankT), (wtm, b_wtm, wT, b_wT)):
                        for q4 in range(4):
                            pb, pbb = next_bank()
                            for j in range(4):
                                tt_ = q4 * 4 + j
                                k.op("pe", lambda e, pb=pb, j=j, tt_=tt_, src=src: e.transpose(
                                    pb[0:NE, j * 128:(j + 1) * 128], src[:, tt_, :], ident[:]),
                                    reads=[srcb, b_ident], writes=[pbb], inc=(j == 3))
                            cp("dve", dst[:, q4 * 512:(q4 + 1) * 512], pb[0:NE, :], [pbb], [dstb])
                    if "aff_s" in debug:
                        aff_s = scr("aff_s", [128, NT * NE], F32)
                        msk_s = scr("msk_s", [128, NT * NE], F32)
                        rnk_s = scr("rnk_s", [128, NT * NE], F32)
                        k.dma("sp", aff_s[:, :], afft[:, :, :].rearrange("p a b -> p (a b)"), reads=[b_aff], writes=[k.buf()])
                        k.dma("sp", msk_s[:, :], mskf, reads=[b_msk], writes=[k.buf()])
                        k.dma("sp", rnk_s[:, :], rnk[:, :, :].rearrange("p a b -> p (a b)"), reads=[b_rnk], writes=[k.buf()])
                    k.barrier()

                with ExitStack() as sE3:
                    def sb3(name, shape, dt):
                        return sE3.enter_context(nc.sbuf_tensor(name, list(shape), dt))
                    iot = sb3("iot", [128, CAP], F32)
                    b_iot = k.buf()
                    k.op("pool", lambda e: e.iota(iot[:], pattern=[[1, CAP]], base=0, channel_multiplier=0,
                                                  allow_small_or_imprecise_dtypes=True), writes=[b_iot])
                    Pe = [sb3("Pe%d" % i, [128, NT, CAP], BF16) for i in range(2)]
                    b_Pe = [k.buf(), k.buf()]
                    h2q = [sb3("h2q%d" % i, [128, NT, 512], BF16) for i in range(2)]
                    b_h2q = [k.buf(), k.buf()]
                    xeT = sb3("xeT", [128, NCH, CAP], BF16)
                    b_xeT = [k.buf() for _ in range(8)]
                    wg = [sb3("wg%d" % i, [128, NCH, 128], BF16) for i in range(2)]
                    wu = [sb3("wu%d" % i, [128, NCH, 128], BF16) for i in range(2)]
                    b_wg, b_wu = [k.buf(), k.buf()], [k.buf(), k.buf()]
                    sgt = [sb3("sgt%d" % i, [128, CAP], F32) for i in range(2)]
                    b_sgt = [k.buf(), k.buf()]
                    hid = sb3("hid", [128, 16, CAP], BF16)
                    b_hid = k.buf()
                    wd = [sb3("wd%d" % i, [128, 16, 512], BF16) for i in range(2)]
                    b_wd = [k.buf(), k.buf()]
                    ysb = [sb3("ysb%d" % i, [128, 512], BF16) for i in range(2)]
                    b_ysb = [k.buf(), k.buf()]
                    h2_v = h2_s.rearrange("(tt p) n -> p tt n", p=128)
                    nexp = NE if (ALLP or "E_full" in phases) else 1
                    qc = 0
                    fc = 0
                    dc_ = 0
                    for ex in range(nexp):
                        P_, bP = Pe[ex % 2], b_Pe[ex % 2]
                        for tt_ in range(NT):
                            ts("dve", P_[:, tt_, :], iot[:], rnk[:, tt_, ex:ex + 1], msk[:, tt_, ex:ex + 1], ALU.is_equal, ALU.mult,
                               [b_iot, b_rnk, b_msk], [bP])
                        for q in range(8):
                            hq, bhq = h2q[qc % 2], b_h2q[qc % 2]
                            qc += 1
                            k.dma("sp" if q % 2 else "act", hq[:], h2_v[:, :, q * 512:(q + 1) * 512], writes=[bhq])
                            for c2 in range(2):
                                pb, pbb = next_bank()
                                for hh in range(2):
                                    cl = c2 * 2 + hh
                                    for tt_ in range(NT):
                                        mm(pb[:, hh * CAP:(hh + 1) * CAP], hq[:, tt_, cl * 128:(cl + 1) * 128], P_[:, tt_, :],
                                           tt_ == 0, tt_ == NT - 1, [bhq, bP], [pbb], inc=(hh == 1 and tt_ == NT - 1))
                                c = q * 4 + c2 * 2
                                cp(evac_eng(), xeT[:, c:c + 2, :], pb[:, :].rearrange("p (a b) -> p a b", b=CAP), [pbb], [b_xeT[q]])
                        for fb in range(16):
                            wg_, bwg = wg[fc % 2], b_wg[fc % 2]
                            wu_, bwu = wu[fc % 2], b_wu[fc % 2]
                            sg_, bsg = sgt[fc % 2], b_sgt[fc % 2]
                            fc += 1
                            k.dma("pool", wg_[:], I("w_gate_e")[ex].rearrange("(c p) n -> p c n", p=128)[:, :, fb * 128:(fb + 1) * 128],
                                  writes=[bwg])
                            k.dma("pool", wu_[:], I("w_up_e")[ex].rearrange("(c p) n -> p c n", p=128)[:, :, fb * 128:(fb + 1) * 128],
                                  writes=[bwu])
                            pb, pbb = next_bank()
                            for c in range(NCH):
                                mm(pb[:, 0:CAP], wg_[:, c, :], xeT[:, c, :], c == 0, c == NCH - 1, [bwg] + b_xeT, [pbb], inc=False)
                            for c in range(NCH):
                                mm(pb[:, CAP:2 * CAP], wu_[:, c, :], xeT[:, c, :], c == 0, c == NCH - 1, [bwu] + b_xeT, [pbb],
                                   inc=(c == NCH - 1))
                            actf(sg_[:], pb[:, 0:CAP], AF.Silu, [pbb], [bsg])
                            tt("dve", hid[:, fb, :], sg_[:], pb[:, CAP:2 * CAP], ALU.mult, [bsg, pbb], [b_hid])
                        for nb2 in range(8):
                            wd_, bwd = wd[dc_ % 2], b_wd[dc_ % 2]
                            dc_ += 1
                            k.dma("pool", wd_[:], I("w_down_e")[ex].rearrange("(c p) n -> p c n", p=128)[:, :, nb2 * 512:(nb2 + 1) * 512],
                                  writes=[bwd])
                            for st2 in range(2):
                                pb, pbb = next_bank()
                                for fb in range(16):
                                    mm(pb[:, :], hid[:, fb, st2 * 128:(st2 + 1) * 128], wd_[:, fb, :], fb == 0, fb == 15,
                                       [b_hid, bwd], [pbb], inc=(fb == 15))
                                ys_, bys = ysb[st2], b_ysb[st2]
                                cp(evac_eng(), ys_[:], pb[:, :], [pbb], [bys])
                                k.dma("sp", y_s[ex * CAP + st2 * 128:ex * CAP + (st2 + 1) * 128, nb2 * 512:(nb2 + 1) * 512], ys_[:],
                                      reads=[bys], writes=[k.buf()])
                    k.barrier()

                with ExitStack() as sF:
                    def sbF(name, shape, dt):
                        return sF.enter_context(nc.sbuf_tensor(name, list(shape), dt))
                    TG = 256
                    sel = sbF("sel", [NE, NE, 128], F32)
                    b_sel = k.buf()
                    k.op("pool", lambda e: e.memset(sel[:], 0.0), writes=[b_sel])
                    k.op("pool", lambda e: e.affine_select(out=sel[:], in_=sel[:], pattern=[[-1, NE], [0, 128]],
                                                           compare_op=ALU.not_equal, fill=1.0, base=0, channel_multiplier=1),
                         reads=[b_sel], writes=[b_sel])
                    slotc = sbF("slotc", [128, 2], F32)
                    b_slotc = k.buf()
                    k.op("pool", lambda e: e.iota(slotc[:], pattern=[[128, 2]], base=0, channel_multiplier=1,
                                                  allow_small_or_imprecise_dtypes=True), writes=[b_slotc])
                    g2rep = sbF("g2rep", [128, D], F32)
                    nfrep = sbF("nfrep", [128, D], F32)
                    b_g2, b_nf = k.buf(), k.buf()
                    k.dma("sp", g2rep[:], modrow_s.rearrange("(m c) p -> m (c p)", m=6)[5:6, :].partition_broadcast(128), writes=[b_g2])
                    k.dma("act", nfrep[:], I("normf_w").rearrange("c p -> (c p)").partition_broadcast(128), writes=[b_nf])
                    PT = sbF("PT", [128, 2 * NE, TG], BF16)
                    b_PT = k.buf()
                    wrep = [sbF("wrep%d" % i, [128, TG], F32) for i in range(2)]
                    b_wrep = [k.buf(), k.buf()]
                    yall = [sbF("yall%d" % i, [128, 2 * NE, 256], BF16) for i in range(2)]
                    b_yall = [k.buf(), k.buf()]
                    x2 = sbF("x2", [128, 2, D], F32)
                    b_x2 = [k.buf(), k.buf()]
                    tmpF = [sbF("tmpF%d" % i, [128, 512], F32) for i in range(2)]
                    b_tmpF = [k.buf(), k.buf()]
                    junkF = sbF("junkF", [128, D], BF16)
                    b_junkF = k.buf()
                    ssF = sbF("ssF", [128, NT], F32)
                    b_ssF = k.buf()
                    k.op("pool", lambda e: e.memset(ssF[:], 0.0), writes=[b_ssF])
                    y_v = y_s.rearrange("(eh p) n -> p eh n", p=128)
                    ntg = S // TG if (ALLP or "F_full" in phases) else 1
                    yc = 0
                    wc = 0
                    for tg in range(ntg):
                        tsl = slice(tg * TG, (tg + 1) * TG)
                        for ti in range(2):
                            k.dma("act", x2[:, ti, :], x1_s[tg * TG + ti * 128:tg * TG + (ti + 1) * 128, :], writes=[b_x2[ti]])
                        for ex in range(NE):
                            pr, prb = next_bank()
                            mm(pr[:, 0:TG], sel[:, ex, :], rankT[:, tsl], True, True, [b_sel, b_rankT], [prb], inc=False)
                            mm(pr[:, TG:2 * TG], sel[:, ex, :], wT[:, tsl], True, True, [b_sel, b_wT], [prb], inc=True)
                            wr_, bwr = wrep[wc % 2], b_wrep[wc % 2]
                            wc += 1
                            cp("act", wr_[:], pr[:, TG:2 * TG], [prb], [bwr])
                            for half in range(2):
                                stt("dve", PT[:, 2 * ex + half, :], pr[:, 0:TG], slotc[:, half:half + 1], wr_[:], ALU.is_equal, ALU.mult,
                                    [prb, b_slotc, bwr], [b_PT])
                        for nb2 in range(16):
                            ya, bya = yall[yc % 2], b_yall[yc % 2]
                            yc += 1
                            k.dma("sp", ya[:], y_v[:, :, nb2 * 256:(nb2 + 1) * 256], writes=[bya])
                            for ti in range(2):
                                pb, pbb = next_bank()
                                for eh in range(2 * NE):
                                    mm(pb[:, 0:256], PT[:, eh, ti * 128:(ti + 1) * 128], ya[:, eh, :], eh == 0, eh == 2 * NE - 1,
                                       [b_PT, bya], [pbb], inc=(eh == 2 * NE - 1))
                                tf, btf = tmpF[ti], b_tmpF[ti]
                                csl = slice(nb2 * 256, (nb2 + 1) * 256)
                                tt("dve", tf[:, 0:256], pb[:, 0:256], g2rep[:, csl], ALU.mult, [pbb, b_g2], [btf])
                                tt("pool", x2[:, ti, csl], x2[:, ti, csl], tf[:, 0:256], ALU.add, [b_x2[ti], btf], [b_x2[ti]])
                        for ti in range(2):
                            col = tg * 2 + ti
                            actf(junkF[:], x2[:, ti, :], AF.Square, [b_x2[ti]], [b_junkF, b_ssF], accum_out=ssF[:, col:col + 1])
                            ts("dve", ssF[:, col:col + 1], ssF[:, col:col + 1], 1.0 / D, 1e-6, ALU.mult, ALU.add, [b_ssF], [b_ssF])
                            k.op("act", lambda e, col=col: e.sqrt(out=ssF[:, col:col + 1], in_=ssF[:, col:col + 1]),
                                 reads=[b_ssF], writes=[b_ssF])
                            k.op("dve", lambda e, col=col: e.reciprocal(out=ssF[:, col:col + 1], in_=ssF[:, col:col + 1]),
                                 reads=[b_ssF], writes=[b_ssF])
                            stt("dve", x2[:, ti, :], x2[:, ti, :], ssF[:, col:col + 1], nfrep[:], ALU.mult, ALU.mult,
                                [b_x2[ti], b_ssF, b_nf], [b_x2[ti]])
                            k.dma("sp", out[tg * TG + ti * 128:tg * TG + (ti + 1) * 128, :], x2[:, ti, :], reads=[b_x2[ti]],
                                  writes=[k.buf()])
                    k.barrier()

        for q in ("sp", "pool", "act"):
            e = k.eng[q]
            for sem, cnt in k.dsem[q]:
                if cnt > 0 and e.known.get(id(sem), 0) < cnt:
                    e.h.wait_ge(sem, cnt)
                    e.known[id(sem)] = cnt
    return nc


_PROG = {}
N_ACTIVE = 4


def _core_inputs(inp, b):
    f = lambda a: np.ascontiguousarray(np.asarray(a, dtype=np.float32))
    return {
        "x": f(inp["x"][b]), "c": f(inp["c"][b]).reshape(32, 128),
        "norm1_w": f(inp["norm1_w"][0]).reshape(32, 128), "norm2_w": f(inp["norm2_w"][0]).reshape(32, 128),
        "normf_w": f(inp["normf_w"]).reshape(32, 128), "w_ada": f(inp["w_ada"][0]),
        "b_ada": f(inp["b_ada"][0]).reshape(192, 128), "w_in": f(inp["w_in"][0]),
        "conv_w": f(inp["conv_w"][0]).reshape(5, 32, 128), "conv_b": f(inp["conv_b"][0]).reshape(32, 128),
        "dt_bias_f": f(inp["dt_bias_f"][0]), "dt_bias_b": f(inp["dt_bias_b"][0]),
        "a_log_f": f(inp["a_log_f"][0]), "a_log_b": f(inp["a_log_b"][0]), "d_skip": f(inp["d_skip"][0]),
        "ssm_norm_w": f(inp["ssm_norm_w"][0]).reshape(16, 128), "w_ssm_out": f(inp["w_ssm_out"][0]),
        "w_attn_out": f(inp["w_attn_out"][0]), "w_o": f(inp["w_o"][0]), "w_router": f(inp["w_router"][0]),
        "w_gate_e": f(inp["w_gate_e"][0]), "w_up_e": f(inp["w_up_e"][0]), "w_down_e": f(inp["w_down_e"][0]),
    }


def kernel(**inputs):
    if "nc" not in _PROG:
        _PROG["nc"] = build_program()
    nc = _PROG["nc"]
    shared = _core_inputs(inputs, 0)
    in_maps = []
    for b in range(N_ACTIVE):
        m = dict(shared)
        m["x"] = np.ascontiguousarray(np.asarray(inputs["x"][b], dtype=np.float32))
        m["c"] = np.ascontiguousarray(np.asarray(inputs["c"][b], dtype=np.float32)).reshape(32, 128)
        in_maps.append(m)
    res = run_bass_kernel_spmd(nc, in_maps, core_ids=list(range(N_ACTIVE)))
    return np.stack([np.asarray(res.results[b]["out"], dtype=np.float32) for b in range(N_ACTIVE)], axis=0)
```

```python
import math
import numpy as np
import concourse.bass as bass
import concourse.mybir as mybir
from concourse.bass_utils import run_bass_kernel_spmd

F32 = mybir.dt.float32
BF16 = mybir.dt.bfloat16
I32 = mybir.dt.int32
AF = mybir.ActivationFunctionType
ALU = mybir.AluOpType
AX = mybir.AxisListType

D = 4096
S = 2048
NT = S // 128
NCH = D // 128
IN_DIM = 23616
OFF_Z, OFF_XBC, OFF_DT, OFF_Q, OFF_K, OFF_V, OFF_G = 0, 2048, 6144, 6208, 9280, 12352, 15424
NEG = -30000.0


class Buf:
    __slots__ = ("name", "w", "r")

    def __init__(self, name):
        self.name = name
        self.w = {}
        self.r = {}


class Eng:
    def __init__(self, name, h, sem):
        self.name, self.h, self.sem = name, h, sem
        self.count = 0
        self.known = {}
        self.pend_r, self.pend_w = [], []


class K:
    def __init__(self, nc, stack):
        self.nc = nc
        self.stack = stack
        self.eng = {}
        for nm, h in (("pe", nc.tensor), ("dve", nc.vector), ("act", nc.scalar),
                      ("pool", nc.gpsimd), ("sp", nc.sync)):
            sem = stack.enter_context(nc.semaphore("s_" + nm))
            self.eng[nm] = Eng(nm, h, sem)
        self.dsem = {}
        for q, n in (("sp", 24), ("pool", 24), ("act", 8)):
            self.dsem[q] = [[stack.enter_context(nc.semaphore("d_%s%d" % (q, i))), 0] for i in range(n)]
        self.dptr = {"sp": 0, "pool": 0, "act": 0}
        self.nbuf = 0

    def buf(self, name=None):
        self.nbuf += 1
        return Buf(name or "b%d" % self.nbuf)

    def _waits(self, e, reads, writes):
        need = {}

        def add(ev):
            if ev is None:
                return
            s, v = ev
            k = id(s)
            if k not in need or need[k][1] < v:
                need[k] = (s, v)
        for b in reads:
            for ev in b.w.values():
                add(ev)
        for b in writes:
            for ev in b.w.values():
                add(ev)
            for ev in b.r.values():
                add(ev)
        for k, (s, v) in need.items():
            if e.name == "pe" and s is e.sem:
                continue
            if e.known.get(k, 0) < v:
                e.h.wait_ge(s, v)
                e.known[k] = v

    def _commit(self, ev, reads, writes):
        kk = id(ev[0])
        for b in reads:
            if kk not in b.r or b.r[kk][1] < ev[1]:
                b.r[kk] = ev
        for b in writes:
            if kk not in b.w or b.w[kk][1] < ev[1]:
                b.w[kk] = ev
            b.r = {}

    def op(self, en, fn, reads=(), writes=(), inc=True):
        e = self.eng[en]
        self._waits(e, reads, writes)
        ins = fn(e.h)
        if not inc:
            e.pend_r.extend(reads)
            e.pend_w.extend(writes)
            return ins
        e.count += 1
        ins.then_inc(e.sem, 1)
        ev = (e.sem, e.count)
        e.known[id(e.sem)] = max(e.known.get(id(e.sem), 0), 0)
        self._commit(ev, list(reads) + e.pend_r, list(writes) + e.pend_w)
        e.pend_r, e.pend_w = [], []
        return ins

    def dma(self, q, out, in_, reads=(), writes=(), **kw):
        e = self.eng[q]
        pool = self.dsem[q]
        i = self.dptr[q]
        self.dptr[q] = (i + 1) % len(pool)
        sem, cnt = pool[i]
        if cnt > 0 and e.known.get(id(sem), 0) < cnt:
            e.h.wait_ge(sem, cnt)
            e.known[id(sem)] = cnt
        self._waits(e, reads, writes)
        e.h.dma_start(out=out, in_=in_, **kw).then_inc(sem, 16)
        pool[i][1] = cnt + 16
        ev = (sem, cnt + 16)
        self._commit(ev, reads, writes)
        return ev

    def wait_all(self, en, bufs):
        e = self.eng[en]
        self._waits(e, bufs, bufs)

    def barrier(self):
        for e in self.eng.values():
            assert not e.pend_r and not e.pend_w, e.name
        for e in self.eng.values():
            for o in self.eng.values():
                if o.count > 0 and e.known.get(id(o.sem), 0) < o.count and o is not e:
                    e.h.wait_ge(o.sem, o.count)
                    e.known[id(o.sem)] = o.count
            for q in self.dsem:
                for sem, cnt in self.dsem[q]:
                    if cnt > 0 and e.known.get(id(sem), 0) < cnt:
                        e.h.wait_ge(sem, cnt)
                        e.known[id(sem)] = cnt


def cdiv(a, b):
    return (a + b - 1) // b


def build_program(phases=("all",), debug=(), feed=()):
    from contextlib import ExitStack
    nc = bass.Bass("TRN2", target_bir_lowering=False)
    ALLP = "all" in phases

    def inp(name, shape, dt=F32):
        return nc.dram_tensor(name, list(shape), dt, kind="ExternalInput").ap()

    def scr(name, shape, dt):
        kind = "ExternalOutput" if name in debug else ("ExternalInput" if name in feed else "Internal")
        return nc.dram_tensor(name, list(shape), dt, kind=kind).ap()

    IN_SHAPES = {
        "x": [S, D], "c": [NCH, 128], "norm1_w": [NCH, 128], "norm2_w": [NCH, 128], "normf_w": [NCH, 128],
        "w_ada": [D, 6 * D], "b_ada": [6 * NCH, 128], "w_in": [D, IN_DIM], "conv_w": [5, NCH, 128],
        "conv_b": [NCH, 128], "dt_bias_f": [32], "dt_bias_b": [32], "a_log_f": [32], "a_log_b": [32],
        "d_skip": [32], "ssm_norm_w": [16, 128], "w_ssm_out": [2048, D], "w_attn_out": [1024, D],
        "w_o": [D, D], "w_router": [D, 16], "w_gate_e": [16, D, 2048], "w_up_e": [16, D, 2048],
        "w_down_e": [16, 2048, D],
    }
    _ins = {}

    def I(name):
        if name not in _ins:
            _ins[name] = inp(name, IN_SHAPES[name])
        return _ins[name]

    out = nc.dram_tensor("out", [S, D], F32, kind="ExternalOutput").ap() if (ALLP or "out" in debug) else None

    modc_s = scr("modc_s", [128, 6 * NCH], F32)
    modrow_s = scr("modrow_s", [6 * NCH, 128], F32)
    hT_s = scr("hT_s", [D, S], BF16) if "hT_s" in debug else None
    zT_s = scr("zT_s", [2048, S], BF16)
    xs_s = scr("xs_s", [S, 2048], BF16)
    xsT_s = scr("xsT_s", [2048, S], BF16)
    BT_s = scr("BT_s", [1024, S], BF16)
    CT_s = scr("CT_s", [1024, S], BF16)
    dt_s = scr("dt_s", [S, 64], F32)
    csT_s = scr("csT_s", [64, S], F32)
    qT_s = scr("qT_s", [3072, S], BF16)
    kT_s = scr("kT_s", [3072, S], BF16)
    v_s = scr("v_s", [S, 3072], BF16)
    sgT_s = scr("sgT_s", [8192, S], BF16)
    ygT_s = scr("ygT_s", [2048, S], BF16)
    oT_s = scr("oT_s", [1024, S], BF16)
    mT_s = scr("mT_s", [D, S], BF16)
    x1_s = scr("x1_s", [S, D], F32)
    h2_s = scr("h2_s", [S, D], BF16)
    y_s = scr("y_s", [16 * 256, D], BF16)

    with ExitStack() as st:
        k = K(nc, st)

        def sb(name, shape, dt):
            return st.enter_context(nc.sbuf_tensor(name, list(shape), dt))

        def ps(name, shape, dt=F32):
            return st.enter_context(nc.psum_tensor(name, list(shape), dt))

        ident = sb("ident", [128, 128], F32)
        identb = sb("identb", [128, 128], BF16)
        b_ident = k.buf("ident")
        k.op("pool", lambda e: e.memset(ident[:], 0.0), writes=[b_ident])
        k.op("pool", lambda e: e.affine_select(out=ident[:], in_=ident[:], pattern=[[-1, 128]],
                                               compare_op=ALU.not_equal, fill=1.0, base=0,
                                               channel_multiplier=1), reads=[b_ident], writes=[b_ident])
        b_identb = k.buf("identb")
        k.op("pool", lambda e: e.tensor_copy(out=identb[:], in_=ident[:]), reads=[b_ident], writes=[b_identb])

        NGB = 6
        psum_banks = [ps("pb%d" % i, [128, 512], F32) for i in range(NGB)]
        pbuf = [k.buf("pb%d" % i) for i in range(NGB)]
        pctr = [0]

        def next_bank(lo=0, hi=NGB):
            i = lo + pctr[0] % (hi - lo)
            pctr[0] += 1
            return psum_banks[i], pbuf[i]
        ptb = [ps("ptb%d" % i, [128, 1024], BF16) for i in range(2)]
        ptbb = [k.buf("ptb%d" % i) for i in range(2)]
        ptctr = [0]

        def next_tbank():
            i = ptctr[0] % 2
            ptctr[0] += 1
            return ptb[i], ptbb[i]

        evac_ctr = [0]

        def evac_eng():
            evac_ctr[0] += 1
            return "act" if evac_ctr[0] % 2 else "dve"

        modc = sb("modc", [128, 6 * NCH], F32)
        b_modc = k.buf("modc")
        n1col = sb("n1col", [128, NCH], F32)
        n2col = sb("n2col", [128, NCH], F32)
        b_ncol = k.buf("ncol")

        def row2col(src_ap, nrows, dst_ap, name):
            with ExitStack() as sx:
                nrow = sx.enter_context(nc.sbuf_tensor("r2c_" + name, [nrows, 128], F32))
                b_nrow = k.buf()
                k.dma("sp", nrow[:], src_ap, writes=[b_nrow])
                pb, pbb = next_bank(0, 5)
                k.op("pe", lambda e: e.transpose(pb[:, 0:nrows], nrow[:], ident[0:nrows, 0:nrows]),
                     reads=[b_nrow, b_ident], writes=[pbb])
                bd = k.buf()
                k.op("dve", lambda e: e.tensor_copy(out=dst_ap, in_=pb[:, 0:nrows]), reads=[pbb], writes=[bd])
                k.barrier()
            return bd

        if ALLP or "p1" in phases or "pB" in phases:
            row2col(I("norm1_w")[:, :], NCH, n1col[:], "n1")
            row2col(I("norm2_w")[:, :], NCH, n2col[:], "n2")

        if ALLP or "p0" in phases:
            with ExitStack() as s0:
                def sb0(name, shape, dt):
                    return s0.enter_context(nc.sbuf_tensor(name, list(shape), dt))
                crow = sb0("crow", [NCH, 128], F32)
                b_crow = k.buf()
                k.dma("sp", crow[:], I("c")[:, :], writes=[b_crow])
                k.op("act", lambda e: e.activation(out=crow[:], in_=crow[:], func=AF.Silu),
                     reads=[b_crow], writes=[b_crow])
                pb, pbb = next_bank(0, 5)
                k.op("pe", lambda e: e.transpose(pb[:, 0:NCH], crow[:], ident[0:NCH, 0:NCH]),
                     reads=[b_crow, b_ident], writes=[pbb])
                ccol = sb0("ccol", [128, NCH], BF16)
                b_ccol = k.buf()
                k.op("dve", lambda e: e.tensor_copy(out=ccol[:], in_=pb[:, 0:NCH]), reads=[pbb], writes=[b_ccol])
                brow = sb0("brow", [64, 3, 128], F32)
                b_brow = k.buf()
                k.dma("sp", brow[:], I("b_ada").rearrange("(a r) p -> r a p", r=64), writes=[b_brow])
                bcol = sb0("bcol", [128, 6 * NCH], F32)
                b_bcol = k.buf()
                for a in range(3):
                    pb, pbb = next_bank(0, 5)
                    k.op("pe", lambda e, pb=pb, a=a: e.transpose(pb[:, 0:64], brow[:, a, :], ident[0:64, 0:64]),
                         reads=[b_brow, b_ident], writes=[pbb])
                    k.op("dve", lambda e, pb=pb, a=a: e.tensor_copy(out=bcol[:, a * 64:(a + 1) * 64], in_=pb[:, 0:64]),
                         reads=[pbb], writes=[b_bcol])
                WA_N = 256
                nwt = 6 * D // WA_N
                NWB = 5
                wbufs = [sb0("wada%d" % i, [128, NCH, WA_N], BF16) for i in range(NWB)]
                wb = [k.buf() for _ in range(NWB)]
                w_ada_v = I("w_ada").rearrange("(c p) n -> p c n", p=128)
                pmod, b_pmod = psum_banks[5], pbuf[5]
                for t in range(nwt):
                    wt, wtb = wbufs[t % NWB], wb[t % NWB]
                    k.dma("pool", wt[:], w_ada_v[:, :, t * WA_N:(t + 1) * WA_N], writes=[wtb])
                    for j in range(WA_N // 128):
                        col = t * (WA_N // 128) + j
                        for c in range(NCH):
                            k.op("pe", lambda e, wt=wt, j=j, c=c, col=col: e.matmul(
                                pmod[:, col:col + 1], lhsT=wt[:, c, j * 128:(j + 1) * 128], rhs=ccol[:, c:c + 1],
                                start=(c == 0), stop=(c == NCH - 1)),
                                reads=[wtb, b_ccol], writes=[b_pmod], inc=(c == NCH - 1))
                k.op("dve", lambda e: e.tensor_tensor(out=modc[:], in0=pmod[:, 0:6 * NCH], in1=bcol[:], op=ALU.add),
                     reads=[b_pmod, b_bcol], writes=[b_modc])
                if "modc_s" in debug:
                    k.dma("sp", modc_s[:, :], modc[:], reads=[b_modc], writes=[k.buf()])
                mrow = sb0("mrow", [96, 2, 128], F32)
                b_mrow = k.buf()
                for a in range(2):
                    pb, pbb = next_bank(0, 5)
                    k.op("pe", lambda e, pb=pb, a=a: e.transpose(pb[0:96, 0:128], modc[:, a * 96:(a + 1) * 96], ident[:]),
                         reads=[b_modc, b_ident], writes=[pbb])
                    k.op("dve", lambda e, pb=pb, a=a: e.tensor_copy(out=mrow[:, a, :], in_=pb[0:96, 0:128]),
                         reads=[pbb], writes=[b_mrow])
                k.dma("sp", modrow_s.rearrange("(a r) p -> r a p", a=2), mrow[:], reads=[b_mrow], writes=[k.buf()])
                k.barrier()

        if not (ALLP or "p0" in phases) and "modc_s" in feed:
            k.dma("sp", modc[:], modc_s[:, :], writes=[b_modc])

        def mm(out, lhsT, rhs, start, stop, reads, writes, inc=True):
            return k.op("pe", lambda e: e.matmul(out, lhsT=lhsT, rhs=rhs, start=start, stop=stop),
                        reads=reads, writes=writes, inc=inc)

        def actf(out, in_, func, reads, writes, **kw):
            return k.op("act", lambda e: e.activation(out=out, in_=in_, func=func, **kw), reads=reads, writes=writes)

        def cp(en, out, in_, reads, writes):
            if en == "act":
                return k.op("act", lambda e: e.copy(out=out, in_=in_), reads=reads, writes=writes)
            return k.op(en, lambda e: e.tensor_copy(out=out, in_=in_), reads=reads, writes=writes)

        def tt(en, out, in0, in1, op, reads, writes):
            return k.op(en, lambda e: e.tensor_tensor(out=out, in0=in0, in1=in1, op=op), reads=reads, writes=writes)

        def ts(en, out, in0, s1, s2, op0, op1, reads, writes):
            if s2 is None:
                return k.op(en, lambda e: e.tensor_scalar(out=out, in0=in0, scalar1=s1, scalar2=None, op0=op0),
                            reads=reads, writes=writes)
            return k.op(en, lambda e: e.tensor_scalar(out=out, in0=in0, scalar1=s1, scalar2=s2, op0=op0, op1=op1),
                        reads=reads, writes=writes)

        def stt(en, out, in0, scalar, in1, op0, op1, reads, writes):
            return k.op(en, lambda e: e.scalar_tensor_tensor(out=out, in0=in0, scalar=scalar, in1=in1, op0=op0, op1=op1),
                        reads=reads, writes=writes)

        def norm_to_hT(sx, src, ncol, m_shift, m_scale, hT, b_hT, tag):
            def sb1(name, shape, dt):
                return sx.enter_context(nc.sbuf_tensor(name + tag, list(shape), dt))
            s1col = sb1("s1col", [128, NCH], F32)
            b_s1 = k.buf()
            stt("dve", s1col[:], modc[:, m_scale * NCH:(m_scale + 1) * NCH], 1.0, ncol[:], ALU.add, ALU.mult,
                [b_modc, b_ncol], [b_s1])
            xts = [sb1("xt%d" % i, [128, D], F32) for i in range(2)]
            b_xt = [k.buf() for _ in range(2)]
            junk = sb1("junk", [128, D], BF16)
            b_junk = k.buf()
            ss = sb1("ss", [128, NT], F32)
            rstd = sb1("rstd", [128, NT], F32)
            b_ss = k.buf()
            k.op("pool", lambda e: e.memset(ss[:], 0.0), writes=[b_ss])
            for tt_ in range(NT):
                xt, bx = xts[tt_ % 2], b_xt[tt_ % 2]
                k.dma("sp", xt[:], src[tt_ * 128:(tt_ + 1) * 128, :], writes=[bx])
                actf(junk[:], xt[:], AF.Square, [bx], [b_junk, b_ss], accum_out=ss[:, tt_:tt_ + 1])
                ts("dve", rstd[:, tt_:tt_ + 1], ss[:, tt_:tt_ + 1], 1.0 / D, 1e-6, ALU.mult, ALU.add, [b_ss], [b_ss])
                k.op("act", lambda e, tt_=tt_: e.sqrt(out=rstd[:, tt_:tt_ + 1], in_=rstd[:, tt_:tt_ + 1]),
                     reads=[b_ss], writes=[b_ss])
                k.op("dve", lambda e, tt_=tt_: e.reciprocal(out=rstd[:, tt_:tt_ + 1], in_=rstd[:, tt_:tt_ + 1]),
                     reads=[b_ss], writes=[b_ss])
                actf(xt[:], xt[:], AF.Copy, [bx, b_ss], [bx], scale=rstd[:, tt_:tt_ + 1])
                for g4 in range(NCH // 4):
                    pb, pbb = next_bank()
                    for j in range(4):
                        c = g4 * 4 + j
                        k.op("pe", lambda e, pb=pb, j=j, c=c, xt=xt: e.transpose(
                            pb[:, j * 128:(j + 1) * 128], xt[:, c * 128:(c + 1) * 128], ident[:]),
                            reads=[bx, b_ident], writes=[pbb], inc=(j == 3))
                    for j in range(4):
                        c = g4 * 4 + j
                        o_ap = hT[:, c, tt_ * 128:(tt_ + 1) * 128]
                        i_ap = pb[:, j * 128:(j + 1) * 128]
                        if evac_eng() == "act":
                            actf(o_ap, i_ap, AF.Identity, [pbb, b_s1, b_modc], [b_hT[tt_]],
                                 scale=s1col[:, c:c + 1], bias=modc[:, m_shift * NCH + c:m_shift * NCH + c + 1])
                        else:
                            ts("dve", o_ap, i_ap, s1col[:, c:c + 1], modc[:, m_shift * NCH + c:m_shift * NCH + c + 1],
                               ALU.mult, ALU.add, [pbb, b_s1, b_modc], [b_hT[tt_]])

        DIL = (1, 4, 16)

        if ALLP or "pA" in phases:
            with ExitStack() as sA:
                def sbA(name, shape, dt):
                    return sA.enter_context(nc.sbuf_tensor(name, list(shape), dt))
                hT = sbA("hT", [128, NCH, S], BF16)
                b_hT = [k.buf("hT%d" % i) for i in range(NT)]
                with ExitStack() as s1:
                    norm_to_hT(s1, I("x"), n1col, 0, 1, hT, b_hT, "a")
                    k.barrier()
                if "hT_s" in debug:
                    k.dma("sp", hT_s.rearrange("(c p) t -> p c t", p=128), hT[:], reads=b_hT, writes=[k.buf()])

                WN = 128
                wts = [sbA("win%d" % i, [128, NCH, WN], BF16) for i in range(2)]
                b_wt = [k.buf() for _ in range(2)]
                wctr = [0]
                w_in_v = I("w_in").rearrange("(c p) n -> p c n", p=128)

                def load_w(col0, n):
                    i = wctr[0] % 2
                    wctr[0] += 1
                    k.dma("pool", wts[i][:, :, 0:n], w_in_v[:, :, col0:col0 + n], writes=[b_wt[i]])
                    return wts[i], b_wt[i]

                def gemm_fm(wt, bw, n0=0):
                    res = []
                    for tb in range(4):
                        pb, pbb = next_bank()
                        for c in range(NCH):
                            mm(pb[:, :], wt[:, c, n0:n0 + 128], hT[:, c, tb * 512:(tb + 1) * 512], c == 0, c == NCH - 1,
                               [bw] + b_hT[4 * tb:4 * tb + 4], [pbb], inc=(c == NCH - 1))
                        res.append((pb, pbb))
                    return res

                def gemm_tm(wt, bw, n, tt_):
                    pb, pbb = next_bank()
                    for c in range(NCH):
                        mm(pb[:, 0:n], hT[:, c, tt_ * 128:(tt_ + 1) * 128], wt[:, c, 0:n], c == 0, c == NCH - 1,
                           [bw, b_hT[tt_]], [pbb], inc=(c == NCH - 1))
                    return pb, pbb

                bufA = sbA("bufA", [128, S + 4], F32)
                bufB = sbA("bufB", [128, S], F32)
                bufCs = [sbA("bufC%d" % i, [128, S], BF16) for i in range(2)]
                bufDs = [sbA("bufD%d" % i, [128, NT, 128], BF16) for i in range(2)]
                bCs = [k.buf(), k.buf()]
                bDs = [k.buf(), k.buf()]
                cctr = [0]

                def rotC():
                    i = cctr[0] % 2
                    cctr[0] += 1
                    return bufCs[i], bCs[i], bufDs[i], bDs[i]
                bufC, bufD = bufCs[0], bufDs[0]
                dtst = sbA("dtst", [128, NT, 64], F32)
                bA, bB, bC, bD, bTM, bDT = (k.buf() for _ in range(6))
                k.op("pool", lambda e: e.memset(bufA[:], 0.0), writes=[bA])

                def fm_job(col0, nblk, dst, func):
                    nonlocal bufC, bC, bufD, bD
                    for blk in range(nblk):
                        bufC, bC, bufD, bD = rotC()
                        wt, bw = load_w(col0 + blk * 128, 128)
                        res = gemm_fm(wt, bw)
                        for tb, (pb, pbb) in enumerate(res):
                            actf(bufC[:, tb * 512:(tb + 1) * 512], pb[:, :], func, [pbb], [bC])
                        k.dma("sp", dst[blk * 128:(blk + 1) * 128, :], bufC[:], reads=[bC], writes=[k.buf()])

                if ALLP or "A_z" in phases:
                    fm_job(OFF_Z, 16, zT_s, AF.Silu)
                if ALLP or "A_dt" in phases:
                    wt, bw = load_w(OFF_DT, 64)
                    for tt_ in range(NT):
                        pb, pbb = gemm_tm(wt, bw, 64, tt_)
                        cp("dve", dtst[:, tt_, :], pb[:, 0:64], [pbb], [bDT])
                    k.dma("sp", dt_s.rearrange("(tt p) n -> p tt n", p=128), dtst[:], reads=[bDT], writes=[k.buf()])
                def fm_to_tm(dstv):
                    for half in range(2):
                        pt, ptbf = next_tbank()
                        for j in range(8):
                            tt_ = half * 8 + j
                            k.op("pe", lambda e, pt=pt, j=j, tt_=tt_: e.transpose(
                                pt[:, j * 128:(j + 1) * 128], bufC[:, tt_ * 128:(tt_ + 1) * 128], identb[:]),
                                reads=[bC, b_identb], writes=[ptbf], inc=(j == 7))
                        cp(evac_eng(), bufD[:, half * 8:(half + 1) * 8, :],
                           pt[:, :].rearrange("p (a b) -> p a b", b=128), [ptbf], [bD])
                    k.dma("sp", dstv, bufD[:], reads=[bD], writes=[k.buf()])

                if ALLP or "A_v" in phases:
                    for blk in range(24):
                        bufC, bC, bufD, bD = rotC()
                        wt, bw = load_w(OFF_V + blk * 128, 128)
                        res = gemm_fm(wt, bw)
                        for tb, (pb, pbb) in enumerate(res):
                            cp(evac_eng(), bufC[:, tb * 512:(tb + 1) * 512], pb[:, :], [pbb], [bC])
                        fm_to_tm(v_s.rearrange("(tt p) n -> p tt n", p=128)[:, :, blk * 128:(blk + 1) * 128])
                if ALLP or "A_g" in phases:
                    fm_job(OFF_G, 64, sgT_s, AF.Sigmoid)
                if ALLP or "A_x" in phases:
                    cwc = sbA("cwc", [128, 6, NCH], F32)
                    for kk in range(5):
                        row2col(I("conv_w")[kk, :, :], NCH, cwc[:, kk, :], "cw%d" % kk)
                    bcw = row2col(I("conv_b")[:, :], NCH, cwc[:, 5, :], "cb")
                    for blk in range(32):
                        bufC, bC, bufD, bD = rotC()
                        wt, bw = load_w(OFF_XBC + blk * 128, 128)
                        res = gemm_fm(wt, bw)
                        for tb, (pb, pbb) in enumerate(res):
                            cp(evac_eng(), bufA[:, 2 + tb * 512:2 + (tb + 1) * 512], pb[:, :], [pbb], [bA])
                        ts("dve", bufB[:], bufA[:, 0:S], cwc[:, 0, blk:blk + 1], cwc[:, 5, blk:blk + 1], ALU.mult, ALU.add,
                           [bA, bcw], [bB])
                        for kk in range(1, 5):
                            stt("dve", bufB[:], bufA[:, kk:kk + S], cwc[:, kk, blk:blk + 1], bufB[:], ALU.mult, ALU.add,
                                [bA, bB, bcw], [bB])
                        actf(bufC[:], bufB[:], AF.Silu, [bB], [bC])
                        if blk >= 24:
                            k.dma("sp", CT_s[(blk - 24) * 128:(blk - 23) * 128, :], bufC[:], reads=[bC], writes=[k.buf()])
                            continue
                        if blk >= 16:
                            k.dma("sp", BT_s[(blk - 16) * 128:(blk - 15) * 128, :], bufC[:], reads=[bC], writes=[k.buf()])
                            continue
                        k.dma("sp", xsT_s[blk * 128:(blk + 1) * 128, :], bufC[:], reads=[bC], writes=[k.buf()])
                        for half in range(2):
                            pt, ptbf = next_tbank()
                            for j in range(8):
                                tt_ = half * 8 + j
                                k.op("pe", lambda e, pt=pt, j=j, tt_=tt_: e.transpose(
                                    pt[:, j * 128:(j + 1) * 128], bufC[:, tt_ * 128:(tt_ + 1) * 128], identb[:]),
                                    reads=[bC, b_identb], writes=[ptbf], inc=(j == 7))
                            cp(evac_eng(), bufD[:, half * 8:(half + 1) * 8, :],
                               pt[:, :].rearrange("p (a b) -> p a b", b=128), [ptbf], [bD])
                        dstv = xs_s.rearrange("(tt p) n -> p tt n", p=128)[:, :, blk * 128:(blk + 1) * 128]
                        k.dma("sp", dstv, bufD[:], reads=[bD], writes=[k.buf()])
                if ALLP or "A_qk" in phases:
                    cost = sbA("cost", [128, S], F32)
                    sint = sbA("sint", [128, S], F32)
                    perm = sbA("perm", [128, 128], F32)
                    colf = sbA("colf", [128, 4], F32)
                    b_tab, b_perm, b_colf = k.buf(), k.buf(), k.buf()
                    k.op("pool", lambda e: e.memset(perm[:], 0.0), writes=[b_perm])
                    for bs in (-64, 64):
                        k.op("pool", lambda e, bs=bs: e.affine_select(out=perm[:], in_=perm[:], pattern=[[-1, 128]],
                                                                      compare_op=ALU.not_equal, fill=1.0, base=bs,
                                                                      channel_multiplier=1), reads=[b_perm], writes=[b_perm])
                    k.op("pool", lambda e: e.iota(colf[:, 0:1], pattern=[[0, 1]], base=0, channel_multiplier=1,
                                                  allow_small_or_imprecise_dtypes=True), writes=[b_colf])
                    ts("dve", colf[:, 3:4], colf[:, 0:1], 64.0, None, ALU.is_ge, None, [b_colf], [b_colf])
                    stt("dve", colf[:, 1:2], colf[:, 3:4], -64.0, colf[:, 0:1], ALU.mult, ALU.add, [b_colf], [b_colf])
                    actf(colf[:, 2:3], colf[:, 1:2], AF.Exp, [b_colf], [b_colf], scale=-math.log(10000.0) / 64.0)
                    ts("dve", colf[:, 3:4], colf[:, 3:4], 2.0, -1.0, ALU.mult, ALU.add, [b_colf], [b_colf])
                    k.op("pool", lambda e: e.iota(bufB[:], pattern=[[1, S]], base=0, channel_multiplier=0,
                                                  allow_small_or_imprecise_dtypes=True), writes=[bB])
                    ts("dve", bufB[:], bufB[:], colf[:, 2:3], None, ALU.mult, None, [bB, b_colf], [bB])
                    TWO_PI = 2.0 * math.pi
                    MAGIC = 12582912.0

                    def sin_table(dst, shift):
                        ts("dve", dst[:], bufB[:], shift, 1.0 / TWO_PI, ALU.add, ALU.mult, [bB], [b_tab])
                        ts("dve", dst[:], dst[:], MAGIC, None, ALU.add, None, [b_tab], [b_tab])
                        ts("dve", dst[:], dst[:], -MAGIC, -TWO_PI, ALU.add, ALU.mult, [b_tab], [b_tab])
                        stt("dve", dst[:], bufB[:], shift, dst[:], ALU.add, ALU.add, [bB, b_tab], [b_tab])
                        ts("dve", dst[:], dst[:], -3.1415925, 3.1415925, ALU.max, ALU.min, [b_tab], [b_tab])
                        actf(dst[:], dst[:], AF.Sin, [b_tab], [b_tab])
                    sin_table(sint, 0.0)
                    ts("dve", sint[:], sint[:], colf[:, 3:4], None, ALU.mult, None, [b_tab, b_colf], [b_tab])
                    sin_table(cost, math.pi / 2.0)
                    tmpf = bufA
                    k.barrier()
                    bBq = [k.buf() for _ in range(4)]
                    bAq = [k.buf() for _ in range(4)]
                    for which, (off, dstT) in enumerate(((OFF_Q, qT_s), (OFF_K, kT_s))):
                        nheads = 24 if (ALLP or "A_qk_full" in phases) else 2
                        for hd in range(nheads):
                            dd = DIL[hd // 8]
                            bufC, bC, bufD, bD = rotC()
                            wt, bw = load_w(off + hd * 128, 128)
                            res = gemm_fm(wt, bw)
                            for tb, (pb, pbb) in enumerate(res):
                                sl = slice(tb * 512, (tb + 1) * 512)
                                cp(evac_eng(), bufB[:, sl], pb[:, :], [pbb], [bBq[tb]])
                            for tb in range(4):
                                sl = slice(tb * 512, (tb + 1) * 512)
                                pb, pbb = next_bank()
                                mm(pb[:, :], perm[:], bufB[:, sl], True, True, [bBq[tb], b_perm], [pbb])
                                tt("dve", tmpf[:, sl], pb[:, :], sint[:, sl], ALU.mult, [pbb, b_tab], [bAq[tb]])
                                tt("pool", bufB[:, sl], bufB[:, sl], cost[:, sl], ALU.mult, [bBq[tb], b_tab, pbb], [bBq[tb]])
                                tt("dve", bufB[:, sl], bufB[:, sl], tmpf[:, sl], ALU.add, [bBq[tb], bAq[tb]], [bBq[tb]])
                                if dd == 1:
                                    cp("act", bufC[:, sl], bufB[:, sl], [bBq[tb]], [bC])
                                else:
                                    n_i = 512 // dd
                                    o_ap = bufC[:, :].rearrange("e (r i) -> e r i", r=dd)[:, :, tb * n_i:(tb + 1) * n_i]
                                    i_ap = bufB[:, sl].rearrange("e (i r) -> e r i", r=dd)
                                    cp("act", o_ap, i_ap, [bBq[tb]], [bC])
                            k.dma("sp", dstT[hd * 128:(hd + 1) * 128, :], bufC[:], reads=[bC], writes=[k.buf()])
                k.barrier()
        if ALLP or "pB" in phases:
            with ExitStack() as sB:
                def sbB(name, shape, dt):
                    return sB.enter_context(nc.sbuf_tensor(name, list(shape), dt))
                triI = sbB("triI", [128, 128], F32)
                triE = sbB("triE", [128, 128], F32)
                onesf = sbB("onesf", [128, 128], F32)
                b_tri = k.buf()
                k.op("pool", lambda e: e.memset(onesf[:], 1.0), writes=[b_tri])
                k.op("pool", lambda e: e.memset(triI[:], 1.0), writes=[b_tri])
                k.op("pool", lambda e: e.affine_select(out=triI[:], in_=triI[:], pattern=[[1, 128]], compare_op=ALU.is_ge,
                                                       fill=0.0, base=0, channel_multiplier=-1), reads=[b_tri], writes=[b_tri])
                k.op("pool", lambda e: e.memset(triE[:], 1.0), writes=[b_tri])
                k.op("pool", lambda e: e.affine_select(out=triE[:], in_=triE[:], pattern=[[1, 128]], compare_op=ALU.is_ge,
                                                       fill=0.0, base=-1, channel_multiplier=-1), reads=[b_tri], writes=[b_tri])
                dtx = sbB("dtx", [128, NT, 64], F32)
                dtv = sbB("dtv", [128, NT, 64], F32)
                da = sbB("da", [128, NT, 64], F32)
                nb_ = sbB("nbias", [128, NT, 64], F32)
                brep = sbB("brep", [128, 64], F32)
                arep = sbB("arep", [128, 64], F32)
                dsk = sbB("dsk", [128, 32], F32)
                b_dtx, b_dtv, b_da, b_nb, b_brep, b_arep, b_dsk = (k.buf() for _ in range(7))
                k.dma("sp", dtx[:], dt_s.rearrange("(j p) n -> p j n", p=128), writes=[b_dtx])
                k.dma("sp", brep[:, 0:32], I("dt_bias_f").partition_broadcast(128), writes=[b_brep])
                k.dma("sp", brep[:, 32:64], I("dt_bias_b").partition_broadcast(128), writes=[b_brep])
                k.dma("sp", arep[:, 0:32], I("a_log_f").partition_broadcast(128), writes=[b_arep])
                k.dma("sp", arep[:, 32:64], I("a_log_b").partition_broadcast(128), writes=[b_arep])
                k.dma("sp", dsk[:], I("d_skip").partition_broadcast(128), writes=[b_dsk])
                actf(arep[:], arep[:], AF.Exp, [b_arep], [b_arep])
                ts("dve", arep[:], arep[:], -1.0, None, ALU.mult, None, [b_arep], [b_arep])
                for j in range(NT):
                    tt("dve", dtx[:, j, :], dtx[:, j, :], brep[:], ALU.add, [b_dtx, b_brep], [b_dtx])
                dtxf = dtx[:, :, :].rearrange("p a b -> p (a b)")
                dtvf = dtv[:, :, :].rearrange("p a b -> p (a b)")
                stt("dve", dtvf, dtxf, -1.0, dtxf, ALU.mult, ALU.max, [b_dtx], [b_dtv])
                actf(dtvf, dtvf, AF.Exp, [b_dtv], [b_dtv], scale=-1.0)
                ts("dve", dtvf, dtvf, 1.0, None, ALU.add, None, [b_dtv], [b_dtv])
                actf(dtvf, dtvf, AF.Ln, [b_dtv], [b_dtv])
                stt("dve", dtvf, dtxf, 0.0, dtvf, ALU.max, ALU.add, [b_dtx, b_dtv], [b_dtv])
                for j in range(NT):
                    tt("dve", da[:, j, :], dtv[:, j, :], arep[:], ALU.mult, [b_dtv, b_arep], [b_da])
                for half, tri in ((0, triI), (1, triE)):
                    for i in range(NT):
                        pbk, pbkb = psum_banks[i // 8], pbuf[i // 8]
                        o0 = (i % 8) * 64 + half * 32
                        for j in range(i + 1):
                            mm(pbk[:, o0:o0 + 32], (tri if j == i else onesf)[:], da[:, j, half * 32:half * 32 + 32],
                               j == 0, j == i, [b_da, b_tri], [pbkb], inc=(half == 1 and i % 8 == 7 and j == i))
                for b2 in range(2):
                    pv = psum_banks[b2][:, :].rearrange("p (a b) -> p a b", b=64)
                    ts("dve", nb_[:, b2 * 8:(b2 + 1) * 8, 0:32], pv[:, :, 0:32], -1.0, None, ALU.mult, None, [pbuf[b2]], [b_nb])
                    cp("act", nb_[:, b2 * 8:(b2 + 1) * 8, 32:64], pv[:, :, 32:64], [pbuf[b2]], [b_nb])
                lndt = sbB("lndt", [128, NT, 64], F32)
                b_lndt = k.buf()
                actf(lndt[:, :, :].rearrange("p a b -> p (a b)"), dtvf, AF.Ln, [b_dtv], [b_lndt])
                nbf = nb_[:, :, :].rearrange("p a b -> p (a b)")
                tt("dve", nbf, nbf, lndt[:, :, :].rearrange("p a b -> p (a b)"), ALU.add, [b_nb, b_lndt], [b_nb])
                csr = sbB("csr", [32, S], F32)
                b_csr = k.buf()
                for half, tri in ((0, triI), (1, triE)):
                    for i in range(NT):
                        bi = 2 + i // 4
                        pbk, pbkb = psum_banks[bi], pbuf[bi]
                        o0 = (i % 4) * 128
                        for j in range(i + 1):
                            mm(pbk[0:32, o0:o0 + 128], da[:, j, half * 32:half * 32 + 32], (tri if j == i else onesf)[:],
                               j == 0, j == i, [b_da, b_tri], [pbkb], inc=(i % 4 == 3 and j == i))
                    for q4 in range(4):
                        cp(evac_eng(), csr[:, q4 * 512:(q4 + 1) * 512], psum_banks[2 + q4][0:32, :], [pbuf[2 + q4]], [b_csr])
                    k.dma("sp", csT_s[half * 32:(half + 1) * 32, :], csr[:], reads=[b_csr], writes=[k.buf()])
                k.barrier()
                if "nb_s" in debug:
                    nb_s = scr("nb_s", [128, NT * 64], F32)
                    dtv_s = scr("dtv_s", [128, NT * 64], F32)
                    k.dma("sp", nb_s[:, :], nb_[:, :, :].rearrange("p a b -> p (a b)"), reads=[b_nb], writes=[k.buf()])
                    k.dma("sp", dtv_s[:, :], dtvf, reads=[b_dtv], writes=[k.buf()])

                GT = sbB("GT", [128, NT, S], BF16)
                BTt = sbB("BTt", [128, S], BF16)
                CTt = sbB("CTt", [128, S], BF16)
                csreps = [[sbB("csrep%d_%d" % (a, i), [128, S], F32) for i in range(2)] for a in range(2)]
                Xhs = [sbB("Xh%d" % a, [128, NT, 64], BF16) for a in range(2)]
                xsThs = [sbB("xsTh%d" % a, [64, S], BF16) for a in range(2)]
                zThs = [sbB("zTh%d" % a, [64, S], BF16) for a in range(2)]
                Eb = [sbB("Eb%d" % i, [128, 512], BF16) for i in range(3)]
                Mb = [sbB("Mb%d" % i, [128, 512], BF16) for i in range(3)]
                ytmp = sbB("ytmp", [64, 512], F32)
                yout = sbB("yout", [64, S], BF16)
                b_GT = [k.buf() for _ in range(NT)]
                b_BT, b_CT, b_ytmp, b_yout = (k.buf() for _ in range(4))
                b_Xs, b_xsTs, b_zTs = ([k.buf(), k.buf()] for _ in range(3))
                b_csreps = [[k.buf(), k.buf()], [k.buf(), k.buf()]]
                b_E = [k.buf() for _ in range(3)]
                b_M = [k.buf() for _ in range(3)]
                sctr = 0
                ngroups = 8 if (ALLP or "B_full" in phases) else 1
                for g in range(ngroups):
                    k.dma("sp", BTt[:], BT_s[g * 128:(g + 1) * 128, :], writes=[b_BT])
                    k.dma("sp", CTt[:], CT_s[g * 128:(g + 1) * 128, :], writes=[b_CT])
                    for j in range(NT):
                        for tb in range(4):
                            pb, pbb = next_bank(0, 4)
                            mm(pb[:, :], BTt[:, j * 128:(j + 1) * 128], CTt[:, tb * 512:(tb + 1) * 512], True, True,
                               [b_BT, b_CT], [pbb])
                            cp(evac_eng(), GT[:, j, tb * 512:(tb + 1) * 512], pb[:, :], [pbb], [b_GT[j]])
                    for kh in range(4):
                        h = g * 4 + kh
                        csrep, b_csrep = csreps[h % 2], b_csreps[h % 2]
                        Xh, b_X = Xhs[h % 2], b_Xs[h % 2]
                        xsTh, b_xsT = xsThs[h % 2], b_xsTs[h % 2]
                        zTh, b_zT = zThs[h % 2], b_zTs[h % 2]
                        k.dma("sp", csrep[0][:], csT_s[h:h + 1, :].partition_broadcast(128), writes=[b_csrep[0]])
                        k.dma("sp", csrep[1][:], csT_s[32 + h:33 + h, :].partition_broadcast(128), writes=[b_csrep[1]])
                        k.dma("sp", Xh[:], xs_s.rearrange("(j p) n -> p j n", p=128)[:, :, h * 64:(h + 1) * 64], writes=[b_X])
                        k.dma("sp", xsTh[:], xsT_s[h * 64:(h + 1) * 64, :], writes=[b_xsT])
                        k.dma("sp", zTh[:], zT_s[h * 64:(h + 1) * 64, :], writes=[b_zT])
                        for tb in range(4):
                            py, pyb = psum_banks[4 + tb % 2], pbuf[4 + tb % 2]
                            segs = []
                            for j in range(NT):
                                if j < 4 * tb:
                                    segs.append((j, 0, tb * 512, (tb + 1) * 512, False))
                                elif j > 4 * tb + 3:
                                    segs.append((j, 1, tb * 512, (tb + 1) * 512, False))
                                else:
                                    segs.append((j, 0, j * 128, (tb + 1) * 512, True))
                                    segs.append((j, 1, tb * 512, (j + 1) * 128, True))
                            for si, (j, dr, t0, t1, diag) in enumerate(segs):
                                w = t1 - t0
                                E_, bE = Eb[sctr % 3], b_E[sctr % 3]
                                M_, bM = Mb[sctr % 3], b_M[sctr % 3]
                                sctr += 1
                                actf(E_[:, 0:w], csrep[dr][:, t0:t1], AF.Exp, [b_csrep[dr], b_nb], [bE],
                                     bias=nb_[:, j, dr * 32 + h:dr * 32 + h + 1], scale=(1.0 if dr == 0 else -1.0))
                                if diag:
                                    if dr == 0:
                                        k.op("pool", lambda e, E_=E_: e.affine_select(
                                            out=E_[:, 0:128], in_=E_[:, 0:128], pattern=[[1, 128]], compare_op=ALU.is_ge,
                                            fill=0.0, base=0, channel_multiplier=-1), reads=[bE], writes=[bE])
                                    else:
                                        k.op("pool", lambda e, E_=E_, w=w: e.affine_select(
                                            out=E_[:, w - 128:w], in_=E_[:, w - 128:w], pattern=[[-1, 128]], compare_op=ALU.is_ge,
                                            fill=0.0, base=0, channel_multiplier=1), reads=[bE], writes=[bE])
                                tt("pool" if (sctr % 3 == 0 and not diag) else "dve", M_[:, 0:w], E_[:, 0:w], GT[:, j, t0:t1],
                                   ALU.mult, [bE, b_GT[j]], [bM])
                                mm(py[0:64, t0 - tb * 512:t1 - tb * 512], Xh[:, j, :], M_[:, 0:w], si == 0, si == len(segs) - 1,
                                   [b_X, bM], [pyb], inc=True)
                            sl = slice(tb * 512, (tb + 1) * 512)
                            stt("dve", ytmp[:, :], xsTh[:, sl], dsk[0:64, h:h + 1], py[0:64, :], ALU.mult, ALU.add,
                                [b_xsT, b_dsk, pyb], [b_ytmp])
                            tt("pool", yout[:, sl], ytmp[:, :], zTh[:, sl], ALU.mult, [b_ytmp, b_zT], [b_yout])
                        k.dma("pool", ygT_s[h * 64:(h + 1) * 64, :], yout[:], reads=[b_yout], writes=[k.buf()])
                k.barrier()
        if ALLP or "pC" in phases:
            with ExitStack() as sC:
                def sbC(name, shape, dt):
                    return sC.enter_context(nc.sbuf_tensor(name, list(shape), dt))
                maskB = sbC("maskB", [128, 384], BF16)
                onesb = sbC("onesb128", [128, 128], BF16)
                b_mask = k.buf()
                k.op("pool", lambda e: e.memset(onesb[:], 1.0), writes=[b_mask])
                k.op("pool", lambda e: e.memset(maskB[:], 1.0), writes=[b_mask])
                k.op("pool", lambda e: e.affine_select(out=maskB[:], in_=maskB[:], pattern=[[1, 384]], compare_op=ALU.is_ge,
                                                       fill=0.0, base=-64, channel_multiplier=-1), reads=[b_mask], writes=[b_mask])
                k.op("pool", lambda e: e.affine_select(out=maskB[:], in_=maskB[:], pattern=[[-1, 384]], compare_op=ALU.is_ge,
                                                       fill=0.0, base=192, channel_multiplier=1), reads=[b_mask], writes=[b_mask])
                Oacc = sbC("Oacc", [128, S], F32)
                Zacc = sbC("Zacc", [128, S], F32)
                qTh = sbC("qTh", [128, S], BF16)
                kTh = sbC("kTh", [128, S], BF16)
                vt = sbC("vt", [128, NT, 128], BF16)
                PBt = [sbC("PBt%d" % i, [128, 8, 384], BF16) for i in range(2)]
                oTo = sbC("oTo", [128, S], BF16)
                b_O, b_Z, b_q, b_k, b_v, b_oT = (k.buf() for _ in range(6))
                b_PB = [[k.buf() for _ in range(8)] for _ in range(2)]
                pctr2 = 0
                sm_scale = 1.0 / math.sqrt(128.0)
                nslots = 8 if (ALLP or "C_full" in phases) else 1
                for hs in range(nslots):
                    for g in range(3):
                        hd = g * 8 + hs
                        d = DIL[g]
                        L = S // d
                        nt = L // 128
                        k.dma("sp", qTh[:], qT_s[hd * 128:(hd + 1) * 128, :], writes=[b_q])
                        k.dma("sp", kTh[:], kT_s[hd * 128:(hd + 1) * 128, :], writes=[b_k])
                        vview = v_s.rearrange("(j kk r) n -> r kk j n", kk=128, r=d)
                        for r in range(d):
                            k.dma("act" if r % 2 else "sp", vt[:, r * nt:(r + 1) * nt, :], vview[r][:, :, hd * 128:(hd + 1) * 128],
                                  writes=[b_v])
                        for c0 in range(0, S, 512):
                            pO, pOb = psum_banks[4], pbuf[4]
                            pZ, pZb = psum_banks[5], pbuf[5]
                            tiles = []
                            for qi in range(4):
                                col = c0 + qi * 128
                                tiles.append((qi, col // L, (col % L) // 128))
                            PB_, bPB = PBt[pctr2 % 2], b_PB[pctr2 % 2]
                            pctr2 += 1
                            slot = 0
                            for r in sorted(set(t_[1] for t_ in tiles)):
                                tl = [t_ for t_ in tiles if t_[1] == r]
                                ilo, ihi = tl[0][2], tl[-1][2]
                                qi0 = tl[0][0]
                                jlo, jhi = max(ilo - 1, 0), min(ihi + 1, nt - 1)
                                info = {}
                                for j in range(jlo, jhi + 1):
                                    a_ = max(j - 1, ilo)
                                    b_ = min(j + 1, ihi)
                                    w = (b_ - a_ + 1) * 128
                                    ps_, psb = next_bank(0, 4)
                                    qc0 = r * L + a_ * 128
                                    mm(ps_[:, 0:w], kTh[:, r * L + j * 128:r * L + (j + 1) * 128], qTh[:, qc0:qc0 + w], True, True,
                                       [b_q, b_k], [psb])
                                    actf(PB_[:, slot, 0:w], ps_[:, 0:w], AF.Exp, [psb], [bPB[slot]], scale=sm_scale)
                                    off = 128 + (a_ - j) * 128
                                    tt("dve", PB_[:, slot, 0:w], PB_[:, slot, 0:w], maskB[:, off:off + w], ALU.mult, [bPB[slot], b_mask], [bPB[slot]])
                                    info[j] = (slot, a_)
                                    slot += 1
                                for i in range(ilo, ihi + 1):
                                    qi = qi0 + (i - ilo)
                                    cj = list(range(max(i - 1, 0), min(i + 1, nt - 1) + 1))
                                    for n, j in enumerate(cj):
                                        sl_, a_ = info[j]
                                        pc = (i - a_) * 128
                                        mm(pO[:, qi * 128:(qi + 1) * 128], vt[:, r * nt + j, :], PB_[:, sl_, pc:pc + 128], n == 0, n == len(cj) - 1,
                                           [b_v, bPB[sl_]], [pOb], inc=False)
                                    for n, j in enumerate(cj):
                                        sl_, a_ = info[j]
                                        pc = (i - a_) * 128
                                        mm(pZ[:, qi * 128:(qi + 1) * 128], onesb[:], PB_[:, sl_, pc:pc + 128], n == 0, n == len(cj) - 1,
                                           [b_mask, bPB[sl_]], [pZb], inc=(n == len(cj) - 1))
                            if d == 1:
                                ovO, ovZ = Oacc[:, c0:c0 + 512], Zacc[:, c0:c0 + 512]
                                pvO, pvZ = pO[:, :], pZ[:, :]
                            elif d == 4:
                                r = c0 // 512
                                ovO = Oacc[:, :].rearrange("e (i r) -> e r i", r=4)[:, r, :]
                                ovZ = Zacc[:, :].rearrange("e (i r) -> e r i", r=4)[:, r, :]
                                pvO, pvZ = pO[:, :], pZ[:, :]
                            else:
                                r0 = c0 // 128
                                ovO = Oacc[:, :].rearrange("e (i r) -> e r i", r=16)[:, r0:r0 + 4, :]
                                ovZ = Zacc[:, :].rearrange("e (i r) -> e r i", r=16)[:, r0:r0 + 4, :]
                                pvO = pO[:, :].rearrange("e (r i) -> e r i", r=4)
                                pvZ = pZ[:, :].rearrange("e (r i) -> e r i", r=4)
                            if g == 0:
                                cp("act", ovO, pvO, [pOb], [b_O])
                                cp("dve", ovZ, pvZ, [pZb], [b_Z])
                            else:
                                tt("dve", ovO, ovO, pvO, ALU.add, [pOb, b_O], [b_O])
                                tt("dve", ovZ, ovZ, pvZ, ALU.add, [pZb, b_Z], [b_Z])
                    k.op("dve", lambda e: e.reciprocal(out=Zacc[:], in_=Zacc[:]), reads=[b_Z], writes=[b_Z])
                    tt("dve", oTo[:], Oacc[:], Zacc[:], ALU.mult, [b_O, b_Z], [b_oT])
                    k.dma("sp", oT_s[hs * 128:(hs + 1) * 128, :], oTo[:], reads=[b_oT], writes=[k.buf()])
                k.barrier()
        if ALLP or "pD" in phases:
            with ExitStack() as sD:
                def sbD(name, shape, dt):
                    return sD.enter_context(nc.sbuf_tensor(name, list(shape), dt))
                yg = sbD("yg", [128, 16, S], BF16)
                oTt = sbD("oTt", [128, 8, S], BF16)
                b_yg, b_oTt = k.buf(), k.buf()
                k.dma("sp", yg[:], ygT_s.rearrange("(c p) t -> p c t", p=128), writes=[b_yg])
                k.dma("act", oTt[:], oT_s.rearrange("(c p) t -> p c t", p=128), writes=[b_oTt])
                wncol = sbD("wncol", [128, 16], F32)
                b_wn = row2col(I("ssm_norm_w")[:, :], 16, wncol[:], "wn")
                onesb = sbD("onesbD", [128, 128], BF16)
                b_ones = k.buf()
                k.op("pool", lambda e: e.memset(onesb[:], 1.0), writes=[b_ones])
                sq = [sbD("sq%d" % i, [128, S], BF16) for i in range(2)]
                b_sq = [k.buf(), k.buf()]
                rrep = sbD("rrep", [128, S], F32)
                b_rrep = k.buf()
                for c in range(16):
                    tt("pool" if c % 2 else "dve", sq[c % 2][:], yg[:, c, :], yg[:, c, :], ALU.mult, [b_yg], [b_sq[c % 2]])
                    for tb in range(4):
                        mm(psum_banks[tb][:, :], onesb[:], sq[c % 2][:, tb * 512:(tb + 1) * 512], c == 0, c == 15,
                           [b_ones, b_sq[c % 2]], [pbuf[tb]], inc=True)
                for tb in range(4):
                    ts("dve", rrep[:, tb * 512:(tb + 1) * 512], psum_banks[tb][:, :], 1.0 / 2048.0, 1e-6, ALU.mult, ALU.add,
                       [pbuf[tb]], [b_rrep])
                k.op("act", lambda e: e.sqrt(out=rrep[:], in_=rrep[:]), reads=[b_rrep], writes=[b_rrep])
                k.op("dve", lambda e: e.reciprocal(out=rrep[:], in_=rrep[:]), reads=[b_rrep], writes=[b_rrep])
                for c in range(16):
                    stt("dve", yg[:, c, :], yg[:, c, :], wncol[:, c:c + 1], rrep[:], ALU.mult, ALU.mult,
                        [b_yg, b_wn, b_rrep], [b_yg])
                if "ynT_s" in debug:
                    ynT_s = scr("ynT_s", [2048, S], BF16)
                    k.dma("sp", ynT_s.rearrange("(c p) t -> p c t", p=128), yg[:], reads=[b_yg], writes=[k.buf()])
                wso = [sbD("wso%d" % i, [128, 16, 128], BF16) for i in range(2)]
                wao = [sbD("wao%d" % i, [128, 8, 128], BF16) for i in range(2)]
                sg1 = [sbD("sg1%d" % i, [128, S], BF16) for i in range(2)]
                sg2 = [sbD("sg2%d" % i, [128, S], BF16) for i in range(2)]
                b_wso, b_wao, b_sg1, b_sg2 = ([k.buf(), k.buf()] for _ in range(4))
                t1 = [sbD("t1%d" % i, [128, 512], F32) for i in range(2)]
                t2 = [sbD("t2%d" % i, [128, 512], F32) for i in range(2)]
                b_t1, b_t2 = [k.buf(), k.buf()], [k.buf(), k.buf()]
                mrg = [sbD("mrg%d" % i, [128, S], BF16) for i in range(2)]
                b_mrg = [k.buf(), k.buf()]
                wso_v = I("w_ssm_out").rearrange("(c p) n -> p c n", p=128)
                wao_v = I("w_attn_out").rearrange("(c p) n -> p c n", p=128)
                ndc = NCH if (ALLP or "D_full" in phases) else 2
                cnt = 0
                for dc in range(ndc):
                    i2 = dc % 2
                    k.dma("pool", wso[i2][:], wso_v[:, :, dc * 128:(dc + 1) * 128], writes=[b_wso[i2]])
                    k.dma("pool", wao[i2][:], wao_v[:, :, dc * 128:(dc + 1) * 128], writes=[b_wao[i2]])
                    k.dma("sp", sg1[i2][:], sgT_s[dc * 128:(dc + 1) * 128, :], writes=[b_sg1[i2]])
                    k.dma("sp", sg2[i2][:], sgT_s[D + dc * 128:D + (dc + 1) * 128, :], writes=[b_sg2[i2]])
                    for tb in range(4):
                        sl = slice(tb * 512, (tb + 1) * 512)
                        p1, p1b = next_bank()
                        for c in range(16):
                            mm(p1[:, :], wso[i2][:, c, :], yg[:, c, sl], c == 0, c == 15, [b_wso[i2], b_yg], [p1b], inc=(c == 15))
                        p2, p2b = next_bank()
                        for c in range(8):
                            mm(p2[:, :], wao[i2][:, c, :], oTt[:, c, sl], c == 0, c == 7, [b_wao[i2], b_oTt], [p2b], inc=(c == 7))
                        j2 = cnt % 2
                        cnt += 1
                        tt("dve", t1[j2][:], p1[:, :], sg1[i2][:, sl], ALU.mult, [p1b, b_sg1[i2]], [b_t1[j2]])
                        tt("dve", t2[j2][:], p2[:, :], sg2[i2][:, sl], ALU.mult, [p2b, b_sg2[i2]], [b_t2[j2]])
                        tt("pool", mrg[i2][:, sl], t1[j2][:], t2[j2][:], ALU.add, [b_t1[j2], b_t2[j2]], [b_mrg[i2]])
                    k.dma("sp", mT_s[dc * 128:(dc + 1) * 128, :], mrg[i2][:], reads=[b_mrg[i2]], writes=[k.buf()])
                k.barrier()
            with ExitStack() as sD:
                def sbD(name, shape, dt):
                    return sD.enter_context(nc.sbuf_tensor(name, list(shape), dt))
                mT = sbD("mT", [128, NCH, S], BF16)
                b_mT = k.buf()
                mT_v = mT_s.rearrange("(c p) t -> p c t", p=128)
                for q4 in range(4):
                    k.dma("sp" if q4 % 2 else "act", mT[:, q4 * 8:(q4 + 1) * 8, :], mT_v[:, q4 * 8:(q4 + 1) * 8, :], writes=[b_mT])
                g1rep = sbD("g1rep", [128, D], F32)
                b_g1 = k.buf()
                k.dma("sp", g1rep[:], modrow_s.rearrange("(m c) p -> m (c p)", m=6)[2:3, :].partition_broadcast(128), writes=[b_g1])
                WB = 256
                wo = [sbD("wo%d" % i, [128, NCH, WB], BF16) for i in range(2)]
                b_wo = [k.buf(), k.buf()]
                xt_ = [sbD("xtD%d" % i, [128, WB], F32) for i in range(3)]
                b_xt_ = [k.buf() for _ in range(3)]
                tmpD = [sbD("tmpD%d" % i, [128, WB], F32) for i in range(2)]
                b_tmpD = [k.buf(), k.buf()]
                wo_v = I("w_o").rearrange("(c p) n -> p c n", p=128)
                nnb = D // WB if (ALLP or "D_full" in phases) else 1
                cnt = 0
                for nb2 in range(nnb):
                    i2 = nb2 % 2
                    cs_ = slice(nb2 * WB, (nb2 + 1) * WB)
                    k.dma("pool", wo[i2][:], wo_v[:, :, cs_], writes=[b_wo[i2]])
                    for tt_ in range(NT):
                        i3 = cnt % 3
                        j2 = cnt % 2
                        cnt += 1
                        k.dma("sp", xt_[i3][:], I("x")[tt_ * 128:(tt_ + 1) * 128, cs_], writes=[b_xt_[i3]])
                        pb, pbb = next_bank()
                        for c in range(NCH):
                            mm(pb[:, 0:WB], mT[:, c, tt_ * 128:(tt_ + 1) * 128], wo[i2][:, c, :], c == 0, c == NCH - 1,
                               [b_mT, b_wo[i2]], [pbb], inc=(c == NCH - 1))
                        tt("dve", tmpD[j2][:], pb[:, 0:WB], g1rep[:, cs_], ALU.mult, [pbb, b_g1], [b_tmpD[j2]])
                        tt("pool", xt_[i3][:], xt_[i3][:], tmpD[j2][:], ALU.add, [b_xt_[i3], b_tmpD[j2]], [b_xt_[i3]])
                        k.dma("act", x1_s[tt_ * 128:(tt_ + 1) * 128, cs_], xt_[i3][:], reads=[b_xt_[i3]], writes=[k.buf()])
                k.barrier()
        CAP = 256
        NE = 16
        if ALLP or "pE" in phases:
            with ExitStack() as sE0:
                def sbE(name, shape, dt):
                    return sE0.enter_context(nc.sbuf_tensor(name, list(shape), dt))
                afft = sbE("afft", [128, NT, NE], F32)
                msk = sbE("msk", [128, NT, NE], F32)
                rnk = sbE("rnk", [128, NT, NE], F32)
                wtm = sbE("wtm", [128, NT, NE], F32)
                rankT = sbE("rankT", [NE, S], F32)
                wT = sbE("wT", [NE, S], F32)
                b_aff, b_msk, b_rnk, b_wtm, b_rankT, b_wT = (k.buf() for _ in range(6))
                with ExitStack() as sE1:
                    def sb1(name, shape, dt):
                        return sE1.enter_context(nc.sbuf_tensor(name, list(shape), dt))
                    h2T = sb1("h2T", [128, NCH, S], BF16)
                    b_h2T = [k.buf() for _ in range(NT)]
                    with ExitStack() as sE2:
                        norm_to_hT(sE2, x1_s, n2col, 3, 4, h2T, b_h2T, "e")
                        k.barrier()
                    if "h2T_s" in debug:
                        h2T_s = scr("h2T_s", [D, S], BF16)
                        k.dma("sp", h2T_s.rearrange("(c p) t -> p c t", p=128), h2T[:], reads=b_h2T, writes=[k.buf()])
                    wr = sb1("wr", [128, NCH, NE], BF16)
                    b_wr = k.buf()
                    k.dma("pool", wr[:], I("w_router").rearrange("(c p) n -> p c n", p=128), writes=[b_wr])
                    lg = sb1("lg", [128, NT, NE], F32)
                    b_lg = k.buf()
                    for tt_ in range(NT):
                        pb, pbb = next_bank()
                        for c in range(NCH):
                            mm(pb[:, 0:NE], h2T[:, c, tt_ * 128:(tt_ + 1) * 128], wr[:, c, :], c == 0, c == NCH - 1,
                               [b_wr, b_h2T[tt_]], [pbb], inc=(c == NCH - 1))
                        cp("dve", lg[:, tt_, :], pb[:, 0:NE], [pbb], [b_lg])
                    mx = sb1("mx", [128, NT], F32)
                    sm = sb1("sm", [128, NT], F32)
                    b_mx = k.buf()
                    k.op("dve", lambda e: e.tensor_reduce(out=mx[:], in_=lg[:], axis=AX.X, op=ALU.max), reads=[b_lg], writes=[b_mx])
                    ts("dve", mx[:], mx[:], -1.0, None, ALU.mult, None, [b_mx], [b_mx])
                    for tt_ in range(NT):
                        actf(afft[:, tt_, :], lg[:, tt_, :], AF.Exp, [b_lg, b_mx], [b_aff], bias=mx[:, tt_:tt_ + 1], scale=1.0)
                    k.op("dve", lambda e: e.tensor_reduce(out=sm[:], in_=afft[:], axis=AX.X, op=ALU.add), reads=[b_aff], writes=[b_mx])
                    k.op("dve", lambda e: e.reciprocal(out=sm[:], in_=sm[:]), reads=[b_mx], writes=[b_mx])
                    for tt_ in range(NT):
                        ts("dve", afft[:, tt_, :], afft[:, tt_, :], sm[:, tt_:tt_ + 1], None, ALU.mult, None, [b_aff, b_mx], [b_aff])
                    h2st = [sb1("h2st%d" % i, [128, D], BF16) for i in range(2)]
                    b_h2st = [k.buf(), k.buf()]
                    for tt_ in range(NT):
                        st_, bst = h2st[tt_ % 2], b_h2st[tt_ % 2]
                        for q8 in range(4):
                            pt, ptbf = next_tbank()
                            for j in range(8):
                                c = q8 * 8 + j
                                k.op("pe", lambda e, pt=pt, j=j, c=c, tt_=tt_: e.transpose(
                                    pt[:, j * 128:(j + 1) * 128], h2T[:, c, tt_ * 128:(tt_ + 1) * 128], identb[:]),
                                    reads=[b_h2T[tt_], b_identb], writes=[ptbf], inc=(j == 7))
                            cp(evac_eng(), st_[:, q8 * 1024:(q8 + 1) * 1024], pt[:, :], [ptbf], [bst])
                        k.dma("sp", h2_s[tt_ * 128:(tt_ + 1) * 128, :], st_[:], reads=[bst], writes=[k.buf()])
                    affT = sb1("affT", [NE, S], F32)
                    b_affT = k.buf()
                    for q4 in range(4):
                        pb, pbb = next_bank()
                        for j in range(4):
                            tt_ = q4 * 4 + j
                            k.op("pe", lambda e, pb=pb, j=j, tt_=tt_: e.transpose(
                                pb[0:NE, j * 128:(j + 1) * 128], afft[:, tt_, :], ident[:]),
                                reads=[b_aff, b_ident], writes=[pbb], inc=(j == 3))
                        cp("dve", affT[:, q4 * 512:(q4 + 1) * 512], pb[0:NE, :], [pbb], [b_affT])
                    lo = sb1("lo", [NE, 1], F32)
                    mid = sb1("mid", [NE, 1], F32)
                    cntc = sb1("cntc", [NE, 1], F32)
                    gec = sb1("gec", [NE, 1], F32)
                    cmpj = sb1("cmpj", [NE, S], F32)
                    b_lo, b_mid, b_cnt, b_ge, b_cmp = (k.buf() for _ in range(5))
                    k.op("dve", lambda e: e.memset(lo[:], 0.0), writes=[b_lo])
                    for it in range(1, 31):
                        wdt = 2.0 ** (-it)
                        ts("dve", mid[:], lo[:], wdt, None, ALU.add, None, [b_lo], [b_mid])
                        ts("dve", cmpj[:], affT[:], mid[:, 0:1], None, ALU.is_ge, None, [b_affT, b_mid], [b_cmp])
                        k.op("dve", lambda e: e.tensor_reduce(out=cntc[:], in_=cmpj[:], axis=AX.X, op=ALU.add),
                             reads=[b_cmp], writes=[b_cnt])
                        ts("dve", gec[:], cntc[:], float(CAP), None, ALU.is_ge, None, [b_cnt], [b_ge])
                        stt("dve", lo[:], gec[:], wdt, lo[:], ALU.mult, ALU.add, [b_ge, b_lo], [b_lo])
                    dg = sb1("dg", [NE, NE], F32)
                    ones16 = sb1("ones16", [NE, 128], F32)
                    threp = sb1("threp", [128, NE], F32)
                    b_dg, b_threp = k.buf(), k.buf()
                    k.op("pool", lambda e: e.memset(ones16[:], 1.0), writes=[b_dg])
                    ts("dve", dg[:], ident[0:NE, 0:NE], lo[:, 0:1], None, ALU.mult, None, [b_lo, b_ident], [b_dg])
                    pb, pbb = next_bank()
                    mm(pb[:, 0:NE], ones16[:], dg[:], True, True, [b_dg], [pbb])
                    cp("dve", threp[:], pb[:, 0:NE], [pbb], [b_threp])
                    for tt_ in range(NT):
                        tt("dve", msk[:, tt_, :], afft[:, tt_, :], threp[:], ALU.is_ge, [b_aff, b_threp], [b_msk])
                    mskf = msk[:, :, :].rearrange("p a b -> p (a b)")
                    tt("dve", wtm[:, :, :].rearrange("p a b -> p (a b)"), mskf, afft[:, :, :].rearrange("p a b -> p (a b)"),
                       ALU.mult, [b_msk, b_aff], [b_wtm])
                    triE2 = sb1("triE2", [128, 128], F32)
                    onesf2 = sb1("onesf2", [128, 128], F32)
                    b_tri2 = k.buf()
                    k.op("pool", lambda e: e.memset(onesf2[:], 1.0), writes=[b_tri2])
                    k.op("pool", lambda e: e.memset(triE2[:], 1.0), writes=[b_tri2])
                    k.op("pool", lambda e: e.affine_select(out=triE2[:], in_=triE2[:], pattern=[[1, 128]], compare_op=ALU.is_ge,
                                                           fill=0.0, base=-1, channel_multiplier=-1), reads=[b_tri2], writes=[b_tri2])
                    pbk, pbkb = next_bank()
                    for i in range(NT):
                        for j in range(i + 1):
                            mm(pbk[:, i * NE:(i + 1) * NE], (triE2 if j == i else onesf2)[:], msk[:, j, :], j == 0, j == i,
                               [b_msk, b_tri2], [pbkb], inc=(i == NT - 1 and j == i))
                    cp("dve", rnk[:, :, :].rearrange("p a b -> p (a b)"), pbk[:, 0:NT * NE], [pbkb], [b_rnk])
                    for src, srcb, dst, dstb in ((rnk, b_rnk, rankT, b_rankT), (wtm, b_wtm, wT, b_wT)):
                        for q4 in range(4):
                            pb, pbb = next_bank()
                            for j in range(4):
                                tt_ = q4 * 4 + j
                                k.op("pe", lambda e, pb=pb, j=j, tt_=tt_, src=src: e.transpose(
                                    pb[0:NE, j * 128:(j + 1) * 128], src[:, tt_, :], ident[:]),
                                    reads=[srcb, b_ident], writes=[pbb], inc=(j == 3))
                            cp("dve", dst[:, q4 * 512:(q4 + 1) * 512], pb[0:NE, :], [pbb], [dstb])
                    if "aff_s" in debug:
                        aff_s = scr("aff_s", [128, NT * NE], F32)
                        msk_s = scr("msk_s", [128, NT * NE], F32)
                        rnk_s = scr("rnk_s", [128, NT * NE], F32)
                        k.dma("sp", aff_s[:, :], afft[:, :, :].rearrange("p a b -> p (a b)"), reads=[b_aff], writes=[k.buf()])
                        k.dma("sp", msk_s[:, :], mskf, reads=[b_msk], writes=[k.buf()])
                        k.dma("sp", rnk_s[:, :], rnk[:, :, :].rearrange("p a b -> p (a b)"), reads=[b_rnk], writes=[k.buf()])
                    k.barrier()

                with ExitStack() as sE3:
                    def sb3(name, shape, dt):
                        return sE3.enter_context(nc.sbuf_tensor(name, list(shape), dt))
                    iot = sb3("iot", [128, CAP], F32)
                    b_iot = k.buf()
                    k.op("pool", lambda e: e.iota(iot[:], pattern=[[1, CAP]], base=0, channel_multiplier=0,
                                                  allow_small_or_imprecise_dtypes=True), writes=[b_iot])
                    Pe = [sb3("Pe%d" % i, [128, NT, CAP], BF16) for i in range(2)]
                    b_Pe = [k.buf(), k.buf()]
                    h2q = [sb3("h2q%d" % i, [128, NT, 512], BF16) for i in range(2)]
                    b_h2q = [k.buf(), k.buf()]
                    xeT = sb3("xeT", [128, NCH, CAP], BF16)
                    b_xeT = [k.buf() for _ in range(8)]
                    wg = [sb3("wg%d" % i, [128, NCH, 128], BF16) for i in range(2)]
                    wu = [sb3("wu%d" % i, [128, NCH, 128], BF16) for i in range(2)]
                    b_wg, b_wu = [k.buf(), k.buf()], [k.buf(), k.buf()]
                    sgt = [sb3("sgt%d" % i, [128, CAP], F32) for i in range(2)]
                    b_sgt = [k.buf(), k.buf()]
                    hid = sb3("hid", [128, 16, CAP], BF16)
                    b_hid = k.buf()
                    wd = [sb3("wd%d" % i, [128, 16, 512], BF16) for i in range(2)]
                    b_wd = [k.buf(), k.buf()]
                    ysb = [sb3("ysb%d" % i, [128, 512], BF16) for i in range(2)]
                    b_ysb = [k.buf(), k.buf()]
                    h2_v = h2_s.rearrange("(tt p) n -> p tt n", p=128)
                    nexp = NE if (ALLP or "E_full" in phases) else 1
                    qc = 0
                    fc = 0
                    dc_ = 0
                    for ex in range(nexp):
                        P_, bP = Pe[ex % 2], b_Pe[ex % 2]
                        for tt_ in range(NT):
                            ts("dve", P_[:, tt_, :], iot[:], rnk[:, tt_, ex:ex + 1], msk[:, tt_, ex:ex + 1], ALU.is_equal, ALU.mult,
                               [b_iot, b_rnk, b_msk], [bP])
                        for q in range(8):
                            hq, bhq = h2q[qc % 2], b_h2q[qc % 2]
                            qc += 1
                            k.dma("sp" if q % 2 else "act", hq[:], h2_v[:, :, q * 512:(q + 1) * 512], writes=[bhq])
                            for c2 in range(2):
                                pb, pbb = next_bank()
                                for hh in range(2):
                                    cl = c2 * 2 + hh
                                    for tt_ in range(NT):
                                        mm(pb[:, hh * CAP:(hh + 1) * CAP], hq[:, tt_, cl * 128:(cl + 1) * 128], P_[:, tt_, :],
                                           tt_ == 0, tt_ == NT - 1, [bhq, bP], [pbb], inc=(hh == 1 and tt_ == NT - 1))
                                c = q * 4 + c2 * 2
                                cp(evac_eng(), xeT[:, c:c + 2, :], pb[:, :].rearrange("p (a b) -> p a b", b=CAP), [pbb], [b_xeT[q]])
                        for fb in range(16):
                            wg_, bwg = wg[fc % 2], b_wg[fc % 2]
                            wu_, bwu = wu[fc % 2], b_wu[fc % 2]
                            sg_, bsg = sgt[fc % 2], b_sgt[fc % 2]
                            fc += 1
                            k.dma("pool", wg_[:], I("w_gate_e")[ex].rearrange("(c p) n -> p c n", p=128)[:, :, fb * 128:(fb + 1) * 128],
                                  writes=[bwg])
                            k.dma("pool", wu_[:], I("w_up_e")[ex].rearrange("(c p) n -> p c n", p=128)[:, :, fb * 128:(fb + 1) * 128],
                                  writes=[bwu])
                            pb, pbb = next_bank()
                            for c in range(NCH):
                                mm(pb[:, 0:CAP], wg_[:, c, :], xeT[:, c, :], c == 0, c == NCH - 1, [bwg] + b_xeT, [pbb], inc=False)
                            for c in range(NCH):
                                mm(pb[:, CAP:2 * CAP], wu_[:, c, :], xeT[:, c, :], c == 0, c == NCH - 1, [bwu] + b_xeT, [pbb],
                                   inc=(c == NCH - 1))
                            actf(sg_[:], pb[:, 0:CAP], AF.Silu, [pbb], [bsg])
                            tt("dve", hid[:, fb, :], sg_[:], pb[:, CAP:2 * CAP], ALU.mult, [bsg, pbb], [b_hid])
                        for nb2 in range(8):
                            wd_, bwd = wd[dc_ % 2], b_wd[dc_ % 2]
                            dc_ += 1
                            k.dma("pool", wd_[:], I("w_down_e")[ex].rearrange("(c p) n -> p c n", p=128)[:, :, nb2 * 512:(nb2 + 1) * 512],
                                  writes=[bwd])
                            for st2 in range(2):
                                pb, pbb = next_bank()
                                for fb in range(16):
                                    mm(pb[:, :], hid[:, fb, st2 * 128:(st2 + 1) * 128], wd_[:, fb, :], fb == 0, fb == 15,
                                       [b_hid, bwd], [pbb], inc=(fb == 15))
                                ys_, bys = ysb[st2], b_ysb[st2]
                                cp(evac_eng(), ys_[:], pb[:, :], [pbb], [bys])
                                k.dma("sp", y_s[ex * CAP + st2 * 128:ex * CAP + (st2 + 1) * 128, nb2 * 512:(nb2 + 1) * 512], ys_[:],
                                      reads=[bys], writes=[k.buf()])
                    k.barrier()

                with ExitStack() as sF:
                    def sbF(name, shape, dt):
                        return sF.enter_context(nc.sbuf_tensor(name, list(shape), dt))
                    TG = 256
                    sel = sbF("sel", [NE, NE, 128], F32)
                    b_sel = k.buf()
                    k.op("pool", lambda e: e.memset(sel[:], 0.0), writes=[b_sel])
                    k.op("pool", lambda e: e.affine_select(out=sel[:], in_=sel[:], pattern=[[-1, NE], [0, 128]],
                                                           compare_op=ALU.not_equal, fill=1.0, base=0, channel_multiplier=1),
                         reads=[b_sel], writes=[b_sel])
                    slotc = sbF("slotc", [128, 2], F32)
                    b_slotc = k.buf()
                    k.op("pool", lambda e: e.iota(slotc[:], pattern=[[128, 2]], base=0, channel_multiplier=1,
                                                  allow_small_or_imprecise_dtypes=True), writes=[b_slotc])
                    g2rep = sbF("g2rep", [128, D], F32)
                    nfrep = sbF("nfrep", [128, D], F32)
                    b_g2, b_nf = k.buf(), k.buf()
                    k.dma("sp", g2rep[:], modrow_s.rearrange("(m c) p -> m (c p)", m=6)[5:6, :].partition_broadcast(128), writes=[b_g2])
                    k.dma("act", nfrep[:], I("normf_w").rearrange("c p -> (c p)").partition_broadcast(128), writes=[b_nf])
                    PTs = [sbF("PT%d" % i, [128, 2 * NE, TG], BF16) for i in range(2)]
                    b_PTs = [k.buf(), k.buf()]
                    wrep = [sbF("wrep%d" % i, [128, TG], F32) for i in range(2)]
                    b_wrep = [k.buf(), k.buf()]
                    yall = [sbF("yall%d" % i, [128, 2 * NE, 256], BF16) for i in range(2)]
                    b_yall = [k.buf(), k.buf()]
                    x2 = sbF("x2", [128, 2, D], F32)
                    b_x2 = [k.buf(), k.buf()]
                    tmpF = [sbF("tmpF%d" % i, [128, 512], F32) for i in range(2)]
                    b_tmpF = [k.buf(), k.buf()]
                    junkF = sbF("junkF", [128, D], BF16)
                    b_junkF = k.buf()
                    ssF = sbF("ssF", [128, NT], F32)
                    b_ssF = k.buf()
                    k.op("pool", lambda e: e.memset(ssF[:], 0.0), writes=[b_ssF])
                    y_v = y_s.rearrange("(eh p) n -> p eh n", p=128)
                    ntg = S // TG if (ALLP or "F_full" in phases) else 1
                    yc = 0
                    wc = 0
                    wcl = [0]

                    def build_PT_expert(tg_, ex):
                        PT_, bPT_ = PTs[tg_ % 2], b_PTs[tg_ % 2]
                        tsl_ = slice(tg_ * TG, (tg_ + 1) * TG)
                        pr, prb = next_bank()
                        mm(pr[:, 0:TG], sel[:, ex, :], rankT[:, tsl_], True, True, [b_sel, b_rankT], [prb], inc=False)
                        mm(pr[:, TG:2 * TG], sel[:, ex, :], wT[:, tsl_], True, True, [b_sel, b_wT], [prb], inc=True)
                        wr_, bwr = wrep[wcl[0] % 2], b_wrep[wcl[0] % 2]
                        wcl[0] += 1
                        cp("act", wr_[:], pr[:, TG:2 * TG], [prb], [bwr])
                        for half in range(2):
                            stt("dve", PT_[:, 2 * ex + half, :], pr[:, 0:TG], slotc[:, half:half + 1], wr_[:], ALU.is_equal, ALU.mult,
                                [prb, b_slotc, bwr], [bPT_])

                    for ex in range(NE):
                        build_PT_expert(0, ex)
                    for tg in range(ntg):
                        PT, b_PT = PTs[tg % 2], b_PTs[tg % 2]
                        for ti in range(2):
                            k.dma("act", x2[:, ti, :], x1_s[tg * TG + ti * 128:tg * TG + (ti + 1) * 128, :], writes=[b_x2[ti]])
                        for nb2 in range(16):
                            if tg + 1 < ntg:
                                build_PT_expert(tg + 1, nb2)
                            ya, bya = yall[yc % 2], b_yall[yc % 2]
                            yc += 1
                            k.dma("sp", ya[:], y_v[:, :, nb2 * 256:(nb2 + 1) * 256], writes=[bya])
                            for ti in range(2):
                                pb, pbb = next_bank()
                                for eh in range(2 * NE):
                                    mm(pb[:, 0:256], PT[:, eh, ti * 128:(ti + 1) * 128], ya[:, eh, :], eh == 0, eh == 2 * NE - 1,
                                       [b_PT, bya], [pbb], inc=(eh == 2 * NE - 1))
                                tf, btf = tmpF[ti], b_tmpF[ti]
                                csl = slice(nb2 * 256, (nb2 + 1) * 256)
                                tt("dve", tf[:, 0:256], pb[:, 0:256], g2rep[:, csl], ALU.mult, [pbb, b_g2], [btf])
                                tt("pool", x2[:, ti, csl], x2[:, ti, csl], tf[:, 0:256], ALU.add, [b_x2[ti], btf], [b_x2[ti]])
                        for ti in range(2):
                            col = tg * 2 + ti
                            actf(junkF[:], x2[:, ti, :], AF.Square, [b_x2[ti]], [b_junkF, b_ssF], accum_out=ssF[:, col:col + 1])
                            ts("dve", ssF[:, col:col + 1], ssF[:, col:col + 1], 1.0 / D, 1e-6, ALU.mult, ALU.add, [b_ssF], [b_ssF])
                            k.op("act", lambda e, col=col: e.sqrt(out=ssF[:, col:col + 1], in_=ssF[:, col:col + 1]),
                                 reads=[b_ssF], writes=[b_ssF])
                            k.op("dve", lambda e, col=col: e.reciprocal(out=ssF[:, col:col + 1], in_=ssF[:, col:col + 1]),
                                 reads=[b_ssF], writes=[b_ssF])
                            stt("dve", x2[:, ti, :], x2[:, ti, :], ssF[:, col:col + 1], nfrep[:], ALU.mult, ALU.mult,
                                [b_x2[ti], b_ssF, b_nf], [b_x2[ti]])
                            k.dma("sp", out[tg * TG + ti * 128:tg * TG + (ti + 1) * 128, :], x2[:, ti, :], reads=[b_x2[ti]],
                                  writes=[k.buf()])
                    k.barrier()

        for q in ("sp", "pool", "act"):
            e = k.eng[q]
            for sem, cnt in k.dsem[q]:
                if cnt > 0 and e.known.get(id(sem), 0) < cnt:
                    e.h.wait_ge(sem, cnt)
                    e.known[id(sem)] = cnt
    return nc


_PROG = {}
N_ACTIVE = 4


def _core_inputs(inp, b):
    f = lambda a: np.ascontiguousarray(np.asarray(a, dtype=np.float32))
    return {
        "x": f(inp["x"][b]), "c": f(inp["c"][b]).reshape(32, 128),
        "norm1_w": f(inp["norm1_w"][0]).reshape(32, 128), "norm2_w": f(inp["norm2_w"][0]).reshape(32, 128),
        "normf_w": f(inp["normf_w"]).reshape(32, 128), "w_ada": f(inp["w_ada"][0]),
        "b_ada": f(inp["b_ada"][0]).reshape(192, 128), "w_in": f(inp["w_in"][0]),
        "conv_w": f(inp["conv_w"][0]).reshape(5, 32, 128), "conv_b": f(inp["conv_b"][0]).reshape(32, 128),
        "dt_bias_f": f(inp["dt_bias_f"][0]), "dt_bias_b": f(inp["dt_bias_b"][0]),
        "a_log_f": f(inp["a_log_f"][0]), "a_log_b": f(inp["a_log_b"][0]), "d_skip": f(inp["d_skip"][0]),
        "ssm_norm_w": f(inp["ssm_norm_w"][0]).reshape(16, 128), "w_ssm_out": f(inp["w_ssm_out"][0]),
        "w_attn_out": f(inp["w_attn_out"][0]), "w_o": f(inp["w_o"][0]), "w_router": f(inp["w_router"][0]),
        "w_gate_e": f(inp["w_gate_e"][0]), "w_up_e": f(inp["w_up_e"][0]), "w_down_e": f(inp["w_down_e"][0]),
    }


def kernel(**inputs):
    if "nc" not in _PROG:
        _PROG["nc"] = build_program()
    nc = _PROG["nc"]
    shared = _core_inputs(inputs, 0)
    in_maps = []
    for b in range(N_ACTIVE):
        m = dict(shared)
        m["x"] = np.ascontiguousarray(np.asarray(inputs["x"][b], dtype=np.float32))
        m["c"] = np.ascontiguousarray(np.asarray(inputs["c"][b], dtype=np.float32)).reshape(32, 128)
        in_maps.append(m)
    res = run_bass_kernel_spmd(nc, in_maps, core_ids=list(range(N_ACTIVE)))
    return np.stack([np.asarray(res.results[b]["out"], dtype=np.float32) for b in range(N_ACTIVE)], axis=0)
```

```python
import math
import numpy as np
import concourse.bass as bass
import concourse.mybir as mybir
from concourse.bass_utils import run_bass_kernel_spmd

F32 = mybir.dt.float32
BF16 = mybir.dt.bfloat16
I32 = mybir.dt.int32
AF = mybir.ActivationFunctionType
ALU = mybir.AluOpType
AX = mybir.AxisListType

D = 4096
S = 2048
NT = S // 128
NCH = D // 128
IN_DIM = 23616
OFF_Z, OFF_XBC, OFF_DT, OFF_Q, OFF_K, OFF_V, OFF_G = 0, 2048, 6144, 6208, 9280, 12352, 15424
NEG = -30000.0


class Buf:
    __slots__ = ("name", "w", "r")

    def __init__(self, name):
        self.name = name
        self.w = {}
        self.r = {}


class Eng:
    def __init__(self, name, h, sem):
        self.name, self.h, self.sem = name, h, sem
        self.count = 0
        self.known = {}
        self.pend_r, self.pend_w = [], []


class K:
    def __init__(self, nc, stack):
        self.nc = nc
        self.stack = stack
        self.eng = {}
        for nm, h in (("pe", nc.tensor), ("dve", nc.vector), ("act", nc.scalar),
                      ("pool", nc.gpsimd), ("sp", nc.sync)):
            sem = stack.enter_context(nc.semaphore("s_" + nm))
            self.eng[nm] = Eng(nm, h, sem)
        self.dsem = {}
        for q, n in (("sp", 24), ("pool", 24), ("act", 8)):
            self.dsem[q] = [[stack.enter_context(nc.semaphore("d_%s%d" % (q, i))), 0] for i in range(n)]
        self.dptr = {"sp": 0, "pool": 0, "act": 0}
        self.nbuf = 0

    def buf(self, name=None):
        self.nbuf += 1
        return Buf(name or "b%d" % self.nbuf)

    def _waits(self, e, reads, writes):
        need = {}

        def add(ev):
            if ev is None:
                return
            s, v = ev
            k = id(s)
            if k not in need or need[k][1] < v:
                need[k] = (s, v)
        for b in reads:
            for ev in b.w.values():
                add(ev)
        for b in writes:
            for ev in b.w.values():
                add(ev)
            for ev in b.r.values():
                add(ev)
        for k, (s, v) in need.items():
            if e.name == "pe" and s is e.sem:
                continue
            if e.known.get(k, 0) < v:
                e.h.wait_ge(s, v)
                e.known[k] = v

    def _commit(self, ev, reads, writes):
        kk = id(ev[0])
        for b in reads:
            if kk not in b.r or b.r[kk][1] < ev[1]:
                b.r[kk] = ev
        for b in writes:
            if kk not in b.w or b.w[kk][1] < ev[1]:
                b.w[kk] = ev
            b.r = {}

    def op(self, en, fn, reads=(), writes=(), inc=True):
        e = self.eng[en]
        self._waits(e, reads, writes)
        ins = fn(e.h)
        if not inc:
            e.pend_r.extend(reads)
            e.pend_w.extend(writes)
            return ins
        e.count += 1
        ins.then_inc(e.sem, 1)
        ev = (e.sem, e.count)
        e.known[id(e.sem)] = max(e.known.get(id(e.sem), 0), 0)
        self._commit(ev, list(reads) + e.pend_r, list(writes) + e.pend_w)
        e.pend_r, e.pend_w = [], []
        return ins

    def dma(self, q, out, in_, reads=(), writes=(), **kw):
        e = self.eng[q]
        pool = self.dsem[q]
        i = self.dptr[q]
        self.dptr[q] = (i + 1) % len(pool)
        sem, cnt = pool[i]
        if cnt > 0 and e.known.get(id(sem), 0) < cnt:
            e.h.wait_ge(sem, cnt)
            e.known[id(sem)] = cnt
        self._waits(e, reads, writes)
        e.h.dma_start(out=out, in_=in_, **kw).then_inc(sem, 16)
        pool[i][1] = cnt + 16
        ev = (sem, cnt + 16)
        self._commit(ev, reads, writes)
        return ev

    def wait_all(self, en, bufs):
        e = self.eng[en]
        self._waits(e, bufs, bufs)

    def barrier(self):
        for e in self.eng.values():
            assert not e.pend_r and not e.pend_w, e.name
        for e in self.eng.values():
            for o in self.eng.values():
                if o.count > 0 and e.known.get(id(o.sem), 0) < o.count and o is not e:
                    e.h.wait_ge(o.sem, o.count)
                    e.known[id(o.sem)] = o.count
            for q in self.dsem:
                for sem, cnt in self.dsem[q]:
                    if cnt > 0 and e.known.get(id(sem), 0) < cnt:
                        e.h.wait_ge(sem, cnt)
                        e.known[id(sem)] = cnt


def cdiv(a, b):
    return (a + b - 1) // b


def build_program(phases=("all",), debug=(), feed=()):
    from contextlib import ExitStack
    nc = bass.Bass("TRN2", target_bir_lowering=False)
    ALLP = "all" in phases

    def inp(name, shape, dt=F32):
        return nc.dram_tensor(name, list(shape), dt, kind="ExternalInput").ap()

    def scr(name, shape, dt):
        kind = "ExternalOutput" if name in debug else ("ExternalInput" if name in feed else "Internal")
        return nc.dram_tensor(name, list(shape), dt, kind=kind).ap()

    IN_SHAPES = {
        "x": [S, D], "c": [NCH, 128], "norm1_w": [NCH, 128], "norm2_w": [NCH, 128], "normf_w": [NCH, 128],
        "w_ada": [D, 6 * D], "b_ada": [6 * NCH, 128], "w_in": [D, IN_DIM], "conv_w": [5, NCH, 128],
        "conv_b": [NCH, 128], "dt_bias_f": [32], "dt_bias_b": [32], "a_log_f": [32], "a_log_b": [32],
        "d_skip": [32], "ssm_norm_w": [16, 128], "w_ssm_out": [2048, D], "w_attn_out": [1024, D],
        "w_o": [D, D], "w_router": [D, 16], "w_gate_e": [16, D, 2048], "w_up_e": [16, D, 2048],
        "w_down_e": [16, 2048, D],
    }
    _ins = {}

    def I(name):
        if name not in _ins:
            _ins[name] = inp(name, IN_SHAPES[name])
        return _ins[name]

    out = nc.dram_tensor("out", [S, D], F32, kind="ExternalOutput").ap() if (ALLP or "out" in debug) else None

    modc_s = scr("modc_s", [128, 6 * NCH], F32)
    modrow_s = scr("modrow_s", [6 * NCH, 128], F32)
    hT_s = scr("hT_s", [D, S], BF16) if "hT_s" in debug else None
    zT_s = scr("zT_s", [2048, S], BF16)
    xs_s = scr("xs_s", [S, 2048], BF16)
    xsT_s = scr("xsT_s", [2048, S], BF16)
    BT_s = scr("BT_s", [1024, S], BF16)
    CT_s = scr("CT_s", [1024, S], BF16)
    dt_s = scr("dt_s", [S, 64], F32)
    csT_s = scr("csT_s", [64, S], F32)
    qT_s = scr("qT_s", [3072, S], BF16)
    kT_s = scr("kT_s", [3072, S], BF16)
    v_s = scr("v_s", [S, 3072], BF16)
    sgT_s = scr("sgT_s", [8192, S], BF16)
    ygT_s = scr("ygT_s", [2048, S], BF16)
    oT_s = scr("oT_s", [1024, S], BF16)
    mT_s = scr("mT_s", [D, S], BF16)
    x1_s = scr("x1_s", [S, D], F32)
    h2_s = scr("h2_s", [S, D], BF16)
    y_s = scr("y_s", [16 * 256, D], BF16)

    with ExitStack() as st:
        k = K(nc, st)

        def sb(name, shape, dt):
            return st.enter_context(nc.sbuf_tensor(name, list(shape), dt))

        def ps(name, shape, dt=F32):
            return st.enter_context(nc.psum_tensor(name, list(shape), dt))

        ident = sb("ident", [128, 128], F32)
        identb = sb("identb", [128, 128], BF16)
        b_ident = k.buf("ident")
        k.op("pool", lambda e: e.memset(ident[:], 0.0), writes=[b_ident])
        k.op("pool", lambda e: e.affine_select(out=ident[:], in_=ident[:], pattern=[[-1, 128]],
                                               compare_op=ALU.not_equal, fill=1.0, base=0,
                                               channel_multiplier=1), reads=[b_ident], writes=[b_ident])
        b_identb = k.buf("identb")
        k.op("pool", lambda e: e.tensor_copy(out=identb[:], in_=ident[:]), reads=[b_ident], writes=[b_identb])

        NGB = 6
        psum_banks = [ps("pb%d" % i, [128, 512], F32) for i in range(NGB)]
        pbuf = [k.buf("pb%d" % i) for i in range(NGB)]
        pctr = [0]

        def next_bank(lo=0, hi=NGB):
            i = lo + pctr[0] % (hi - lo)
            pctr[0] += 1
            return psum_banks[i], pbuf[i]
        ptb = [ps("ptb%d" % i, [128, 1024], BF16) for i in range(2)]
        ptbb = [k.buf("ptb%d" % i) for i in range(2)]
        ptctr = [0]

        def next_tbank():
            i = ptctr[0] % 2
            ptctr[0] += 1
            return ptb[i], ptbb[i]

        evac_ctr = [0]

        def evac_eng():
            evac_ctr[0] += 1
            return "act" if evac_ctr[0] % 2 else "dve"

        modc = sb("modc", [128, 6 * NCH], F32)
        b_modc = k.buf("modc")
        n1col = sb("n1col", [128, NCH], F32)
        n2col = sb("n2col", [128, NCH], F32)
        b_ncol = k.buf("ncol")

        def row2col(src_ap, nrows, dst_ap, name):
            with ExitStack() as sx:
                nrow = sx.enter_context(nc.sbuf_tensor("r2c_" + name, [nrows, 128], F32))
                b_nrow = k.buf()
                k.dma("sp", nrow[:], src_ap, writes=[b_nrow])
                pb, pbb = next_bank(0, 5)
                k.op("pe", lambda e: e.transpose(pb[:, 0:nrows], nrow[:], ident[0:nrows, 0:nrows]),
                     reads=[b_nrow, b_ident], writes=[pbb])
                bd = k.buf()
                k.op("dve", lambda e: e.tensor_copy(out=dst_ap, in_=pb[:, 0:nrows]), reads=[pbb], writes=[bd])
                k.barrier()
            return bd

        if ALLP or "p1" in phases or "pB" in phases:
            row2col(I("norm1_w")[:, :], NCH, n1col[:], "n1")
            row2col(I("norm2_w")[:, :], NCH, n2col[:], "n2")

        if ALLP or "p0" in phases:
            with ExitStack() as s0:
                def sb0(name, shape, dt):
                    return s0.enter_context(nc.sbuf_tensor(name, list(shape), dt))
                crow = sb0("crow", [NCH, 128], F32)
                b_crow = k.buf()
                k.dma("sp", crow[:], I("c")[:, :], writes=[b_crow])
                k.op("act", lambda e: e.activation(out=crow[:], in_=crow[:], func=AF.Silu),
                     reads=[b_crow], writes=[b_crow])
                pb, pbb = next_bank(0, 5)
                k.op("pe", lambda e: e.transpose(pb[:, 0:NCH], crow[:], ident[0:NCH, 0:NCH]),
                     reads=[b_crow, b_ident], writes=[pbb])
                ccol = sb0("ccol", [128, NCH], BF16)
                b_ccol = k.buf()
                k.op("dve", lambda e: e.tensor_copy(out=ccol[:], in_=pb[:, 0:NCH]), reads=[pbb], writes=[b_ccol])
                brow = sb0("brow", [64, 3, 128], F32)
                b_brow = k.buf()
                k.dma("sp", brow[:], I("b_ada").rearrange("(a r) p -> r a p", r=64), writes=[b_brow])
                bcol = sb0("bcol", [128, 6 * NCH], F32)
                b_bcol = k.buf()
                for a in range(3):
                    pb, pbb = next_bank(0, 5)
                    k.op("pe", lambda e, pb=pb, a=a: e.transpose(pb[:, 0:64], brow[:, a, :], ident[0:64, 0:64]),
                         reads=[b_brow, b_ident], writes=[pbb])
                    k.op("dve", lambda e, pb=pb, a=a: e.tensor_copy(out=bcol[:, a * 64:(a + 1) * 64], in_=pb[:, 0:64]),
                         reads=[pbb], writes=[b_bcol])
                WA_N = 256
                nwt = 6 * D // WA_N
                NWB = 5
                wbufs = [sb0("wada%d" % i, [128, NCH, WA_N], BF16) for i in range(NWB)]
                wb = [k.buf() for _ in range(NWB)]
                w_ada_v = I("w_ada").rearrange("(c p) n -> p c n", p=128)
                pmod, b_pmod = psum_banks[5], pbuf[5]
                for t in range(nwt):
                    wt, wtb = wbufs[t % NWB], wb[t % NWB]
                    k.dma("pool", wt[:], w_ada_v[:, :, t * WA_N:(t + 1) * WA_N], writes=[wtb])
                    for j in range(WA_N // 128):
                        col = t * (WA_N // 128) + j
                        for c in range(NCH):
                            k.op("pe", lambda e, wt=wt, j=j, c=c, col=col: e.matmul(
                                pmod[:, col:col + 1], lhsT=wt[:, c, j * 128:(j + 1) * 128], rhs=ccol[:, c:c + 1],
                                start=(c == 0), stop=(c == NCH - 1)),
                                reads=[wtb, b_ccol], writes=[b_pmod], inc=(c == NCH - 1))
                k.op("dve", lambda e: e.tensor_tensor(out=modc[:], in0=pmod[:, 0:6 * NCH], in1=bcol[:], op=ALU.add),
                     reads=[b_pmod, b_bcol], writes=[b_modc])
                if "modc_s" in debug:
                    k.dma("sp", modc_s[:, :], modc[:], reads=[b_modc], writes=[k.buf()])
                mrow = sb0("mrow", [96, 2, 128], F32)
                b_mrow = k.buf()
                for a in range(2):
                    pb, pbb = next_bank(0, 5)
                    k.op("pe", lambda e, pb=pb, a=a: e.transpose(pb[0:96, 0:128], modc[:, a * 96:(a + 1) * 96], ident[:]),
                         reads=[b_modc, b_ident], writes=[pbb])
                    k.op("dve", lambda e, pb=pb, a=a: e.tensor_copy(out=mrow[:, a, :], in_=pb[0:96, 0:128]),
                         reads=[pbb], writes=[b_mrow])
                k.dma("sp", modrow_s.rearrange("(a r) p -> r a p", a=2), mrow[:], reads=[b_mrow], writes=[k.buf()])
                k.barrier()

        if not (ALLP or "p0" in phases) and "modc_s" in feed:
            k.dma("sp", modc[:], modc_s[:, :], writes=[b_modc])

        def mm(out, lhsT, rhs, start, stop, reads, writes, inc=True):
            return k.op("pe", lambda e: e.matmul(out, lhsT=lhsT, rhs=rhs, start=start, stop=stop),
                        reads=reads, writes=writes, inc=inc)

        def actf(out, in_, func, reads, writes, **kw):
            return k.op("act", lambda e: e.activation(out=out, in_=in_, func=func, **kw), reads=reads, writes=writes)

        def cp(en, out, in_, reads, writes):
            if en == "act":
                return k.op("act", lambda e: e.copy(out=out, in_=in_), reads=reads, writes=writes)
            return k.op(en, lambda e: e.tensor_copy(out=out, in_=in_), reads=reads, writes=writes)

        def tt(en, out, in0, in1, op, reads, writes):
            return k.op(en, lambda e: e.tensor_tensor(out=out, in0=in0, in1=in1, op=op), reads=reads, writes=writes)

        def ts(en, out, in0, s1, s2, op0, op1, reads, writes):
            if s2 is None:
                return k.op(en, lambda e: e.tensor_scalar(out=out, in0=in0, scalar1=s1, scalar2=None, op0=op0),
                            reads=reads, writes=writes)
            return k.op(en, lambda e: e.tensor_scalar(out=out, in0=in0, scalar1=s1, scalar2=s2, op0=op0, op1=op1),
                        reads=reads, writes=writes)

        def stt(en, out, in0, scalar, in1, op0, op1, reads, writes):
            return k.op(en, lambda e: e.scalar_tensor_tensor(out=out, in0=in0, scalar=scalar, in1=in1, op0=op0, op1=op1),
                        reads=reads, writes=writes)

        def norm_to_hT(sx, src, ncol, m_shift, m_scale, hT, b_hT, tag):
            def sb1(name, shape, dt):
                return sx.enter_context(nc.sbuf_tensor(name + tag, list(shape), dt))
            s1col = sb1("s1col", [128, NCH], F32)
            b_s1 = k.buf()
            stt("dve", s1col[:], modc[:, m_scale * NCH:(m_scale + 1) * NCH], 1.0, ncol[:], ALU.add, ALU.mult,
                [b_modc, b_ncol], [b_s1])
            xts = [sb1("xt%d" % i, [128, D], F32) for i in range(2)]
            b_xt = [k.buf() for _ in range(2)]
            junk = sb1("junk", [128, D], BF16)
            b_junk = k.buf()
            ss = sb1("ss", [128, NT], F32)
            rstd = sb1("rstd", [128, NT], F32)
            b_ss = k.buf()
            k.op("pool", lambda e: e.memset(ss[:], 0.0), writes=[b_ss])
            for tt_ in range(NT):
                xt, bx = xts[tt_ % 2], b_xt[tt_ % 2]
                k.dma("sp", xt[:], src[tt_ * 128:(tt_ + 1) * 128, :], writes=[bx])
                actf(junk[:], xt[:], AF.Square, [bx], [b_junk, b_ss], accum_out=ss[:, tt_:tt_ + 1])
                ts("dve", rstd[:, tt_:tt_ + 1], ss[:, tt_:tt_ + 1], 1.0 / D, 1e-6, ALU.mult, ALU.add, [b_ss], [b_ss])
                k.op("act", lambda e, tt_=tt_: e.sqrt(out=rstd[:, tt_:tt_ + 1], in_=rstd[:, tt_:tt_ + 1]),
                     reads=[b_ss], writes=[b_ss])
                k.op("dve", lambda e, tt_=tt_: e.reciprocal(out=rstd[:, tt_:tt_ + 1], in_=rstd[:, tt_:tt_ + 1]),
                     reads=[b_ss], writes=[b_ss])
                actf(xt[:], xt[:], AF.Copy, [bx, b_ss], [bx], scale=rstd[:, tt_:tt_ + 1])
                for g4 in range(NCH // 4):
                    pb, pbb = next_bank()
                    for j in range(4):
                        c = g4 * 4 + j
                        k.op("pe", lambda e, pb=pb, j=j, c=c, xt=xt: e.transpose(
                            pb[:, j * 128:(j + 1) * 128], xt[:, c * 128:(c + 1) * 128], ident[:]),
                            reads=[bx, b_ident], writes=[pbb], inc=(j == 3))
                    for j in range(4):
                        c = g4 * 4 + j
                        o_ap = hT[:, c, tt_ * 128:(tt_ + 1) * 128]
                        i_ap = pb[:, j * 128:(j + 1) * 128]
                        if evac_eng() == "act":
                            actf(o_ap, i_ap, AF.Identity, [pbb, b_s1, b_modc], [b_hT[tt_]],
                                 scale=s1col[:, c:c + 1], bias=modc[:, m_shift * NCH + c:m_shift * NCH + c + 1])
                        else:
                            ts("dve", o_ap, i_ap, s1col[:, c:c + 1], modc[:, m_shift * NCH + c:m_shift * NCH + c + 1],
                               ALU.mult, ALU.add, [pbb, b_s1, b_modc], [b_hT[tt_]])

        DIL = (1, 4, 16)

        if ALLP or "pA" in phases:
            with ExitStack() as sA:
                def sbA(name, shape, dt):
                    return sA.enter_context(nc.sbuf_tensor(name, list(shape), dt))
                hT = sbA("hT", [128, NCH, S], BF16)
                b_hT = [k.buf("hT%d" % i) for i in range(NT)]
                with ExitStack() as s1:
                    norm_to_hT(s1, I("x"), n1col, 0, 1, hT, b_hT, "a")
                    k.barrier()
                if "hT_s" in debug:
                    k.dma("sp", hT_s.rearrange("(c p) t -> p c t", p=128), hT[:], reads=b_hT, writes=[k.buf()])

                WN = 128
                wts = [sbA("win%d" % i, [128, NCH, WN], BF16) for i in range(2)]
                b_wt = [k.buf() for _ in range(2)]
                wctr = [0]
                w_in_v = I("w_in").rearrange("(c p) n -> p c n", p=128)

                def load_w(col0, n):
                    i = wctr[0] % 2
                    wctr[0] += 1
                    k.dma("pool", wts[i][:, :, 0:n], w_in_v[:, :, col0:col0 + n], writes=[b_wt[i]])
                    return wts[i], b_wt[i]

                def gemm_fm(wt, bw, n0=0):
                    res = []
                    for tb in range(4):
                        pb, pbb = next_bank()
                        for c in range(NCH):
                            mm(pb[:, :], wt[:, c, n0:n0 + 128], hT[:, c, tb * 512:(tb + 1) * 512], c == 0, c == NCH - 1,
                               [bw] + b_hT[4 * tb:4 * tb + 4], [pbb], inc=(c == NCH - 1))
                        res.append((pb, pbb))
                    return res

                def gemm_tm(wt, bw, n, tt_):
                    pb, pbb = next_bank()
                    for c in range(NCH):
                        mm(pb[:, 0:n], hT[:, c, tt_ * 128:(tt_ + 1) * 128], wt[:, c, 0:n], c == 0, c == NCH - 1,
                           [bw, b_hT[tt_]], [pbb], inc=(c == NCH - 1))
                    return pb, pbb

                bufA = sbA("bufA", [128, S + 4], F32)
                bufB = sbA("bufB", [128, S], F32)
                bufCs = [sbA("bufC%d" % i, [128, S], BF16) for i in range(2)]
                bufDs = [sbA("bufD%d" % i, [128, NT, 128], BF16) for i in range(2)]
                bCs = [k.buf(), k.buf()]
                bDs = [k.buf(), k.buf()]
                cctr = [0]

                def rotC():
                    i = cctr[0] % 2
                    cctr[0] += 1
                    return bufCs[i], bCs[i], bufDs[i], bDs[i]
                bufC, bufD = bufCs[0], bufDs[0]
                dtst = sbA("dtst", [128, NT, 64], F32)
                bA, bB, bC, bD, bTM, bDT = (k.buf() for _ in range(6))
                k.op("pool", lambda e: e.memset(bufA[:], 0.0), writes=[bA])

                def fm_job(col0, nblk, dst, func):
                    nonlocal bufC, bC, bufD, bD
                    for blk in range(nblk):
                        bufC, bC, bufD, bD = rotC()
                        wt, bw = load_w(col0 + blk * 128, 128)
                        res = gemm_fm(wt, bw)
                        for tb, (pb, pbb) in enumerate(res):
                            actf(bufC[:, tb * 512:(tb + 1) * 512], pb[:, :], func, [pbb], [bC])
                        k.dma("sp", dst[blk * 128:(blk + 1) * 128, :], bufC[:], reads=[bC], writes=[k.buf()])

                if ALLP or "A_z" in phases:
                    fm_job(OFF_Z, 16, zT_s, AF.Silu)
                if ALLP or "A_dt" in phases:
                    wt, bw = load_w(OFF_DT, 64)
                    for tt_ in range(NT):
                        pb, pbb = gemm_tm(wt, bw, 64, tt_)
                        cp("dve", dtst[:, tt_, :], pb[:, 0:64], [pbb], [bDT])
                    k.dma("sp", dt_s.rearrange("(tt p) n -> p tt n", p=128), dtst[:], reads=[bDT], writes=[k.buf()])
                def fm_to_tm(dstv):
                    for half in range(2):
                        pt, ptbf = next_tbank()
                        for j in range(8):
                            tt_ = half * 8 + j
                            k.op("pe", lambda e, pt=pt, j=j, tt_=tt_: e.transpose(
                                pt[:, j * 128:(j + 1) * 128], bufC[:, tt_ * 128:(tt_ + 1) * 128], identb[:]),
                                reads=[bC, b_identb], writes=[ptbf], inc=(j == 7))
                        cp(evac_eng(), bufD[:, half * 8:(half + 1) * 8, :],
                           pt[:, :].rearrange("p (a b) -> p a b", b=128), [ptbf], [bD])
                    k.dma("sp", dstv, bufD[:], reads=[bD], writes=[k.buf()])

                if ALLP or "A_v" in phases:
                    for blk in range(24):
                        bufC, bC, bufD, bD = rotC()
                        wt, bw = load_w(OFF_V + blk * 128, 128)
                        res = gemm_fm(wt, bw)
                        for tb, (pb, pbb) in enumerate(res):
                            cp(evac_eng(), bufC[:, tb * 512:(tb + 1) * 512], pb[:, :], [pbb], [bC])
                        fm_to_tm(v_s.rearrange("(tt p) n -> p tt n", p=128)[:, :, blk * 128:(blk + 1) * 128])
                if ALLP or "A_g" in phases:
                    fm_job(OFF_G, 64, sgT_s, AF.Sigmoid)
                if ALLP or "A_x" in phases:
                    cwc = sbA("cwc", [128, 6, NCH], F32)
                    for kk in range(5):
                        row2col(I("conv_w")[kk, :, :], NCH, cwc[:, kk, :], "cw%d" % kk)
                    bcw = row2col(I("conv_b")[:, :], NCH, cwc[:, 5, :], "cb")
                    for blk in range(32):
                        bufC, bC, bufD, bD = rotC()
                        wt, bw = load_w(OFF_XBC + blk * 128, 128)
                        res = gemm_fm(wt, bw)
                        for tb, (pb, pbb) in enumerate(res):
                            cp(evac_eng(), bufA[:, 2 + tb * 512:2 + (tb + 1) * 512], pb[:, :], [pbb], [bA])
                        ts("dve", bufB[:], bufA[:, 0:S], cwc[:, 0, blk:blk + 1], cwc[:, 5, blk:blk + 1], ALU.mult, ALU.add,
                           [bA, bcw], [bB])
                        for kk in range(1, 5):
                            stt("dve", bufB[:], bufA[:, kk:kk + S], cwc[:, kk, blk:blk + 1], bufB[:], ALU.mult, ALU.add,
                                [bA, bB, bcw], [bB])
                        actf(bufC[:], bufB[:], AF.Silu, [bB], [bC])
                        if blk >= 24:
                            k.dma("sp", CT_s[(blk - 24) * 128:(blk - 23) * 128, :], bufC[:], reads=[bC], writes=[k.buf()])
                            continue
                        if blk >= 16:
                            k.dma("sp", BT_s[(blk - 16) * 128:(blk - 15) * 128, :], bufC[:], reads=[bC], writes=[k.buf()])
                            continue
                        k.dma("sp", xsT_s[blk * 128:(blk + 1) * 128, :], bufC[:], reads=[bC], writes=[k.buf()])
                        for half in range(2):
                            pt, ptbf = next_tbank()
                            for j in range(8):
                                tt_ = half * 8 + j
                                k.op("pe", lambda e, pt=pt, j=j, tt_=tt_: e.transpose(
                                    pt[:, j * 128:(j + 1) * 128], bufC[:, tt_ * 128:(tt_ + 1) * 128], identb[:]),
                                    reads=[bC, b_identb], writes=[ptbf], inc=(j == 7))
                            cp(evac_eng(), bufD[:, half * 8:(half + 1) * 8, :],
                               pt[:, :].rearrange("p (a b) -> p a b", b=128), [ptbf], [bD])
                        dstv = xs_s.rearrange("(tt p) n -> p tt n", p=128)[:, :, blk * 128:(blk + 1) * 128]
                        k.dma("sp", dstv, bufD[:], reads=[bD], writes=[k.buf()])
                if ALLP or "A_qk" in phases:
                    cost = sbA("cost", [128, S], F32)
                    sint = sbA("sint", [128, S], F32)
                    perm = sbA("perm", [128, 128], F32)
                    colf = sbA("colf", [128, 4], F32)
                    b_tab, b_perm, b_colf = k.buf(), k.buf(), k.buf()
                    k.op("pool", lambda e: e.memset(perm[:], 0.0), writes=[b_perm])
                    for bs in (-64, 64):
                        k.op("pool", lambda e, bs=bs: e.affine_select(out=perm[:], in_=perm[:], pattern=[[-1, 128]],
                                                                      compare_op=ALU.not_equal, fill=1.0, base=bs,
                                                                      channel_multiplier=1), reads=[b_perm], writes=[b_perm])
                    k.op("pool", lambda e: e.iota(colf[:, 0:1], pattern=[[0, 1]], base=0, channel_multiplier=1,
                                                  allow_small_or_imprecise_dtypes=True), writes=[b_colf])
                    ts("dve", colf[:, 3:4], colf[:, 0:1], 64.0, None, ALU.is_ge, None, [b_colf], [b_colf])
                    stt("dve", colf[:, 1:2], colf[:, 3:4], -64.0, colf[:, 0:1], ALU.mult, ALU.add, [b_colf], [b_colf])
                    actf(colf[:, 2:3], colf[:, 1:2], AF.Exp, [b_colf], [b_colf], scale=-math.log(10000.0) / 64.0)
                    ts("dve", colf[:, 3:4], colf[:, 3:4], 2.0, -1.0, ALU.mult, ALU.add, [b_colf], [b_colf])
                    k.op("pool", lambda e: e.iota(bufB[:], pattern=[[1, S]], base=0, channel_multiplier=0,
                                                  allow_small_or_imprecise_dtypes=True), writes=[bB])
                    ts("dve", bufB[:], bufB[:], colf[:, 2:3], None, ALU.mult, None, [bB, b_colf], [bB])
                    TWO_PI = 2.0 * math.pi
                    MAGIC = 12582912.0

                    def sin_table(dst, shift):
                        ts("dve", dst[:], bufB[:], shift, 1.0 / TWO_PI, ALU.add, ALU.mult, [bB], [b_tab])
                        ts("dve", dst[:], dst[:], MAGIC, None, ALU.add, None, [b_tab], [b_tab])
                        ts("dve", dst[:], dst[:], -MAGIC, -TWO_PI, ALU.add, ALU.mult, [b_tab], [b_tab])
                        stt("dve", dst[:], bufB[:], shift, dst[:], ALU.add, ALU.add, [bB, b_tab], [b_tab])
                        ts("dve", dst[:], dst[:], -3.1415925, 3.1415925, ALU.max, ALU.min, [b_tab], [b_tab])
                        actf(dst[:], dst[:], AF.Sin, [b_tab], [b_tab])
                    sin_table(sint, 0.0)
                    ts("dve", sint[:], sint[:], colf[:, 3:4], None, ALU.mult, None, [b_tab, b_colf], [b_tab])
                    sin_table(cost, math.pi / 2.0)
                    tmpf = bufA
                    k.barrier()
                    bBq = [k.buf() for _ in range(4)]
                    bAq = [k.buf() for _ in range(4)]
                    for which, (off, dstT) in enumerate(((OFF_Q, qT_s), (OFF_K, kT_s))):
                        nheads = 24 if (ALLP or "A_qk_full" in phases) else 2
                        for hd in range(nheads):
                            dd = DIL[hd // 8]
                            bufC, bC, bufD, bD = rotC()
                            wt, bw = load_w(off + hd * 128, 128)
                            res = gemm_fm(wt, bw)
                            for tb, (pb, pbb) in enumerate(res):
                                sl = slice(tb * 512, (tb + 1) * 512)
                                cp(evac_eng(), bufB[:, sl], pb[:, :], [pbb], [bBq[tb]])
                            for tb in range(4):
                                sl = slice(tb * 512, (tb + 1) * 512)
                                pb, pbb = next_bank()
                                mm(pb[:, :], perm[:], bufB[:, sl], True, True, [bBq[tb], b_perm], [pbb])
                                tt("dve", tmpf[:, sl], pb[:, :], sint[:, sl], ALU.mult, [pbb, b_tab], [bAq[tb]])
                                tt("pool", bufB[:, sl], bufB[:, sl], cost[:, sl], ALU.mult, [bBq[tb], b_tab, pbb], [bBq[tb]])
                                tt("dve", bufB[:, sl], bufB[:, sl], tmpf[:, sl], ALU.add, [bBq[tb], bAq[tb]], [bBq[tb]])
                                if dd == 1:
                                    cp("act", bufC[:, sl], bufB[:, sl], [bBq[tb]], [bC])
                                else:
                                    n_i = 512 // dd
                                    o_ap = bufC[:, :].rearrange("e (r i) -> e r i", r=dd)[:, :, tb * n_i:(tb + 1) * n_i]
                                    i_ap = bufB[:, sl].rearrange("e (i r) -> e r i", r=dd)
                                    cp("act", o_ap, i_ap, [bBq[tb]], [bC])
                            k.dma("sp", dstT[hd * 128:(hd + 1) * 128, :], bufC[:], reads=[bC], writes=[k.buf()])
                k.barrier()
        if ALLP or "pB" in phases:
            with ExitStack() as sB:
                def sbB(name, shape, dt):
                    return sB.enter_context(nc.sbuf_tensor(name, list(shape), dt))
                triI = sbB("triI", [128, 128], F32)
                triE = sbB("triE", [128, 128], F32)
                onesf = sbB("onesf", [128, 128], F32)
                b_tri = k.buf()
                k.op("pool", lambda e: e.memset(onesf[:], 1.0), writes=[b_tri])
                k.op("pool", lambda e: e.memset(triI[:], 1.0), writes=[b_tri])
                k.op("pool", lambda e: e.affine_select(out=triI[:], in_=triI[:], pattern=[[1, 128]], compare_op=ALU.is_ge,
                                                       fill=0.0, base=0, channel_multiplier=-1), reads=[b_tri], writes=[b_tri])
                k.op("pool", lambda e: e.memset(triE[:], 1.0), writes=[b_tri])
                k.op("pool", lambda e: e.affine_select(out=triE[:], in_=triE[:], pattern=[[1, 128]], compare_op=ALU.is_ge,
                                                       fill=0.0, base=-1, channel_multiplier=-1), reads=[b_tri], writes=[b_tri])
                dtx = sbB("dtx", [128, NT, 64], F32)
                dtv = sbB("dtv", [128, NT, 64], F32)
                da = sbB("da", [128, NT, 64], F32)
                nb_ = sbB("nbias", [128, NT, 64], F32)
                brep = sbB("brep", [128, 64], F32)
                arep = sbB("arep", [128, 64], F32)
                dsk = sbB("dsk", [128, 32], F32)
                b_dtx, b_dtv, b_da, b_nb, b_brep, b_arep, b_dsk = (k.buf() for _ in range(7))
                k.dma("sp", dtx[:], dt_s.rearrange("(j p) n -> p j n", p=128), writes=[b_dtx])
                k.dma("sp", brep[:, 0:32], I("dt_bias_f").partition_broadcast(128), writes=[b_brep])
                k.dma("sp", brep[:, 32:64], I("dt_bias_b").partition_broadcast(128), writes=[b_brep])
                k.dma("sp", arep[:, 0:32], I("a_log_f").partition_broadcast(128), writes=[b_arep])
                k.dma("sp", arep[:, 32:64], I("a_log_b").partition_broadcast(128), writes=[b_arep])
                k.dma("sp", dsk[:], I("d_skip").partition_broadcast(128), writes=[b_dsk])
                actf(arep[:], arep[:], AF.Exp, [b_arep], [b_arep])
                ts("dve", arep[:], arep[:], -1.0, None, ALU.mult, None, [b_arep], [b_arep])
                for j in range(NT):
                    tt("dve", dtx[:, j, :], dtx[:, j, :], brep[:], ALU.add, [b_dtx, b_brep], [b_dtx])
                dtxf = dtx[:, :, :].rearrange("p a b -> p (a b)")
                dtvf = dtv[:, :, :].rearrange("p a b -> p (a b)")
                stt("dve", dtvf, dtxf, -1.0, dtxf, ALU.mult, ALU.max, [b_dtx], [b_dtv])
                actf(dtvf, dtvf, AF.Exp, [b_dtv], [b_dtv], scale=-1.0)
                ts("dve", dtvf, dtvf, 1.0, None, ALU.add, None, [b_dtv], [b_dtv])
                actf(dtvf, dtvf, AF.Ln, [b_dtv], [b_dtv])
                stt("dve", dtvf, dtxf, 0.0, dtvf, ALU.max, ALU.add, [b_dtx, b_dtv], [b_dtv])
                for j in range(NT):
                    tt("dve", da[:, j, :], dtv[:, j, :], arep[:], ALU.mult, [b_dtv, b_arep], [b_da])
                for half, tri in ((0, triI), (1, triE)):
                    for i in range(NT):
                        pbk, pbkb = psum_banks[i // 8], pbuf[i // 8]
                        o0 = (i % 8) * 64 + half * 32
                        for j in range(i + 1):
                            mm(pbk[:, o0:o0 + 32], (tri if j == i else onesf)[:], da[:, j, half * 32:half * 32 + 32],
                               j == 0, j == i, [b_da, b_tri], [pbkb], inc=(half == 1 and i % 8 == 7 and j == i))
                for b2 in range(2):
                    pv = psum_banks[b2][:, :].rearrange("p (a b) -> p a b", b=64)
                    ts("dve", nb_[:, b2 * 8:(b2 + 1) * 8, 0:32], pv[:, :, 0:32], -1.0, None, ALU.mult, None, [pbuf[b2]], [b_nb])
                    cp("act", nb_[:, b2 * 8:(b2 + 1) * 8, 32:64], pv[:, :, 32:64], [pbuf[b2]], [b_nb])
                lndt = sbB("lndt", [128, NT, 64], F32)
                b_lndt = k.buf()
                actf(lndt[:, :, :].rearrange("p a b -> p (a b)"), dtvf, AF.Ln, [b_dtv], [b_lndt])
                nbf = nb_[:, :, :].rearrange("p a b -> p (a b)")
                tt("dve", nbf, nbf, lndt[:, :, :].rearrange("p a b -> p (a b)"), ALU.add, [b_nb, b_lndt], [b_nb])
                csr = sbB("csr", [32, S], F32)
                b_csr = k.buf()
                for half, tri in ((0, triI), (1, triE)):
                    for i in range(NT):
                        bi = 2 + i // 4
                        pbk, pbkb = psum_banks[bi], pbuf[bi]
                        o0 = (i % 4) * 128
                        for j in range(i + 1):
                            mm(pbk[0:32, o0:o0 + 128], da[:, j, half * 32:half * 32 + 32], (tri if j == i else onesf)[:],
                               j == 0, j == i, [b_da, b_tri], [pbkb], inc=(i % 4 == 3 and j == i))
                    for q4 in range(4):
                        cp(evac_eng(), csr[:, q4 * 512:(q4 + 1) * 512], psum_banks[2 + q4][0:32, :], [pbuf[2 + q4]], [b_csr])
                    k.dma("sp", csT_s[half * 32:(half + 1) * 32, :], csr[:], reads=[b_csr], writes=[k.buf()])
                k.barrier()
                if "nb_s" in debug:
                    nb_s = scr("nb_s", [128, NT * 64], F32)
                    dtv_s = scr("dtv_s", [128, NT * 64], F32)
                    k.dma("sp", nb_s[:, :], nb_[:, :, :].rearrange("p a b -> p (a b)"), reads=[b_nb], writes=[k.buf()])
                    k.dma("sp", dtv_s[:, :], dtvf, reads=[b_dtv], writes=[k.buf()])

                GT = sbB("GT", [128, NT, S], BF16)
                BTt = sbB("BTt", [128, S], BF16)
                CTt = sbB("CTt", [128, S], BF16)
                csreps = [[sbB("csrep%d_%d" % (a, i), [128, S], F32) for i in range(2)] for a in range(2)]
                Xhs = [sbB("Xh%d" % a, [128, NT, 64], BF16) for a in range(2)]
                xsThs = [sbB("xsTh%d" % a, [64, S], BF16) for a in range(2)]
                zThs = [sbB("zTh%d" % a, [64, S], BF16) for a in range(2)]
                Eb = [sbB("Eb%d" % i, [128, 512], BF16) for i in range(3)]
                Mb = [sbB("Mb%d" % i, [128, 512], BF16) for i in range(3)]
                ytmp = sbB("ytmp", [64, 512], F32)
                yout = sbB("yout", [64, S], BF16)
                b_GT = [k.buf() for _ in range(NT)]
                b_BT, b_CT, b_ytmp, b_yout = (k.buf() for _ in range(4))
                b_Xs, b_xsTs, b_zTs = ([k.buf(), k.buf()] for _ in range(3))
                b_csreps = [[k.buf(), k.buf()], [k.buf(), k.buf()]]
                b_E = [k.buf() for _ in range(3)]
                b_M = [k.buf() for _ in range(3)]
                sctr = 0
                ngroups = 8 if (ALLP or "B_full" in phases) else 1
                for g in range(ngroups):
                    k.dma("sp", BTt[:], BT_s[g * 128:(g + 1) * 128, :], writes=[b_BT])
                    k.dma("sp", CTt[:], CT_s[g * 128:(g + 1) * 128, :], writes=[b_CT])
                    for j in range(NT):
                        for tb in range(4):
                            pb, pbb = next_bank(0, 4)
                            mm(pb[:, :], BTt[:, j * 128:(j + 1) * 128], CTt[:, tb * 512:(tb + 1) * 512], True, True,
                               [b_BT, b_CT], [pbb])
                            cp(evac_eng(), GT[:, j, tb * 512:(tb + 1) * 512], pb[:, :], [pbb], [b_GT[j]])
                    for kh in range(4):
                        h = g * 4 + kh
                        csrep, b_csrep = csreps[h % 2], b_csreps[h % 2]
                        Xh, b_X = Xhs[h % 2], b_Xs[h % 2]
                        xsTh, b_xsT = xsThs[h % 2], b_xsTs[h % 2]
                        zTh, b_zT = zThs[h % 2], b_zTs[h % 2]
                        k.dma("sp", csrep[0][:], csT_s[h:h + 1, :].partition_broadcast(128), writes=[b_csrep[0]])
                        k.dma("sp", csrep[1][:], csT_s[32 + h:33 + h, :].partition_broadcast(128), writes=[b_csrep[1]])
                        k.dma("sp", Xh[:], xs_s.rearrange("(j p) n -> p j n", p=128)[:, :, h * 64:(h + 1) * 64], writes=[b_X])
                        k.dma("sp", xsTh[:], xsT_s[h * 64:(h + 1) * 64, :], writes=[b_xsT])
                        k.dma("sp", zTh[:], zT_s[h * 64:(h + 1) * 64, :], writes=[b_zT])
                        for tb in range(4):
                            py, pyb = psum_banks[4 + tb % 2], pbuf[4 + tb % 2]
                            segs = []
                            for j in range(NT):
                                if j < 4 * tb:
                                    segs.append((j, 0, tb * 512, (tb + 1) * 512, False))
                                elif j > 4 * tb + 3:
                                    segs.append((j, 1, tb * 512, (tb + 1) * 512, False))
                                else:
                                    segs.append((j, 0, j * 128, (tb + 1) * 512, True))
                                    segs.append((j, 1, tb * 512, (j + 1) * 128, True))
                            for si, (j, dr, t0, t1, diag) in enumerate(segs):
                                w = t1 - t0
                                E_, bE = Eb[sctr % 3], b_E[sctr % 3]
                                M_, bM = Mb[sctr % 3], b_M[sctr % 3]
                                sctr += 1
                                actf(E_[:, 0:w], csrep[dr][:, t0:t1], AF.Exp, [b_csrep[dr], b_nb], [bE],
                                     bias=nb_[:, j, dr * 32 + h:dr * 32 + h + 1], scale=(1.0 if dr == 0 else -1.0))
                                if diag:
                                    if dr == 0:
                                        k.op("pool", lambda e, E_=E_: e.affine_select(
                                            out=E_[:, 0:128], in_=E_[:, 0:128], pattern=[[1, 128]], compare_op=ALU.is_ge,
                                            fill=0.0, base=0, channel_multiplier=-1), reads=[bE], writes=[bE])
                                    else:
                                        k.op("pool", lambda e, E_=E_, w=w: e.affine_select(
                                            out=E_[:, w - 128:w], in_=E_[:, w - 128:w], pattern=[[-1, 128]], compare_op=ALU.is_ge,
                                            fill=0.0, base=0, channel_multiplier=1), reads=[bE], writes=[bE])
                                tt("pool" if (sctr % 3 == 0 and not diag) else "dve", M_[:, 0:w], E_[:, 0:w], GT[:, j, t0:t1],
                                   ALU.mult, [bE, b_GT[j]], [bM])
                                mm(py[0:64, t0 - tb * 512:t1 - tb * 512], Xh[:, j, :], M_[:, 0:w], si == 0, si == len(segs) - 1,
                                   [b_X, bM], [pyb], inc=True)
                            sl = slice(tb * 512, (tb + 1) * 512)
                            stt("dve", ytmp[:, :], xsTh[:, sl], dsk[0:64, h:h + 1], py[0:64, :], ALU.mult, ALU.add,
                                [b_xsT, b_dsk, pyb], [b_ytmp])
                            tt("pool", yout[:, sl], ytmp[:, :], zTh[:, sl], ALU.mult, [b_ytmp, b_zT], [b_yout])
                        k.dma("pool", ygT_s[h * 64:(h + 1) * 64, :], yout[:], reads=[b_yout], writes=[k.buf()])
                k.barrier()
        if ALLP or "pC" in phases:
            with ExitStack() as sC:
                def sbC(name, shape, dt):
                    return sC.enter_context(nc.sbuf_tensor(name, list(shape), dt))
                maskB = sbC("maskB", [128, 384], BF16)
                onesb = sbC("onesb128", [128, 128], BF16)
                b_mask = k.buf()
                k.op("pool", lambda e: e.memset(onesb[:], 1.0), writes=[b_mask])
                k.op("pool", lambda e: e.memset(maskB[:], 1.0), writes=[b_mask])
                k.op("pool", lambda e: e.affine_select(out=maskB[:], in_=maskB[:], pattern=[[1, 384]], compare_op=ALU.is_ge,
                                                       fill=0.0, base=-64, channel_multiplier=-1), reads=[b_mask], writes=[b_mask])
                k.op("pool", lambda e: e.affine_select(out=maskB[:], in_=maskB[:], pattern=[[-1, 384]], compare_op=ALU.is_ge,
                                                       fill=0.0, base=192, channel_multiplier=1), reads=[b_mask], writes=[b_mask])
                Oacc = sbC("Oacc", [128, S], F32)
                Zacc = sbC("Zacc", [128, S], F32)
                qTh = sbC("qTh", [128, S], BF16)
                kTh = sbC("kTh", [128, S], BF16)
                vt = sbC("vt", [128, NT, 128], BF16)
                PBt = [sbC("PBt%d" % i, [128, 8, 384], BF16) for i in range(2)]
                oTo = sbC("oTo", [128, S], BF16)
                b_O, b_Z, b_q, b_k, b_v, b_oT = (k.buf() for _ in range(6))
                b_PB = [[k.buf() for _ in range(8)] for _ in range(2)]
                pctr2 = 0
                sm_scale = 1.0 / math.sqrt(128.0)
                nslots = 8 if (ALLP or "C_full" in phases) else 1
                for hs in range(nslots):
                    for g in range(3):
                        hd = g * 8 + hs
                        d = DIL[g]
                        L = S // d
                        nt = L // 128
                        k.dma("sp", qTh[:], qT_s[hd * 128:(hd + 1) * 128, :], writes=[b_q])
                        k.dma("sp", kTh[:], kT_s[hd * 128:(hd + 1) * 128, :], writes=[b_k])
                        vview = v_s.rearrange("(j kk r) n -> r kk j n", kk=128, r=d)
                        for r in range(d):
                            k.dma("act" if r % 2 else "sp", vt[:, r * nt:(r + 1) * nt, :], vview[r][:, :, hd * 128:(hd + 1) * 128],
                                  writes=[b_v])
                        for c0 in range(0, S, 512):
                            pO, pOb = psum_banks[4], pbuf[4]
                            pZ, pZb = psum_banks[5], pbuf[5]
                            tiles = []
                            for qi in range(4):
                                col = c0 + qi * 128
                                tiles.append((qi, col // L, (col % L) // 128))
                            PB_, bPB = PBt[pctr2 % 2], b_PB[pctr2 % 2]
                            pctr2 += 1
                            slot = 0
                            for r in sorted(set(t_[1] for t_ in tiles)):
                                tl = [t_ for t_ in tiles if t_[1] == r]
                                ilo, ihi = tl[0][2], tl[-1][2]
                                qi0 = tl[0][0]
                                jlo, jhi = max(ilo - 1, 0), min(ihi + 1, nt - 1)
                                info = {}
                                for j in range(jlo, jhi + 1):
                                    a_ = max(j - 1, ilo)
                                    b_ = min(j + 1, ihi)
                                    w = (b_ - a_ + 1) * 128
                                    ps_, psb = next_bank(0, 4)
                                    qc0 = r * L + a_ * 128
                                    mm(ps_[:, 0:w], kTh[:, r * L + j * 128:r * L + (j + 1) * 128], qTh[:, qc0:qc0 + w], True, True,
                                       [b_q, b_k], [psb])
                                    actf(PB_[:, slot, 0:w], ps_[:, 0:w], AF.Exp, [psb], [bPB[slot]], scale=sm_scale)
                                    off = 128 + (a_ - j) * 128
                                    tt("dve", PB_[:, slot, 0:w], PB_[:, slot, 0:w], maskB[:, off:off + w], ALU.mult, [bPB[slot], b_mask], [bPB[slot]])
                                    info[j] = (slot, a_)
                                    slot += 1
                                for i in range(ilo, ihi + 1):
                                    qi = qi0 + (i - ilo)
                                    cj = list(range(max(i - 1, 0), min(i + 1, nt - 1) + 1))
                                    for n, j in enumerate(cj):
                                        sl_, a_ = info[j]
                                        pc = (i - a_) * 128
                                        mm(pO[:, qi * 128:(qi + 1) * 128], vt[:, r * nt + j, :], PB_[:, sl_, pc:pc + 128], n == 0, n == len(cj) - 1,
                                           [b_v, bPB[sl_]], [pOb], inc=False)
                                    for n, j in enumerate(cj):
                                        sl_, a_ = info[j]
                                        pc = (i - a_) * 128
                                        mm(pZ[:, qi * 128:(qi + 1) * 128], onesb[:], PB_[:, sl_, pc:pc + 128], n == 0, n == len(cj) - 1,
                                           [b_mask, bPB[sl_]], [pZb], inc=(n == len(cj) - 1))
                            if d == 1:
                                ovO, ovZ = Oacc[:, c0:c0 + 512], Zacc[:, c0:c0 + 512]
                                pvO, pvZ = pO[:, :], pZ[:, :]
                            elif d == 4:
                                r = c0 // 512
                                ovO = Oacc[:, :].rearrange("e (i r) -> e r i", r=4)[:, r, :]
                                ovZ = Zacc[:, :].rearrange("e (i r) -> e r i", r=4)[:, r, :]
                                pvO, pvZ = pO[:, :], pZ[:, :]
                            else:
                                r0 = c0 // 128
                                ovO = Oacc[:, :].rearrange("e (i r) -> e r i", r=16)[:, r0:r0 + 4, :]
                                ovZ = Zacc[:, :].rearrange("e (i r) -> e r i", r=16)[:, r0:r0 + 4, :]
                                pvO = pO[:, :].rearrange("e (r i) -> e r i", r=4)
                                pvZ = pZ[:, :].rearrange("e (r i) -> e r i", r=4)
                            if g == 0:
                                cp("act", ovO, pvO, [pOb], [b_O])
                                cp("dve", ovZ, pvZ, [pZb], [b_Z])
                            else:
                                tt("dve", ovO, ovO, pvO, ALU.add, [pOb, b_O], [b_O])
                                tt("dve", ovZ, ovZ, pvZ, ALU.add, [pZb, b_Z], [b_Z])
                    k.op("dve", lambda e: e.reciprocal(out=Zacc[:], in_=Zacc[:]), reads=[b_Z], writes=[b_Z])
                    tt("dve", oTo[:], Oacc[:], Zacc[:], ALU.mult, [b_O, b_Z], [b_oT])
                    k.dma("sp", oT_s[hs * 128:(hs + 1) * 128, :], oTo[:], reads=[b_oT], writes=[k.buf()])
                k.barrier()
        if ALLP or "pD" in phases:
            with ExitStack() as sD:
                def sbD(name, shape, dt):
                    return sD.enter_context(nc.sbuf_tensor(name, list(shape), dt))
                yg = sbD("yg", [128, 16, S], BF16)
                oTt = sbD("oTt", [128, 8, S], BF16)
                b_yg, b_oTt = k.buf(), k.buf()
                k.dma("sp", yg[:], ygT_s.rearrange("(c p) t -> p c t", p=128), writes=[b_yg])
                k.dma("act", oTt[:], oT_s.rearrange("(c p) t -> p c t", p=128), writes=[b_oTt])
                wncol = sbD("wncol", [128, 16], F32)
                b_wn = row2col(I("ssm_norm_w")[:, :], 16, wncol[:], "wn")
                onesb = sbD("onesbD", [128, 128], BF16)
                b_ones = k.buf()
                k.op("pool", lambda e: e.memset(onesb[:], 1.0), writes=[b_ones])
                sq = [sbD("sq%d" % i, [128, S], BF16) for i in range(2)]
                b_sq = [k.buf(), k.buf()]
                rrep = sbD("rrep", [128, S], F32)
                b_rrep = k.buf()
                for c in range(16):
                    tt("pool" if c % 2 else "dve", sq[c % 2][:], yg[:, c, :], yg[:, c, :], ALU.mult, [b_yg], [b_sq[c % 2]])
                    for tb in range(4):
                        mm(psum_banks[tb][:, :], onesb[:], sq[c % 2][:, tb * 512:(tb + 1) * 512], c == 0, c == 15,
                           [b_ones, b_sq[c % 2]], [pbuf[tb]], inc=True)
                for tb in range(4):
                    ts("dve", rrep[:, tb * 512:(tb + 1) * 512], psum_banks[tb][:, :], 1.0 / 2048.0, 1e-6, ALU.mult, ALU.add,
                       [pbuf[tb]], [b_rrep])
                k.op("act", lambda e: e.sqrt(out=rrep[:], in_=rrep[:]), reads=[b_rrep], writes=[b_rrep])
                k.op("dve", lambda e: e.reciprocal(out=rrep[:], in_=rrep[:]), reads=[b_rrep], writes=[b_rrep])
                for c in range(16):
                    stt("dve", yg[:, c, :], yg[:, c, :], wncol[:, c:c + 1], rrep[:], ALU.mult, ALU.mult,
                        [b_yg, b_wn, b_rrep], [b_yg])
                if "ynT_s" in debug:
                    ynT_s = scr("ynT_s", [2048, S], BF16)
                    k.dma("sp", ynT_s.rearrange("(c p) t -> p c t", p=128), yg[:], reads=[b_yg], writes=[k.buf()])
                wso = [sbD("wso%d" % i, [128, 16, 128], BF16) for i in range(3)]
                wao = [sbD("wao%d" % i, [128, 8, 128], BF16) for i in range(3)]
                sg1 = [sbD("sg1%d" % i, [128, S], BF16) for i in range(2)]
                sg2 = [sbD("sg2%d" % i, [128, S], BF16) for i in range(2)]
                b_wso, b_wao, b_sg1, b_sg2 = ([k.buf(), k.buf(), k.buf()] for _ in range(4))
                t1 = [sbD("t1%d" % i, [128, 512], F32) for i in range(2)]
                t2 = [sbD("t2%d" % i, [128, 512], F32) for i in range(2)]
                b_t1, b_t2 = [k.buf(), k.buf()], [k.buf(), k.buf()]
                mrg = [sbD("mrg%d" % i, [128, S], BF16) for i in range(2)]
                b_mrg = [k.buf(), k.buf()]
                wso_v = I("w_ssm_out").rearrange("(c p) n -> p c n", p=128)
                wao_v = I("w_attn_out").rearrange("(c p) n -> p c n", p=128)
                ndc = NCH if (ALLP or "D_full" in phases) else 2
                cnt = 0
                for dc in range(ndc):
                    i2 = dc % 2
                    i3 = dc % 3
                    k.dma("pool", wso[i3][:], wso_v[:, :, dc * 128:(dc + 1) * 128], writes=[b_wso[i3]])
                    k.dma("pool", wao[i3][:], wao_v[:, :, dc * 128:(dc + 1) * 128], writes=[b_wao[i3]])
                    k.dma("sp", sg1[i2][:], sgT_s[dc * 128:(dc + 1) * 128, :], writes=[b_sg1[i2]])
                    k.dma("sp", sg2[i2][:], sgT_s[D + dc * 128:D + (dc + 1) * 128, :], writes=[b_sg2[i2]])
                    for tb in range(4):
                        sl = slice(tb * 512, (tb + 1) * 512)
                        p1, p1b = next_bank()
                        for c in range(16):
                            mm(p1[:, :], wso[i3][:, c, :], yg[:, c, sl], c == 0, c == 15, [b_wso[i3], b_yg], [p1b], inc=(c == 15))
                        p2, p2b = next_bank()
                        for c in range(8):
                            mm(p2[:, :], wao[i3][:, c, :], oTt[:, c, sl], c == 0, c == 7, [b_wao[i3], b_oTt], [p2b], inc=(c == 7))
                        j2 = cnt % 2
                        cnt += 1
                        tt("dve", t1[j2][:], p1[:, :], sg1[i2][:, sl], ALU.mult, [p1b, b_sg1[i2]], [b_t1[j2]])
                        tt("dve", t2[j2][:], p2[:, :], sg2[i2][:, sl], ALU.mult, [p2b, b_sg2[i2]], [b_t2[j2]])
                        tt("pool", mrg[i2][:, sl], t1[j2][:], t2[j2][:], ALU.add, [b_t1[j2], b_t2[j2]], [b_mrg[i2]])
                    k.dma("act", mT_s[dc * 128:(dc + 1) * 128, :], mrg[i2][:], reads=[b_mrg[i2]], writes=[k.buf()])
                k.barrier()
            with ExitStack() as sD:
                def sbD(name, shape, dt):
                    return sD.enter_context(nc.sbuf_tensor(name, list(shape), dt))
                mT = sbD("mT", [128, NCH, S], BF16)
                b_mT = k.buf()
                mT_v = mT_s.rearrange("(c p) t -> p c t", p=128)
                for q4 in range(4):
                    k.dma("sp" if q4 % 2 else "act", mT[:, q4 * 8:(q4 + 1) * 8, :], mT_v[:, q4 * 8:(q4 + 1) * 8, :], writes=[b_mT])
                g1rep = sbD("g1rep", [128, D], F32)
                b_g1 = k.buf()
                k.dma("sp", g1rep[:], modrow_s.rearrange("(m c) p -> m (c p)", m=6)[2:3, :].partition_broadcast(128), writes=[b_g1])
                WB = 256
                wo = [sbD("wo%d" % i, [128, NCH, WB], BF16) for i in range(2)]
                b_wo = [k.buf(), k.buf()]
                xt_ = [sbD("xtD%d" % i, [128, WB], F32) for i in range(3)]
                b_xt_ = [k.buf() for _ in range(3)]
                tmpD = [sbD("tmpD%d" % i, [128, WB], F32) for i in range(2)]
                b_tmpD = [k.buf(), k.buf()]
                wo_v = I("w_o").rearrange("(c p) n -> p c n", p=128)
                nnb = D // WB if (ALLP or "D_full" in phases) else 1
                cnt = 0
                for nb2 in range(nnb):
                    i2 = nb2 % 2
                    cs_ = slice(nb2 * WB, (nb2 + 1) * WB)
                    k.dma("pool", wo[i2][:], wo_v[:, :, cs_], writes=[b_wo[i2]])
                    for tt_ in range(NT):
                        i3 = cnt % 3
                        j2 = cnt % 2
                        cnt += 1
                        k.dma("sp", xt_[i3][:], I("x")[tt_ * 128:(tt_ + 1) * 128, cs_], writes=[b_xt_[i3]])
                        pb, pbb = next_bank()
                        for c in range(NCH):
                            mm(pb[:, 0:WB], mT[:, c, tt_ * 128:(tt_ + 1) * 128], wo[i2][:, c, :], c == 0, c == NCH - 1,
                               [b_mT, b_wo[i2]], [pbb], inc=(c == NCH - 1))
                        tt("dve", tmpD[j2][:], pb[:, 0:WB], g1rep[:, cs_], ALU.mult, [pbb, b_g1], [b_tmpD[j2]])
                        tt("pool", xt_[i3][:], xt_[i3][:], tmpD[j2][:], ALU.add, [b_xt_[i3], b_tmpD[j2]], [b_xt_[i3]])
                        k.dma("act", x1_s[tt_ * 128:(tt_ + 1) * 128, cs_], xt_[i3][:], reads=[b_xt_[i3]], writes=[k.buf()])
                k.barrier()
        CAP = 256
        NE = 16
        if ALLP or "pE" in phases:
            with ExitStack() as sE0:
                def sbE(name, shape, dt):
                    return sE0.enter_context(nc.sbuf_tensor(name, list(shape), dt))
                afft = sbE("afft", [128, NT, NE], F32)
                msk = sbE("msk", [128, NT, NE], F32)
                rnk = sbE("rnk", [128, NT, NE], F32)
                wtm = sbE("wtm", [128, NT, NE], F32)
                rankT = sbE("rankT", [NE, S], F32)
                wT = sbE("wT", [NE, S], F32)
                b_aff, b_msk, b_rnk, b_wtm, b_rankT, b_wT = (k.buf() for _ in range(6))
                with ExitStack() as sE1:
                    def sb1(name, shape, dt):
                        return sE1.enter_context(nc.sbuf_tensor(name, list(shape), dt))
                    h2T = sb1("h2T", [128, NCH, S], BF16)
                    b_h2T = [k.buf() for _ in range(NT)]
                    with ExitStack() as sE2:
                        norm_to_hT(sE2, x1_s, n2col, 3, 4, h2T, b_h2T, "e")
                        k.barrier()
                    if "h2T_s" in debug:
                        h2T_s = scr("h2T_s", [D, S], BF16)
                        k.dma("sp", h2T_s.rearrange("(c p) t -> p c t", p=128), h2T[:], reads=b_h2T, writes=[k.buf()])
                    wr = sb1("wr", [128, NCH, NE], BF16)
                    b_wr = k.buf()
                    k.dma("pool", wr[:], I("w_router").rearrange("(c p) n -> p c n", p=128), writes=[b_wr])
                    lg = sb1("lg", [128, NT, NE], F32)
                    b_lg = k.buf()
                    for tt_ in range(NT):
                        pb, pbb = next_bank()
                        for c in range(NCH):
                            mm(pb[:, 0:NE], h2T[:, c, tt_ * 128:(tt_ + 1) * 128], wr[:, c, :], c == 0, c == NCH - 1,
                               [b_wr, b_h2T[tt_]], [pbb], inc=(c == NCH - 1))
                        cp("dve", lg[:, tt_, :], pb[:, 0:NE], [pbb], [b_lg])
                    mx = sb1("mx", [128, NT], F32)
                    sm = sb1("sm", [128, NT], F32)
                    b_mx = k.buf()
                    k.op("dve", lambda e: e.tensor_reduce(out=mx[:], in_=lg[:], axis=AX.X, op=ALU.max), reads=[b_lg], writes=[b_mx])
                    ts("dve", mx[:], mx[:], -1.0, None, ALU.mult, None, [b_mx], [b_mx])
                    for tt_ in range(NT):
                        actf(afft[:, tt_, :], lg[:, tt_, :], AF.Exp, [b_lg, b_mx], [b_aff], bias=mx[:, tt_:tt_ + 1], scale=1.0)
                    k.op("dve", lambda e: e.tensor_reduce(out=sm[:], in_=afft[:], axis=AX.X, op=ALU.add), reads=[b_aff], writes=[b_mx])
                    k.op("dve", lambda e: e.reciprocal(out=sm[:], in_=sm[:]), reads=[b_mx], writes=[b_mx])
                    for tt_ in range(NT):
                        ts("dve", afft[:, tt_, :], afft[:, tt_, :], sm[:, tt_:tt_ + 1], None, ALU.mult, None, [b_aff, b_mx], [b_aff])
                    h2st = [sb1("h2st%d" % i, [128, D], BF16) for i in range(2)]
                    b_h2st = [k.buf(), k.buf()]
                    for tt_ in range(NT):
                        st_, bst = h2st[tt_ % 2], b_h2st[tt_ % 2]
                        for q8 in range(4):
                            pt, ptbf = next_tbank()
                            for j in range(8):
                                c = q8 * 8 + j
                                k.op("pe", lambda e, pt=pt, j=j, c=c, tt_=tt_: e.transpose(
                                    pt[:, j * 128:(j + 1) * 128], h2T[:, c, tt_ * 128:(tt_ + 1) * 128], identb[:]),
                                    reads=[b_h2T[tt_], b_identb], writes=[ptbf], inc=(j == 7))
                            cp(evac_eng(), st_[:, q8 * 1024:(q8 + 1) * 1024], pt[:, :], [ptbf], [bst])
                        k.dma("sp", h2_s[tt_ * 128:(tt_ + 1) * 128, :], st_[:], reads=[bst], writes=[k.buf()])
                    affT = sb1("affT", [NE, S], F32)
                    b_affT = k.buf()
                    for q4 in range(4):
                        pb, pbb = next_bank()
                        for j in range(4):
                            tt_ = q4 * 4 + j
                            k.op("pe", lambda e, pb=pb, j=j, tt_=tt_: e.transpose(
                                pb[0:NE, j * 128:(j + 1) * 128], afft[:, tt_, :], ident[:]),
                                reads=[b_aff, b_ident], writes=[pbb], inc=(j == 3))
                        cp("dve", affT[:, q4 * 512:(q4 + 1) * 512], pb[0:NE, :], [pbb], [b_affT])
                    lo = sb1("lo", [NE, 1], F32)
                    mid = sb1("mid", [NE, 1], F32)
                    cntc = sb1("cntc", [NE, 1], F32)
                    gec = sb1("gec", [NE, 1], F32)
                    cmpj = sb1("cmpj", [NE, S], F32)
                    b_lo, b_mid, b_cnt, b_ge, b_cmp = (k.buf() for _ in range(5))
                    k.op("dve", lambda e: e.memset(lo[:], 0.0), writes=[b_lo])
                    for it in range(1, 31):
                        wdt = 2.0 ** (-it)
                        ts("dve", mid[:], lo[:], wdt, None, ALU.add, None, [b_lo], [b_mid])
                        ts("dve", cmpj[:], affT[:], mid[:, 0:1], None, ALU.is_ge, None, [b_affT, b_mid], [b_cmp])
                        k.op("dve", lambda e: e.tensor_reduce(out=cntc[:], in_=cmpj[:], axis=AX.X, op=ALU.add),
                             reads=[b_cmp], writes=[b_cnt])
                        ts("dve", gec[:], cntc[:], float(CAP), None, ALU.is_ge, None, [b_cnt], [b_ge])
                        stt("dve", lo[:], gec[:], wdt, lo[:], ALU.mult, ALU.add, [b_ge, b_lo], [b_lo])
                    dg = sb1("dg", [NE, NE], F32)
                    ones16 = sb1("ones16", [NE, 128], F32)
                    threp = sb1("threp", [128, NE], F32)
                    b_dg, b_threp = k.buf(), k.buf()
                    k.op("pool", lambda e: e.memset(ones16[:], 1.0), writes=[b_dg])
                    ts("dve", dg[:], ident[0:NE, 0:NE], lo[:, 0:1], None, ALU.mult, None, [b_lo, b_ident], [b_dg])
                    pb, pbb = next_bank()
                    mm(pb[:, 0:NE], ones16[:], dg[:], True, True, [b_dg], [pbb])
                    cp("dve", threp[:], pb[:, 0:NE], [pbb], [b_threp])
                    for tt_ in range(NT):
                        tt("dve", msk[:, tt_, :], afft[:, tt_, :], threp[:], ALU.is_ge, [b_aff, b_threp], [b_msk])
                    mskf = msk[:, :, :].rearrange("p a b -> p (a b)")
                    tt("dve", wtm[:, :, :].rearrange("p a b -> p (a b)"), mskf, afft[:, :, :].rearrange("p a b -> p (a b)"),
                       ALU.mult, [b_msk, b_aff], [b_wtm])
                    triE2 = sb1("triE2", [128, 128], F32)
                    onesf2 = sb1("onesf2", [128, 128], F32)
                    b_tri2 = k.buf()
                    k.op("pool", lambda e: e.memset(onesf2[:], 1.0), writes=[b_tri2])
                    k.op("pool", lambda e: e.memset(triE2[:], 1.0), writes=[b_tri2])
                    k.op("pool", lambda e: e.affine_select(out=triE2[:], in_=triE2[:], pattern=[[1, 128]], compare_op=ALU.is_ge,
                                                           fill=0.0, base=-1, channel_multiplier=-1), reads=[b_tri2], writes=[b_tri2])
                    pbk, pbkb = next_bank()
                    for i in range(NT):
                        for j in range(i + 1):
                            mm(pbk[:, i * NE:(i + 1) * NE], (triE2 if j == i else onesf2)[:], msk[:, j, :], j == 0, j == i,
                               [b_msk, b_tri2], [pbkb], inc=(i == NT - 1 and j == i))
                    cp("dve", rnk[:, :, :].rearrange("p a b -> p (a b)"), pbk[:, 0:NT * NE], [pbkb], [b_rnk])
                    for src, srcb, dst, dstb in ((rnk, b_rnk, rankT, b_rankT), (wtm, b_wtm, wT, b_wT)):
                        for q4 in range(4):
                            pb, pbb = next_bank()
                            for j in range(4):
                                tt_ = q4 * 4 + j
                                k.op("pe", lambda e, pb=pb, j=j, tt_=tt_, src=src: e.transpose(
                                    pb[0:NE, j * 128:(j + 1) * 128], src[:, tt_, :], ident[:]),
                                    reads=[srcb, b_ident], writes=[pbb], inc=(j == 3))
                            cp("dve", dst[:, q4 * 512:(q4 + 1) * 512], pb[0:NE, :], [pbb], [dstb])
                    if "aff_s" in debug:
                        aff_s = scr("aff_s", [128, NT * NE], F32)
                        msk_s = scr("msk_s", [128, NT * NE], F32)
                        rnk_s = scr("rnk_s", [128, NT * NE], F32)
                        k.dma("sp", aff_s[:, :], afft[:, :, :].rearrange("p a b -> p (a b)"), reads=[b_aff], writes=[k.buf()])
                        k.dma("sp", msk_s[:, :], mskf, reads=[b_msk], writes=[k.buf()])
                        k.dma("sp", rnk_s[:, :], rnk[:, :, :].rearrange("p a b -> p (a b)"), reads=[b_rnk], writes=[k.buf()])
                    k.barrier()

                with ExitStack() as sE3:
                    def sb3(name, shape, dt):
                        return sE3.enter_context(nc.sbuf_tensor(name, list(shape), dt))
                    iot = sb3("iot", [128, CAP], F32)
                    b_iot = k.buf()
                    k.op("pool", lambda e: e.iota(iot[:], pattern=[[1, CAP]], base=0, channel_multiplier=0,
                                                  allow_small_or_imprecise_dtypes=True), writes=[b_iot])
                    Pe = [sb3("Pe%d" % i, [128, NT, CAP], BF16) for i in range(2)]
                    b_Pe = [k.buf(), k.buf()]
                    h2q = [sb3("h2q%d" % i, [128, NT, 512], BF16) for i in range(2)]
                    b_h2q = [k.buf(), k.buf()]
                    xeT = sb3("xeT", [128, NCH, CAP], BF16)
                    b_xeT = [k.buf() for _ in range(8)]
                    wg = [sb3("wg%d" % i, [128, NCH, 128], BF16) for i in range(2)]
                    wu = [sb3("wu%d" % i, [128, NCH, 128], BF16) for i in range(2)]
                    b_wg, b_wu = [k.buf(), k.buf()], [k.buf(), k.buf()]
                    sgt = [sb3("sgt%d" % i, [128, CAP], F32) for i in range(2)]
                    b_sgt = [k.buf(), k.buf()]
                    hid = sb3("hid", [128, 16, CAP], BF16)
                    b_hid = k.buf()
                    wd = [sb3("wd%d" % i, [128, 16, 512], BF16) for i in range(2)]
                    b_wd = [k.buf(), k.buf()]
                    ysb = [sb3("ysb%d" % i, [128, 512], BF16) for i in range(2)]
                    b_ysb = [k.buf(), k.buf()]
                    h2_v = h2_s.rearrange("(tt p) n -> p tt n", p=128)
                    nexp = NE if (ALLP or "E_full" in phases) else 1
                    qc = 0
                    fc = 0
                    dc_ = 0
                    for ex in range(nexp):
                        P_, bP = Pe[ex % 2], b_Pe[ex % 2]
                        for tt_ in range(NT):
                            ts("dve", P_[:, tt_, :], iot[:], rnk[:, tt_, ex:ex + 1], msk[:, tt_, ex:ex + 1], ALU.is_equal, ALU.mult,
                               [b_iot, b_rnk, b_msk], [bP])
                        for q in range(8):
                            hq, bhq = h2q[qc % 2], b_h2q[qc % 2]
                            qc += 1
                            k.dma("sp" if q % 2 else "act", hq[:], h2_v[:, :, q * 512:(q + 1) * 512], writes=[bhq])
                            for c2 in range(2):
                                pb, pbb = next_bank()
                                for hh in range(2):
                                    cl = c2 * 2 + hh
                                    for tt_ in range(NT):
                                        mm(pb[:, hh * CAP:(hh + 1) * CAP], hq[:, tt_, cl * 128:(cl + 1) * 128], P_[:, tt_, :],
                                           tt_ == 0, tt_ == NT - 1, [bhq, bP], [pbb], inc=(hh == 1 and tt_ == NT - 1))
                                c = q * 4 + c2 * 2
                                cp(evac_eng(), xeT[:, c:c + 2, :], pb[:, :].rearrange("p (a b) -> p a b", b=CAP), [pbb], [b_xeT[q]])
                        for fb in range(16):
                            wg_, bwg = wg[fc % 2], b_wg[fc % 2]
                            wu_, bwu = wu[fc % 2], b_wu[fc % 2]
                            sg_, bsg = sgt[fc % 2], b_sgt[fc % 2]
                            fc += 1
                            k.dma("pool", wg_[:], I("w_gate_e")[ex].rearrange("(c p) n -> p c n", p=128)[:, :, fb * 128:(fb + 1) * 128],
                                  writes=[bwg])
                            k.dma("pool", wu_[:], I("w_up_e")[ex].rearrange("(c p) n -> p c n", p=128)[:, :, fb * 128:(fb + 1) * 128],
                                  writes=[bwu])
                            pb, pbb = next_bank()
                            for c in range(NCH):
                                mm(pb[:, 0:CAP], wg_[:, c, :], xeT[:, c, :], c == 0, c == NCH - 1, [bwg] + b_xeT, [pbb], inc=False)
                            for c in range(NCH):
                                mm(pb[:, CAP:2 * CAP], wu_[:, c, :], xeT[:, c, :], c == 0, c == NCH - 1, [bwu] + b_xeT, [pbb],
                                   inc=(c == NCH - 1))
                            actf(sg_[:], pb[:, 0:CAP], AF.Silu, [pbb], [bsg])
                            tt("dve", hid[:, fb, :], sg_[:], pb[:, CAP:2 * CAP], ALU.mult, [bsg, pbb], [b_hid])
                        for nb2 in range(8):
                            wd_, bwd = wd[dc_ % 2], b_wd[dc_ % 2]
                            dc_ += 1
                            k.dma("pool", wd_[:], I("w_down_e")[ex].rearrange("(c p) n -> p c n", p=128)[:, :, nb2 * 512:(nb2 + 1) * 512],
                                  writes=[bwd])
                            for st2 in range(2):
                                pb, pbb = next_bank()
                                for fb in range(16):
                                    mm(pb[:, :], hid[:, fb, st2 * 128:(st2 + 1) * 128], wd_[:, fb, :], fb == 0, fb == 15,
                                       [b_hid, bwd], [pbb], inc=(fb == 15))
                                ys_, bys = ysb[st2], b_ysb[st2]
                                cp(evac_eng(), ys_[:], pb[:, :], [pbb], [bys])
                                k.dma("sp", y_s[ex * CAP + st2 * 128:ex * CAP + (st2 + 1) * 128, nb2 * 512:(nb2 + 1) * 512], ys_[:],
                                      reads=[bys], writes=[k.buf()])
                    k.barrier()

                with ExitStack() as sF:
                    def sbF(name, shape, dt):
                        return sF.enter_context(nc.sbuf_tensor(name, list(shape), dt))
                    TG = 256
                    sel = sbF("sel", [NE, NE, 128], F32)
                    b_sel = k.buf()
                    k.op("pool", lambda e: e.memset(sel[:], 0.0), writes=[b_sel])
                    k.op("pool", lambda e: e.affine_select(out=sel[:], in_=sel[:], pattern=[[-1, NE], [0, 128]],
                                                           compare_op=ALU.not_equal, fill=1.0, base=0, channel_multiplier=1),
                         reads=[b_sel], writes=[b_sel])
                    slotc = sbF("slotc", [128, 2], F32)
                    b_slotc = k.buf()
                    k.op("pool", lambda e: e.iota(slotc[:], pattern=[[128, 2]], base=0, channel_multiplier=1,
                                                  allow_small_or_imprecise_dtypes=True), writes=[b_slotc])
                    g2rep = sbF("g2rep", [128, D], F32)
                    nfrep = sbF("nfrep", [128, D], F32)
                    b_g2, b_nf = k.buf(), k.buf()
                    k.dma("sp", g2rep[:], modrow_s.rearrange("(m c) p -> m (c p)", m=6)[5:6, :].partition_broadcast(128), writes=[b_g2])
                    k.dma("act", nfrep[:], I("normf_w").rearrange("c p -> (c p)").partition_broadcast(128), writes=[b_nf])
                    PTs = [sbF("PT%d" % i, [128, 2 * NE, TG], BF16) for i in range(2)]
                    b_PTs = [k.buf(), k.buf()]
                    wrep = [sbF("wrep%d" % i, [128, TG], F32) for i in range(2)]
                    b_wrep = [k.buf(), k.buf()]
                    yall = [sbF("yall%d" % i, [128, 2 * NE, 256], BF16) for i in range(2)]
                    b_yall = [k.buf(), k.buf()]
                    x2 = sbF("x2", [128, 2, D], F32)
                    b_x2 = [k.buf(), k.buf()]
                    tmpF = [sbF("tmpF%d" % i, [128, 512], F32) for i in range(2)]
                    b_tmpF = [k.buf(), k.buf()]
                    junkF = sbF("junkF", [128, D], BF16)
                    b_junkF = k.buf()
                    ssF = sbF("ssF", [128, NT], F32)
                    b_ssF = k.buf()
                    k.op("pool", lambda e: e.memset(ssF[:], 0.0), writes=[b_ssF])
                    y_v = y_s.rearrange("(eh p) n -> p eh n", p=128)
                    ntg = S // TG if (ALLP or "F_full" in phases) else 1
                    yc = 0
                    wc = 0
                    wcl = [0]

                    def build_PT_expert(tg_, ex):
                        PT_, bPT_ = PTs[tg_ % 2], b_PTs[tg_ % 2]
                        tsl_ = slice(tg_ * TG, (tg_ + 1) * TG)
                        pr, prb = next_bank()
                        mm(pr[:, 0:TG], sel[:, ex, :], rankT[:, tsl_], True, True, [b_sel, b_rankT], [prb], inc=False)
                        mm(pr[:, TG:2 * TG], sel[:, ex, :], wT[:, tsl_], True, True, [b_sel, b_wT], [prb], inc=True)
                        wr_, bwr = wrep[wcl[0] % 2], b_wrep[wcl[0] % 2]
                        wcl[0] += 1
                        cp("act", wr_[:], pr[:, TG:2 * TG], [prb], [bwr])
                        for half in range(2):
                            stt("dve", PT_[:, 2 * ex + half, :], pr[:, 0:TG], slotc[:, half:half + 1], wr_[:], ALU.is_equal, ALU.mult,
                                [prb, b_slotc, bwr], [bPT_])

                    for ex in range(NE):
                        build_PT_expert(0, ex)
                    for tg in range(ntg):
                        PT, b_PT = PTs[tg % 2], b_PTs[tg % 2]
                        for ti in range(2):
                            k.dma("act", x2[:, ti, :], x1_s[tg * TG + ti * 128:tg * TG + (ti + 1) * 128, :], writes=[b_x2[ti]])
                        for nb2 in range(16):
                            if tg + 1 < ntg:
                                build_PT_expert(tg + 1, nb2)
                            ya, bya = yall[yc % 2], b_yall[yc % 2]
                            yc += 1
                            k.dma("sp", ya[:], y_v[:, :, nb2 * 256:(nb2 + 1) * 256], writes=[bya])
                            for ti in range(2):
                                pb, pbb = next_bank()
                                for eh in range(2 * NE):
                                    mm(pb[:, 0:256], PT[:, eh, ti * 128:(ti + 1) * 128], ya[:, eh, :], eh == 0, eh == 2 * NE - 1,
                                       [b_PT, bya], [pbb], inc=(eh == 2 * NE - 1))
                                tf, btf = tmpF[ti], b_tmpF[ti]
                                csl = slice(nb2 * 256, (nb2 + 1) * 256)
                                tt("dve", tf[:, 0:256], pb[:, 0:256], g2rep[:, csl], ALU.mult, [pbb, b_g2], [btf])
                                tt("pool", x2[:, ti, csl], x2[:, ti, csl], tf[:, 0:256], ALU.add, [b_x2[ti], btf], [b_x2[ti]])
                        for ti in range(2):
                            col = tg * 2 + ti
                            actf(junkF[:], x2[:, ti, :], AF.Square, [b_x2[ti]], [b_junkF, b_ssF], accum_out=ssF[:, col:col + 1])
                            ts("dve", ssF[:, col:col + 1], ssF[:, col:col + 1], 1.0 / D, 1e-6, ALU.mult, ALU.add, [b_ssF], [b_ssF])
                            k.op("act", lambda e, col=col: e.sqrt(out=ssF[:, col:col + 1], in_=ssF[:, col:col + 1]),
                                 reads=[b_ssF], writes=[b_ssF])
                            k.op("dve", lambda e, col=col: e.reciprocal(out=ssF[:, col:col + 1], in_=ssF[:, col:col + 1]),
                                 reads=[b_ssF], writes=[b_ssF])
                            stt("dve", x2[:, ti, :], x2[:, ti, :], ssF[:, col:col + 1], nfrep[:], ALU.mult, ALU.mult,
                                [b_x2[ti], b_ssF, b_nf], [b_x2[ti]])
                            k.dma("sp", out[tg * TG + ti * 128:tg * TG + (ti + 1) * 128, :], x2[:, ti, :], reads=[b_x2[ti]],
                                  writes=[k.buf()])
                    k.barrier()

        for q in ("sp", "pool", "act"):
            e = k.eng[q]
            for sem, cnt in k.dsem[q]:
                if cnt > 0 and e.known.get(id(sem), 0) < cnt:
                    e.h.wait_ge(sem, cnt)
                    e.known[id(sem)] = cnt
    return nc


_PROG = {}
N_ACTIVE = 4


def _core_inputs(inp, b):
    f = lambda a: np.ascontiguousarray(np.asarray(a, dtype=np.float32))
    return {
        "x": f(inp["x"][b]), "c": f(inp["c"][b]).reshape(32, 128),
        "norm1_w": f(inp["norm1_w"][0]).reshape(32, 128), "norm2_w": f(inp["norm2_w"][0]).reshape(32, 128),
        "normf_w": f(inp["normf_w"]).reshape(32, 128), "w_ada": f(inp["w_ada"][0]),
        "b_ada": f(inp["b_ada"][0]).reshape(192, 128), "w_in": f(inp["w_in"][0]),
        "conv_w": f(inp["conv_w"][0]).reshape(5, 32, 128), "conv_b": f(inp["conv_b"][0]).reshape(32, 128),
        "dt_bias_f": f(inp["dt_bias_f"][0]), "dt_bias_b": f(inp["dt_bias_b"][0]),
        "a_log_f": f(inp["a_log_f"][0]), "a_log_b": f(inp["a_log_b"][0]), "d_skip": f(inp["d_skip"][0]),
        "ssm_norm_w": f(inp["ssm_norm_w"][0]).reshape(16, 128), "w_ssm_out": f(inp["w_ssm_out"][0]),
        "w_attn_out": f(inp["w_attn_out"][0]), "w_o": f(inp["w_o"][0]), "w_router": f(inp["w_router"][0]),
        "w_gate_e": f(inp["w_gate_e"][0]), "w_up_e": f(inp["w_up_e"][0]), "w_down_e": f(inp["w_down_e"][0]),
    }


def kernel(**inputs):
    if "nc" not in _PROG:
        _PROG["nc"] = build_program()
    nc = _PROG["nc"]
    shared = _core_inputs(inputs, 0)
    in_maps = []
    for b in range(N_ACTIVE):
        m = dict(shared)
        m["x"] = np.ascontiguousarray(np.asarray(inputs["x"][b], dtype=np.float32))
        m["c"] = np.ascontiguousarray(np.asarray(inputs["c"][b], dtype=np.float32)).reshape(32, 128)
        in_maps.append(m)
    res = run_bass_kernel_spmd(nc, in_maps, core_ids=list(range(N_ACTIVE)))
    return np.stack([np.asarray(res.results[b]["out"], dtype=np.float32) for b in range(N_ACTIVE)], axis=0)
```
